# Optimizing a Trainium2 kernel written in Bass

```python
import math
import jax
import jax.numpy as jnp
from jax import lax

D_MODEL = 1024
BATCH = 4
SEQ = 8192
DEPTH = 2

QBLOCK = 128
HEAD_DIM = 64
NSA_HEADS = 8
NSA_GROUPS = 2
NSA_HPG = NSA_HEADS // NSA_GROUPS
CMP_LEN = 32
CMP_STRIDE = 16
CMP_HIDDEN = 256
SEL_BLOCK = 64
N_SELECT = 16
WINDOW = 512
FORCE_BONUS = 1.0e4
DIFF_HEADS = 4
DIFF_DIM = 64
MLA_HEADS = 8
MLA_NOPE = 64
MLA_ROPE = 32
MLA_V = 64
Q_LORA = 256
KV_LORA = 128
ROPE_THETA = 10000.0
N_EXPERTS = 16
N_GROUPS = 4
EXPERTS_PER_GROUP = N_EXPERTS // N_GROUPS
TOP_K = 2
D_FF_EXPERT = 512
DEEPNORM_ALPHA = (2 * DEPTH) ** 0.25
DEEPNORM_BETA = (8 * DEPTH) ** -0.25
LN_EPS = 1e-5
RMS_EPS = 1e-6
NSA_WIDTH = NSA_HEADS * HEAD_DIM
NSA_KV = NSA_GROUPS * HEAD_DIM
DIFF_WIDTH = DIFF_HEADS * 2 * DIFF_DIM
MLA_WIDTH = MLA_HEADS * MLA_V
IN_SPLITS = (NSA_WIDTH,) + (NSA_KV,) * 6 + (3 * NSA_HEADS, DIFF_WIDTH, DIFF_WIDTH, DIFF_WIDTH, Q_LORA, KV_LORA, MLA_ROPE, 3 * D_MODEL)
IN_WIDTH = sum(IN_SPLITS)

kernel_name = 'hybrid_nsa_diff_mla_moe_deepnorm'


def layer_norm(x, g, b):
    xf = x.astype(jnp.float32)
    mu = jnp.mean(xf, -1, keepdims=True)
    var = jnp.mean(jnp.square(xf - mu), -1, keepdims=True)
    return ((xf - mu) * lax.rsqrt(var + LN_EPS)).astype(x.dtype) * g + b


def rms_norm(x, g):
    xf = x.astype(jnp.float32)
    return (xf * lax.rsqrt(jnp.mean(xf * xf, -1, keepdims=True) + RMS_EPS)).astype(x.dtype) * g


def masked_softmax(s, mask):
    s = jnp.where(mask, s, -1e30)
    mx = jnp.max(s, -1, keepdims=True)
    e = jnp.where(mask, jnp.exp(s - mx), 0.0)
    return e / jnp.maximum(jnp.sum(e, -1, keepdims=True), 1e-30)


def alibi_slopes():
    n = NSA_HEADS + DIFF_HEADS
    return 2.0 ** (-8.0 * jnp.arange(1, n + 1, dtype=jnp.float32) / n)


def rope(x, pos):
    half = x.shape[-1] // 2
    freqs = ROPE_THETA ** (-jnp.arange(half, dtype=jnp.float32) / half)
    ang = pos.astype(jnp.float32)[:, None] * freqs[None, :]
    shape = (1, x.shape[1]) + (1,) * (x.ndim - 3) + (half,)
    cos = jnp.cos(ang).reshape(shape).astype(x.dtype)
    sin = jnp.sin(ang).reshape(shape).astype(x.dtype)
    x1, x2 = x[..., :half], x[..., half:]
    return jnp.concatenate([x1 * cos - x2 * sin, x1 * sin + x2 * cos], -1)


def split_cols(a, widths):
    out, o = [], 0
    for w in widths:
        out.append(a[..., o:o + w])
        o += w
    return out


def sweep_query_blocks(block_fn, seq):
    n = seq // QBLOCK
    out = lax.map(block_fn, jnp.arange(n, dtype=jnp.int32) * QBLOCK)
    out = jnp.moveaxis(out, 0, 1)
    return out.reshape((out.shape[0], n * QBLOCK) + out.shape[3:])


def compress_blocks(x, pos_emb, w1, w2):
    B, S, G, Dh = x.shape
    ratio = CMP_LEN // CMP_STRIDE
    n_chunk = S // CMP_STRIDE
    n_cmp = n_chunk - ratio + 1
    chunks = x.reshape(B, n_chunk, CMP_STRIDE, G, Dh)
    blocks = jnp.concatenate([chunks[:, j:j + n_cmp] for j in range(ratio)], axis=2)
    blocks = blocks + pos_emb[:, None, :]
    flat = jnp.moveaxis(blocks, 3, 2).reshape(B, n_cmp, G, CMP_LEN * Dh)
    return jax.nn.gelu(flat @ w1) @ w2


def nsa_mixer(q, kc, vc, ks, vs, kw, vw, gates, cmp_pos_k, cmp_w1_k, cmp_w2_k, cmp_pos_v, cmp_w1_v, cmp_w2_v, slopes):
    B, S = q.shape[:2]
    G, Dh = NSA_GROUPS, HEAD_DIM
    scale = Dh ** -0.5
    ratio = CMP_LEN // CMP_STRIDE
    n_chunk = S // CMP_STRIDE
    n_sb = S // SEL_BLOCK
    n_sel = min(N_SELECT, n_sb)
    chunks_per_sb = SEL_BLOCK // CMP_STRIDE
    k_cmp = compress_blocks(kc, cmp_pos_k, cmp_w1_k, cmp_w2_k)
    v_cmp = compress_blocks(vc, cmp_pos_v, cmp_w1_v, cmp_w2_v)
    n_cmp = k_cmp.shape[1]
    cmp_end = jnp.arange(n_cmp) * CMP_STRIDE + (CMP_LEN - 1)
    ks_blk = ks.transpose(0, 2, 1, 3).reshape(B, G, n_sb, SEL_BLOCK, Dh)
    vs_blk = vs.transpose(0, 2, 1, 3).reshape(B, G, n_sb, SEL_BLOCK, Dh)
    kw_pad = jnp.pad(kw, ((0, 0), (WINDOW, 0), (0, 0), (0, 0)))
    vw_pad = jnp.pad(vw, ((0, 0), (WINDOW, 0), (0, 0), (0, 0)))
    slope = slopes.reshape(G, NSA_HPG)[None, :, :, None, None]
    sb_ids = jnp.arange(n_sb)
    gather_blocks = jax.vmap(jax.vmap(lambda a, i: a[i]))

    def block(q0):
        t = q0 + jnp.arange(QBLOCK)
        qb = lax.dynamic_slice_in_dim(q, q0, QBLOCK, 1).reshape(B, QBLOCK, G, NSA_HPG, Dh)
        gb = lax.dynamic_slice_in_dim(gates, q0, QBLOCK, 1).reshape(B, QBLOCK, G, NSA_HPG, 3)
        s_c = jnp.einsum('bqghd,bcgd->bghqc', qb, k_cmp).astype(jnp.float32) * scale
        dist_c = t[:, None] - cmp_end[None, :]
        p_c = masked_softmax(s_c - slope * dist_c.astype(jnp.float32), dist_c >= 0)
        o_c = jnp.einsum('bghqc,bcgd->bqghd', p_c.astype(q.dtype), v_cmp)
        p_grp = jnp.sum(p_c, 2)
        p_pad = jnp.pad(p_grp, ((0, 0), (0, 0), (0, 0), (ratio - 1, ratio - 1)))
        chunk = p_pad[..., ratio - 1:ratio - 1 + n_chunk]
        for j in range(1, ratio):
            chunk = chunk + p_pad[..., ratio - 1 - j:ratio - 1 - j + n_chunk]
        sb_score = chunk.reshape(B, G, QBLOCK, n_sb, chunks_per_sb).sum(-1)
        cur = t // SEL_BLOCK
        forced = (sb_ids[None, :] == 0) | (sb_ids[None, :] == cur[:, None]) | (sb_ids[None, :] == cur[:, None] - 1)
        started = sb_ids[None, :] * SEL_BLOCK <= t[:, None]
        sb_score = jnp.where(forced, sb_score + FORCE_BONUS, sb_score)
        sb_score = jnp.where(started, sb_score, -FORCE_BONUS)
        _, sel = lax.top_k(sb_score, n_sel)
        sel_flat = sel.reshape(B, G, QBLOCK * n_sel)
        k_sel = gather_blocks(ks_blk, sel_flat).reshape(B, G, QBLOCK, n_sel * SEL_BLOCK, Dh)
        v_sel = gather_blocks(vs_blk, sel_flat).reshape(B, G, QBLOCK, n_sel * SEL_BLOCK, Dh)
        pos_sel = (sel[..., None] * SEL_BLOCK + jnp.arange(SEL_BLOCK)).reshape(B, G, QBLOCK, n_sel * SEL_BLOCK)
        dist_s = (t[None, None, :, None] - pos_sel)[:, :, None]
        s_s = jnp.einsum('bqghd,bgqkd->bghqk', qb, k_sel).astype(jnp.float32) * scale
        p_s = masked_softmax(s_s - slope * dist_s.astype(jnp.float32), dist_s >= 0)
        o_s = jnp.einsum('bghqk,bgqkd->bqghd', p_s.astype(q.dtype), v_sel)
        k_w = lax.dynamic_slice_in_dim(kw_pad, q0, WINDOW + QBLOCK, 1)
        v_w = lax.dynamic_slice_in_dim(vw_pad, q0, WINDOW + QBLOCK, 1)
        s_pos = q0 - WINDOW + jnp.arange(WINDOW + QBLOCK)
        dist_w = t[:, None] - s_pos[None, :]
        mask_w = (dist_w >= 0) & (dist_w < WINDOW) & (s_pos[None, :] >= 0)
        s_w = jnp.einsum('bqghd,bkgd->bghqk', qb, k_w).astype(jnp.float32) * scale
        p_w = masked_softmax(s_w - slope * dist_w.astype(jnp.float32), mask_w)
        o_w = jnp.einsum('bghqk,bkgd->bqghd', p_w.astype(q.dtype), v_w)
        o = gb[..., 0:1] * o_c + gb[..., 1:2] * o_s + gb[..., 2:3] * o_w
        return o.reshape(B, QBLOCK, NSA_HEADS * Dh)

    return sweep_query_blocks(block, S)


def diff_mixer(q, k, v, lam_params, subln_g, lam_init, slopes):
    B, S = q.shape[:2]
    d = DIFF_DIM
    scale = d ** -0.5
    q1, q2 = q[..., :d], q[..., d:]
    k1, k2 = k[..., :d], k[..., d:]
    lp = lam_params.astype(jnp.float32)
    lam = jnp.exp(jnp.sum(lp[0] * lp[1])) - jnp.exp(jnp.sum(lp[2] * lp[3])) + lam_init
    slope = slopes[None, :, None, None]
    key_pos = jnp.arange(S)

    def block(q0):
        t = q0 + jnp.arange(QBLOCK)
        dist = t[:, None] - key_pos[None, :]
        mask = dist >= 0
        bias = -slope * dist.astype(jnp.float32)
        q1b = lax.dynamic_slice_in_dim(q1, q0, QBLOCK, 1)
        q2b = lax.dynamic_slice_in_dim(q2, q0, QBLOCK, 1)
        a1 = masked_softmax(jnp.einsum('bqhd,bkhd->bhqk', q1b, k1).astype(jnp.float32) * scale + bias, mask)
        a2 = masked_softmax(jnp.einsum('bqhd,bkhd->bhqk', q2b, k2).astype(jnp.float32) * scale + bias, mask)
        a = a1 - lam * a2
        return jnp.einsum('bhqk,bkhd->bqhd', a.astype(v.dtype), v)

    o = sweep_query_blocks(block, S)
    o = rms_norm(o, subln_g) * (1.0 - lam_init)
    return o.reshape(B, S, DIFF_HEADS * 2 * d)


def mla_mixer(c_q, c_kv, k_r, q_norm_g, kv_norm_g, w_uq, w_ukv):
    B, S = c_q.shape[:2]
    pos = jnp.arange(S)
    q = (rms_norm(c_q, q_norm_g) @ w_uq).reshape(B, S, MLA_HEADS, MLA_NOPE + MLA_ROPE)
    q_n, q_r = q[..., :MLA_NOPE], rope(q[..., MLA_NOPE:], pos)
    kv = (rms_norm(c_kv, kv_norm_g) @ w_ukv).reshape(B, S, MLA_HEADS, MLA_NOPE + MLA_V)
    k_n, v = kv[..., :MLA_NOPE], kv[..., MLA_NOPE:]
    k_r = rope(k_r, pos)
    scale = (MLA_NOPE + MLA_ROPE) ** -0.5
    key_pos = jnp.arange(S)

    def block(q0):
        t = q0 + jnp.arange(QBLOCK)
        qn = lax.dynamic_slice_in_dim(q_n, q0, QBLOCK, 1)
        qr = lax.dynamic_slice_in_dim(q_r, q0, QBLOCK, 1)
        s = (jnp.einsum('bqhd,bkhd->bhqk', qn, k_n) + jnp.einsum('bqhr,bkr->bhqk', qr, k_r)).astype(jnp.float32) * scale
        p = masked_softmax(s, key_pos[None, :] <= t[:, None])
        return jnp.einsum('bhqk,bkhd->bqhd', p.astype(v.dtype), v)

    o = sweep_query_blocks(block, S)
    return o.reshape(B, S, MLA_HEADS * MLA_V)


def mixer_block(h, layer, w_in, cmp_pos_k, cmp_w1_k, cmp_w2_k, cmp_pos_v, cmp_w1_v, cmp_w2_v, diff_lambda, diff_subln_g, mla_q_norm_g, mla_kv_norm_g, mla_w_uq, mla_w_ukv, w_br_nsa, w_br_diff, w_br_mla, w_out):
    B, S, _ = h.shape
    nq, kc, vc, ks, vs, kw, vw, ng, dq, dk, dv, cq, ckv, kr, mg = split_cols(h @ w_in, IN_SPLITS)

    def heads(a, n):
        return a.reshape(B, S, n, -1)

    slopes = alibi_slopes()
    o_nsa = nsa_mixer(heads(nq, NSA_HEADS), heads(kc, NSA_GROUPS), heads(vc, NSA_GROUPS), heads(ks, NSA_GROUPS), heads(vs, NSA_GROUPS), heads(kw, NSA_GROUPS), heads(vw, NSA_GROUPS), jax.nn.sigmoid(heads(ng, NSA_HEADS)), cmp_pos_k, cmp_w1_k, cmp_w2_k, cmp_pos_v, cmp_w1_v, cmp_w2_v, slopes[:NSA_HEADS])
    lam_init = 0.8 - 0.6 * math.exp(-0.3 * layer)
    o_diff = diff_mixer(heads(dq, DIFF_HEADS), heads(dk, DIFF_HEADS), heads(dv, DIFF_HEADS), diff_lambda, diff_subln_g, lam_init, slopes[NSA_HEADS:])
    o_mla = mla_mixer(cq, ckv, kr, mla_q_norm_g, mla_kv_norm_g, mla_w_uq, mla_w_ukv)
    g = jax.nn.sigmoid(mg).reshape(B, S, 3, D_MODEL)
    y = g[:, :, 0] * (o_nsa @ w_br_nsa) + g[:, :, 1] * (o_diff @ w_br_diff) + g[:, :, 2] * (o_mla @ w_br_mla)
    return y @ w_out


def moe_ffn(h, router_w, router_b, w1, w3, w2):
    B, S, D = h.shape
    hf = h.reshape(B * S, D)
    aff = jax.nn.sigmoid((hf @ router_w).astype(jnp.float32))
    sel = (aff + router_b.astype(jnp.float32)).reshape(-1, N_GROUPS, EXPERTS_PER_GROUP)
    grp_score = jnp.sum(lax.top_k(sel, TOP_K)[0], -1)
    grp = jnp.argmax(grp_score, -1)
    in_grp = jnp.einsum('tge,tg->te', sel, jax.nn.one_hot(grp, N_GROUPS, dtype=jnp.float32))
    _, local = lax.top_k(in_grp, TOP_K)
    eidx = grp[:, None] * EXPERTS_PER_GROUP + local
    gate = jnp.take_along_axis(aff, eidx, axis=1)
    gate = gate / jnp.sum(gate, -1, keepdims=True)
    combine = jnp.einsum('tk,tke->te', gate, jax.nn.one_hot(eidx, N_EXPERTS, dtype=jnp.float32)).astype(h.dtype)
    y = jnp.zeros_like(hf)
    for e in range(N_EXPERTS):
        y = y + combine[:, e:e + 1] * ((jax.nn.silu(hf @ w1[e]) * (hf @ w3[e])) @ w2[e])
    return y.reshape(B, S, D)


def setup_inputs(seed: int = 0) -> dict:
    key = jax.random.key(seed)
    ks = jax.random.split(key, 32)
    L, D = DEPTH, D_MODEL

    def nrm(k, shape, scale):
        return jax.random.normal(k, shape, jnp.float32) * scale

    return {
        'x': nrm(ks[0], (BATCH, SEQ, D), 1.0),
        'ln_in_g': 1.0 + nrm(ks[1], (D,), 0.02),
        'ln_in_b': nrm(ks[2], (D,), 0.02),
        'w_in': nrm(ks[3], (L, D, IN_WIDTH), D ** -0.5),
        'cmp_pos_k': nrm(ks[4], (L, CMP_LEN, HEAD_DIM), 0.1),
        'cmp_w1_k': nrm(ks[5], (L, CMP_LEN * HEAD_DIM, CMP_HIDDEN), (CMP_LEN * HEAD_DIM) ** -0.5),
        'cmp_w2_k': nrm(ks[6], (L, CMP_HIDDEN, HEAD_DIM), CMP_HIDDEN ** -0.5),
        'cmp_pos_v': nrm(ks[7], (L, CMP_LEN, HEAD_DIM), 0.1),
        'cmp_w1_v': nrm(ks[8], (L, CMP_LEN * HEAD_DIM, CMP_HIDDEN), (CMP_LEN * HEAD_DIM) ** -0.5),
        'cmp_w2_v': nrm(ks[9], (L, CMP_HIDDEN, HEAD_DIM), CMP_HIDDEN ** -0.5),
        'diff_lambda': nrm(ks[10], (L, 4, DIFF_DIM), 0.1),
        'diff_subln_g': 1.0 + nrm(ks[11], (L, 2 * DIFF_DIM), 0.02),
        'mla_q_norm_g': 1.0 + nrm(ks[12], (L, Q_LORA), 0.02),
        'mla_kv_norm_g': 1.0 + nrm(ks[13], (L, KV_LORA), 0.02),
        'mla_w_uq': nrm(ks[14], (L, Q_LORA, MLA_HEADS * (MLA_NOPE + MLA_ROPE)), Q_LORA ** -0.5),
        'mla_w_ukv': nrm(ks[15], (L, KV_LORA, MLA_HEADS * (MLA_NOPE + MLA_V)), KV_LORA ** -0.5),
        'w_br_nsa': nrm(ks[16], (L, NSA_WIDTH, D), NSA_WIDTH ** -0.5),
        'w_br_diff': nrm(ks[17], (L, DIFF_WIDTH, D), DIFF_WIDTH ** -0.5),
        'w_br_mla': nrm(ks[18], (L, MLA_WIDTH, D), MLA_WIDTH ** -0.5),
        'w_out': nrm(ks[19], (L, D, D), D ** -0.5 * DEEPNORM_BETA),
        'ln1_g': 1.0 + nrm(ks[20], (L, D), 0.02),
        'ln1_b': nrm(ks[21], (L, D), 0.02),
        'router_w': nrm(ks[22], (D, N_EXPERTS), D ** -0.5),
        'router_b': nrm(ks[23], (N_EXPERTS,), 0.01),
        'moe_w1': nrm(ks[24], (L, N_EXPERTS, D, D_FF_EXPERT), D ** -0.5),
        'moe_w3': nrm(ks[25], (L, N_EXPERTS, D, D_FF_EXPERT), D ** -0.5),
        'moe_w2': nrm(ks[26], (L, N_EXPERTS, D_FF_EXPERT, D), D_FF_EXPERT ** -0.5 * DEEPNORM_BETA),
        'ln2_g': 1.0 + nrm(ks[27], (L, D), 0.02),
        'ln2_b': nrm(ks[28], (L, D), 0.02),
    }


def reference(x, ln_in_g, ln_in_b, w_in, cmp_pos_k, cmp_w1_k, cmp_w2_k, cmp_pos_v, cmp_w1_v, cmp_w2_v, diff_lambda, diff_subln_g, mla_q_norm_g, mla_kv_norm_g, mla_w_uq, mla_w_ukv, w_br_nsa, w_br_diff, w_br_mla, w_out, ln1_g, ln1_b, router_w, router_b, moe_w1, moe_w3, moe_w2, ln2_g, ln2_b):
    h = layer_norm(x, ln_in_g, ln_in_b)
    for l in range(DEPTH):
        mix = mixer_block(h, l, w_in[l], cmp_pos_k[l], cmp_w1_k[l], cmp_w2_k[l], cmp_pos_v[l], cmp_w1_v[l], cmp_w2_v[l], diff_lambda[l], diff_subln_g[l], mla_q_norm_g[l], mla_kv_norm_g[l], mla_w_uq[l], mla_w_ukv[l], w_br_nsa[l], w_br_diff[l], w_br_mla[l], w_out[l])
        h = layer_norm(DEEPNORM_ALPHA * h + mix, ln1_g[l], ln1_b[l])
        ffn = moe_ffn(h, router_w, router_b, moe_w1[l], moe_w3[l], moe_w2[l])
        h = layer_norm(DEEPNORM_ALPHA * h + ffn, ln2_g[l], ln2_b[l])
    return h
```

```python
import numpy as np
from contextlib import ExitStack
import concourse.bass as bass
import concourse.mybir as mybir

F32 = mybir.dt.float32
BF16 = mybir.dt.bfloat16
AF = mybir.ActivationFunctionType
ALU = mybir.AluOpType
AX = mybir.AxisListType

COMPUTE = ("pe", "act", "dve", "pool")
DMAQ = ("sp", "poolq", "actq")
NSLOT = 32
NSLOT_HW = 18


class Buf:
    __slots__ = ("w", "r")

    def __init__(self):
        self.w = None
        self.r = []


class Op:
    __slots__ = ("q", "fn", "deps", "inc", "sem", "val", "is_dma", "slot", "incv")

    def __init__(self, q, fn, is_dma):
        self.q = q
        self.fn = fn
        self.deps = []
        self.inc = False
        self.sem = None
        self.val = 0
        self.is_dma = is_dma
        self.slot = -1
        self.incv = 16


class V:
    __slots__ = ("ap", "bufs")

    def __init__(self, ap, bufs):
        self.ap = ap
        self.bufs = bufs


class Tl:
    def __init__(self, t):
        self.t = t
        self._b = {}

    def buf(self, key=None):
        b = self._b.get(key)
        if b is None:
            b = self._b[key] = Buf()
        return b

    def __getitem__(self, idx):
        return V(self.t[idx], [self.buf(None)])

    def k(self, key):
        return _Keyed(self, key)

    def ks(self, keys, idx):
        return V(self.t[idx], [self.buf(k) for k in keys])


class _Keyed:
    def __init__(self, tl, key):
        self.tl = tl
        self.key = key

    def __getitem__(self, idx):
        return V(self.tl.t[idx], [self.tl.buf(self.key)])


def rv(v, ap):
    return V(ap, v.bufs)


class KB:
    def __init__(self):
        self.nc = bass.Bass("TRN2", target_bir_lowering=False, num_devices=8)
        self.es = ExitStack()
        self.ops = {q: [] for q in ("pe", "act", "dve", "pool", "sp")}
        self.allops = []
        self.ndma = 0
        self.ndma_sw = 0
        self.slot_last = [None] * NSLOT
        self.n = 0
        self.outs = []

    def sb(self, shape, dt=F32, name=None):
        self.n += 1
        return Tl(self.es.enter_context(self.nc.sbuf_tensor(name or "sb%d" % self.n, list(shape), dt)))

    def ps(self, shape, dt=F32, name=None):
        self.n += 1
        return Tl(self.es.enter_context(self.nc.psum_tensor(name or "ps%d" % self.n, list(shape), dt)))

    def dram(self, name, shape, dt, kind):
        return Tl(self.nc.dram_tensor(name, list(shape), dt, kind=kind).ap())

    def inp(self, name, shape, dt):
        return self.dram(name, shape, dt, "ExternalInput")

    def out(self, name, shape, dt):
        return self.dram(name, shape, dt, "ExternalOutput")

    def rec(self, q, fn, reads, writes):
        is_dma = q in DMAQ
        o = Op(q, fn, is_dma)
        deps = set()
        for b in reads:
            if b.w is not None:
                deps.add(b.w)
        for b in writes:
            if b.w is not None:
                deps.add(b.w)
            for r in b.r:
                deps.add(r)
        if is_dma:
            if q == "poolq":
                o.slot = NSLOT_HW + (self.ndma_sw % (NSLOT - NSLOT_HW))
                self.ndma_sw += 1
            else:
                o.slot = self.ndma % NSLOT_HW
                self.ndma += 1
            prev = self.slot_last[o.slot]
            if prev is not None:
                deps.add(prev)
            self.slot_last[o.slot] = o
            o.inc = True
        for d in deps:
            if d is o:
                continue
            if (not d.is_dma) and (not is_dma) and d.q == "pe" and q == "pe":
                continue
            d.inc = True
            o.deps.append(d)
        for b in reads:
            b.r.append(o)
        for b in writes:
            b.w = o
            b.r = []
        st = {"poolq": "pool", "actq": "act"}.get(q, q)
        self.ops[st].append(o)
        self.allops.append(o)
        return o

    def op(self, q, meth, outs, ins, *args, **kw):
        reads = [b for v in ins.values() for b in v.bufs]
        writes = [b for v in outs.values() for b in v.bufs]
        kwargs = dict(kw)
        for k, v in outs.items():
            kwargs[k] = v.ap
        for k, v in ins.items():
            kwargs[k] = v.ap

        def fn(e):
            return getattr(e, meth)(*args, **kwargs)
        return self.rec(q, fn, reads, writes)

    def dma(self, out, in_, q="sp"):
        o = self.op(q, "dma_start", {"out": out}, {"in_": in_})
        return o

    def mm(self, out, lhsT, rhs, start=True, stop=True):
        ins = {"lhsT": lhsT, "rhs": rhs}
        reads = [b for v in ins.values() for b in v.bufs]
        writes = list(out.bufs)
        oa, la, ra = out.ap, lhsT.ap, rhs.ap

        def fn(e):
            return e.matmul(oa, la, ra, start=start, stop=stop)
        return self.rec("pe", fn, reads + (writes if not start else []), writes)

    def tr(self, out, in_, ident):
        oa, ia, da = out.ap, in_.ap, ident.ap

        def fn(e):
            return e.transpose(oa, ia, da)
        return self.rec("pe", fn, in_.bufs + ident.bufs, out.bufs)

    def act(self, out, in_, func, bias=None, scale=None, q="act"):
        ins = {"in_": in_}
        kw = {}
        if isinstance(bias, V):
            ins["bias"] = bias
        elif bias is not None:
            kw["bias"] = bias
        if isinstance(scale, V):
            ins["scale"] = scale
        elif scale is not None:
            kw["scale"] = scale
        return self.op(q, "activation", {"out": out}, ins, func=func, **kw)

    def tt(self, out, in0, in1, op, q="dve"):
        return self.op(q, "tensor_tensor", {"out": out}, {"in0": in0, "in1": in1}, op=op)

    def ts(self, out, in0, s1, op0, s2=None, op1=None, q="dve"):
        ins = {"in0": in0}
        kw = {}
        if isinstance(s1, V):
            ins["scalar1"] = s1
        else:
            kw["scalar1"] = s1
        if isinstance(s2, V):
            ins["scalar2"] = s2
        else:
            kw["scalar2"] = s2
        if op1 is not None:
            kw["op1"] = op1
        return self.op(q, "tensor_scalar", {"out": out}, ins, op0=op0, **kw)

    def copy(self, out, in_, q="dve"):
        if q == "act":
            return self.op("act", "copy", {"out": out}, {"in_": in_})
        return self.op(q, "tensor_copy", {"out": out}, {"in_": in_})

    def memset(self, out, val, q="pool"):
        return self.op(q, "memset", {}, {}, out.ap, val) if False else self._memset(out, val, q)

    def _memset(self, out, val, q):
        oa = out.ap

        def fn(e):
            return e.memset(oa, val)
        return self.rec(q, fn, [], out.bufs)

    def cc(self, kind, rg, in_, out, incv=1):
        ia, oa = in_.ap.opt(), out.ap.opt()

        def fn(e):
            return e.collective_compute(kind, ALU.bypass, replica_groups=rg, ins=[ia], outs=[oa])
        o = self.rec("poolq", fn, in_.bufs, out.bufs)
        o.incv = incv
        return o

    def barrier(self):
        lasts = [self.ops[st][-1] for st in self.ops if self.ops[st]] + [o for o in self.slot_last if o is not None]
        for st in ("pe", "act", "dve", "pool", "sp"):
            o = Op(st, (lambda e: e.nop()), False)
            for d in lasts:
                d.inc = True
                o.deps.append(d)
            self.ops[st].append(o)
            self.allops.append(o)

    class _Scope:
        def __init__(self, kb):
            self.kb = kb

        def __enter__(self):
            self.old = self.kb.es
            self.kb.es = ExitStack()
            return self

        def __exit__(self, *a):
            self.kb.barrier()
            self.kb.es.close()
            self.kb.es = self.old

    def scope(self):
        return KB._Scope(self)

    def finish(self):
        nc = self.nc
        final = [o for o in self.outs]
        with ExitStack() as es:
            esem = {q: es.enter_context(nc.semaphore("s_" + q)) for q in COMPUTE + ("sp",)}
            dsem = [es.enter_context(nc.semaphore("d_%d" % i)) for i in range(NSLOT)]
            cnt = {q: 0 for q in COMPUTE + ("sp",)}
            dcnt = [0] * NSLOT
            for o in self.allops:
                if not o.inc:
                    continue
                if o.is_dma:
                    dcnt[o.slot] += o.incv
                    o.sem = dsem[o.slot]
                    o.val = dcnt[o.slot]
                else:
                    cnt[o.q] += 1
                    o.sem = esem[o.q]
                    o.val = cnt[o.q]
            block = es.enter_context(nc.Block())
            streams = self.ops

            def run(st, eng):
                waited = {}
                for o in streams[st]:
                    for d in o.deps:
                        key = id(d.sem)
                        if waited.get(key, 0) >= d.val:
                            continue
                        eng.wait_ge(d.sem, d.val)
                        waited[key] = d.val
                    ins = o.fn(eng)
                    if o.inc:
                        ins.then_inc(o.sem, o.incv if o.is_dma else 1)
                if st == "sp":
                    for o in final:
                        if waited.get(id(o.sem), 0) < o.val:
                            eng.wait_ge(o.sem, o.val)
                            waited[id(o.sem)] = o.val

            block.tensor(lambda e: run("pe", e))
            block.scalar(lambda e: run("act", e))
            block.vector(lambda e: run("dve", e))
            block.gpsimd(lambda e: run("pool", e))
            block.sync(lambda e: run("sp", e))
        self.es.close()
        return nc

import math
import numpy as np
import ml_dtypes
from concourse.bass_utils import run_bass_kernel_spmd

NPBF = ml_dtypes.bfloat16
D = 1024
T = 8192
S = 8192
NCH = T // 512
EPS_LN = 1e-5
EPS_RMS = 1e-6
ALPHA = 4 ** 0.25
OFF = {}
_o = 0
for _n, _w in [("nq", 512), ("kc", 128), ("vc", 128), ("ks", 128), ("vs", 128), ("kw", 128), ("vw", 128), ("ng", 24),
               ("dq", 512), ("dk", 512), ("dv", 512), ("cq", 256), ("ckv", 128), ("kr", 32), ("mg", 3072)]:
    OFF[_n] = (_o, _w)
    _o += _w
NA = OFF["mg"][0]


def run(nc, in_maps):
    res = run_bass_kernel_spmd(nc, in_maps, core_ids=list(range(8)))
    return res.results


def consts(K):
    c = {}
    c["ones32"] = K.sb([128, 128], F32)
    K.memset(c["ones32"][:, :], 1.0)
    c["onesb"] = K.sb([128, 128], BF16)
    K.memset(c["onesb"][:, :], 1.0)
    ident = K.sb([128, 128], F32)
    K.memset(ident[:, :], 0.0)
    K.op("pool", "affine_select", {"out": ident[:, :]}, {"in_": ident[:, :]}, pattern=[[-1, 128]],
         compare_op=ALU.not_equal, fill=1.0, base=0, channel_multiplier=1)
    c["ident32"] = ident
    return c


def ln_fm(K, c, src, g, b, out32, outb, psA, psB, tmp, N=512, eps=EPS_LN, nft=8, pre=None):
    sq = tmp["sq"]
    Dn = nft * 128
    for ft in range(nft):
        K.mm(psA[:, :N], c["ones32"][:, :], src[:, ft, :], start=(ft == 0), stop=(ft == nft - 1))
    for ft in range(nft):
        K.act(sq[:, ft, :], src[:, ft, :], AF.Square)
    for ft in range(nft):
        K.mm(psB[:, :N], c["ones32"][:, :], sq[:, ft, :], start=(ft == 0), stop=(ft == nft - 1))
    mean, msq, rstd = tmp["mean"], tmp["msq"], tmp["rstd"]
    K.ts(mean[:, :], psA[:, :N], 1.0 / Dn, ALU.mult)
    K.tt(msq[:, :], mean[:, :], mean[:, :], ALU.mult)
    K.op("dve", "scalar_tensor_tensor", {"out": rstd[:, :]}, {"in0": psB[:, :N], "in1": msq[:, :]},
         scalar=1.0 / Dn, op0=ALU.mult, op1=ALU.subtract)
    K.ts(rstd[:, :], rstd[:, :], eps, ALU.add)
    K.act(rstd[:, :], rstd[:, :], AF.Sqrt)
    K.op("dve", "reciprocal", {"out": rstd[:, :]}, {"in_": rstd[:, :]})
    for ft in range(nft):
        t = sq
        K.tt(t[:, ft, :], src[:, ft, :], mean[:, :], ALU.subtract)
        K.tt(t[:, ft, :], t[:, ft, :], rstd[:, :], ALU.mult, q="pool")
        K.act(out32[:, ft, :], t[:, ft, :], AF.Identity, bias=b[:, ft:ft + 1], scale=g[:, ft:ft + 1])
        if outb is not None:
            K.copy(outb[:, ft, :], out32[:, ft, :], q="act")


def rms_fm(K, c, src, g, outb, psA, tmp, nft, N=512):
    sq = tmp["sq"]
    Dn = nft * 128
    for ft in range(nft):
        K.act(sq[:, ft, :], src[:, ft, :], AF.Square)
    for ft in range(nft):
        K.mm(psA[:, :N], c["ones32"][:, :], sq[:, ft, :], start=(ft == 0), stop=(ft == nft - 1))
    rstd = tmp["rstd"]
    K.ts(rstd[:, :], psA[:, :N], 1.0 / Dn, ALU.mult, EPS_RMS, ALU.add)
    K.act(rstd[:, :], rstd[:, :], AF.Sqrt)
    K.op("dve", "reciprocal", {"out": rstd[:, :]}, {"in_": rstd[:, :]})
    for ft in range(nft):
        K.tt(sq[:, ft, :], src[:, ft, :], rstd[:, :], ALU.mult)
        K.act(outb[:, ft, :], sq[:, ft, :], AF.Copy if False else AF.Identity, scale=g[:, ft:ft + 1])


def build_A(K, c, layer0, io):
    if layer0:
        x = io["x"]; lng = io["ln_g"]; lnb = io["ln_b"]
        hT_out = io["hT32"]
    else:
        hT_in = io["hT32"]
    w_in = io["w_in"]; w_uq = io["w_uq"]; w_ukv = io["w_ukv"]
    qg = io["qg"]; kvg = io["kvg"]; cosT = io["cosT"]; sinT = io["sinT"]; protT = io["protT"]
    fm_outs = {n: io[n] for n in ("nqT", "kcT", "vcT", "ksT", "kwT", "ngT", "dqT", "dkT", "qmT", "kropeT", "knopeT")}
    tm_outs = {n: io[n] for n in ("vs", "vw", "dv", "vm")}

    W = K.sb([128, 8, NA], BF16)
    for kt in range(8):
        K.dma(W.k(kt)[:, kt, :], w_in[kt * 128:(kt + 1) * 128, 0:NA], q="sp")
    Wuq = K.sb([128, 2, 768], BF16)
    for kt in range(2):
        K.dma(Wuq[:, kt, :], w_uq[kt * 128:(kt + 1) * 128, :])
    Wukv = K.sb([128, 1024], BF16)
    K.dma(Wukv[:, :], w_ukv[:, :])
    qg_s = K.sb([128, 2]); K.dma(qg_s[:, :], qg[:, :])
    kvg_s = K.sb([128, 1]); K.dma(kvg_s[:, :], kvg[:, :])
    prot_s = K.sb([32, 32]); K.dma(prot_s[:, :], protT[:, :])
    if layer0:
        g_s = K.sb([128, 8]); K.dma(g_s[:, :], lng[:, :])
        b_s = K.sb([128, 8]); K.dma(b_s[:, :], lnb[:, :])

    def Wv(kt, c0, n):
        return W.k(kt)[:, kt, c0:c0 + n]

    ps = [K.ps([128, 512]) for _ in range(8)]
    pi = [0]

    def nps():
        pi[0] = (pi[0] + 1) % 6
        return ps[pi[0]]
    psA, psB = ps[6], ps[7]

    xT = K.sb([128, 8, 512], F32)
    tmp = {"sq": K.sb([128, 8, 512], F32), "mean": K.sb([128, 512]), "msq": K.sb([128, 512]), "rstd": K.sb([128, 512])}
    h32 = K.sb([128, 8, 512], F32)
    hb = K.sb([128, 8, 512], BF16)
    xc = [K.sb([128, 4, D], F32) for _ in range(2)] if layer0 else None
    stg = [K.sb([128, 512], BF16) for _ in range(4)]
    si = [0]

    def nstg():
        si[0] = (si[0] + 1) % 4
        return stg[si[0]]
    cq32 = K.sb([128, 2, 512], F32)
    cqb = K.sb([128, 2, 512], BF16)
    ckv32 = K.sb([128, 1, 512], F32)
    ckvb = K.sb([128, 1, 512], BF16)
    cos_s = K.sb([32, 512]); sin_s = K.sb([32, 512])
    xs32 = K.sb([96, 512], F32)
    r1 = K.sb([32, 512], F32); r2 = K.sb([32, 512], F32)
    qstg = [K.sb([96, 512], BF16) for _ in range(2)]

    evq = [0]

    def evac(out, in_, func=None):
        evq[0] += 1
        if func is not None:
            K.act(out, in_, func)
        elif evq[0] % 2:
            K.copy(out, in_, q="act")
        else:
            K.copy(out, in_, q="dve")

    for ch in range(NCH):
        t0 = ch * 512
        tsl = slice(t0, t0 + 512)
        if layer0:
            xcc = xc[ch % 2]
            for sub in range(4):
                K.dma(xcc.k(sub)[:, sub, :], x[t0 + sub * 128:t0 + (sub + 1) * 128, :], q="sp")
            for ft in range(8):
                p = nps()
                for sub in range(4):
                    K.tr(p[:, sub * 128:(sub + 1) * 128], xcc.k(sub)[:, sub, ft * 128:(ft + 1) * 128], c["ident32"][:, :])
                evac(xT[:, ft, :], p[:, :])
            ln_fm(K, c, xT, g_s, b_s, h32, hb, psA, psB, tmp)
            for ft in range(8):
                (K.dma(hT_out.k((ch, ft))[ft * 128:(ft + 1) * 128, tsl], h32[:, ft, :], q="poolq"))
        else:
            for ft in range(8):
                K.dma(h32[:, ft, :], hT_in[ft * 128:(ft + 1) * 128, tsl], q="sp")
            for ft in range(8):
                K.copy(hb[:, ft, :], h32[:, ft, :], q=("act" if ft % 2 else "dve"))
        K.dma(cos_s[:, :], cosT[:, tsl]); K.dma(sin_s[:, :], sinT[:, tsl])

        def proj_fm(c0, M):
            p = nps()
            for kt in range(8):
                K.mm(p[:M, :], Wv(kt, c0, M), hb[:, kt, :], start=(kt == 0), stop=(kt == 7))
            return p

        def fm_to(name, r0, M, p, func=None):
            s_ = nstg()
            evac(s_[:M, :], p[:M, :], func)
            (K.dma(fm_outs[name].k((ch, r0))[r0:r0 + M, tsl], s_[:M, :], q="poolq"))

        for name in ("nq", "kc", "vc", "ks", "kw", "dq", "dk"):
            c0, w = OFF[name]
            for m0 in range(0, w, 128):
                fm_to(name + "T", m0, 128, proj_fm(c0 + m0, 128))
        fm_to("ngT", 0, 24, proj_fm(OFF["ng"][0], 24), AF.Sigmoid)
        for name in ("vs", "vw", "dv"):
            c0, w = OFF[name]
            for sub in range(4):
                p = nps()
                for kt in range(8):
                    K.mm(p[:, :w], hb[:, kt, sub * 128:(sub + 1) * 128], Wv(kt, c0, w), start=(kt == 0), stop=(kt == 7))
                s_ = nstg()
                evac(s_[:, :w], p[:, :w])
                (K.dma(tm_outs[name].k((ch, sub))[t0 + sub * 128:t0 + (sub + 1) * 128, :], s_[:, :w], q="poolq"))
        for m in range(2):
            p = proj_fm(OFF["cq"][0] + m * 128, 128)
            evac(cq32[:, m, :], p[:, :])
        rms_fm(K, c, cq32, qg_s, cqb, psA, tmp, 2)
        for h in range(8):
            p = nps()
            for kt in range(2):
                K.mm(p[:96, :], Wuq[:, kt, h * 96:(h + 1) * 96], cqb[:, kt, :], start=(kt == 0), stop=(kt == 1))
            K.copy(xs32[:, :], p[:96, :], q="act")
            p2 = nps()
            K.mm(p2[:32, :], prot_s[:, :], xs32[0:32, :])
            K.tt(r1[:, :], xs32[0:32, :], cos_s[:, :], ALU.mult, q="pool")
            K.tt(r2[:, :], p2[:32, :], sin_s[:, :], ALU.mult)
            qs = qstg[h % 2]
            K.tt(qs[0:32, :], r1[:, :], r2[:, :], ALU.add)
            K.copy(qs[32:64, :], xs32[32:64, :], q="pool")
            K.copy(qs[64:96, :], xs32[64:96, :], q="pool")
            (K.dma(fm_outs["qmT"].k((ch, h))[h * 96:(h + 1) * 96, tsl], qs[:, :], q="poolq"))
        p = proj_fm(OFF["ckv"][0], 128)
        evac(ckv32[:, 0, :], p[:, :])
        rms_fm(K, c, ckv32, kvg_s, ckvb, psA, tmp, 1)
        for m in range(4):
            p = nps()
            K.mm(p[:, :], Wukv[:, m * 128:(m + 1) * 128], ckvb[:, 0, :])
            fm_to("knopeT", m * 128, 128, p)
        for sub in range(4):
            p = nps()
            K.mm(p[:, :], ckvb[:, 0, sub * 128:(sub + 1) * 128], Wukv[:, 512:1024])
            s_ = nstg()
            evac(s_[:, :], p[:, :])
            (K.dma(tm_outs["vm"].k((ch, sub))[t0 + sub * 128:t0 + (sub + 1) * 128, :], s_[:, :], q="poolq"))
        p = proj_fm(OFF["kr"][0], 32)
        K.copy(xs32[0:32, :], p[:32, :], q="act")
        p2 = nps()
        K.mm(p2[:32, :], prot_s[:, :], xs32[0:32, :])
        K.tt(r1[:, :], xs32[0:32, :], cos_s[:, :], ALU.mult, q="pool")
        K.tt(r2[:, :], p2[:32, :], sin_s[:, :], ALU.mult)
        s_ = nstg()
        K.tt(s_[0:32, :], r1[:, :], r2[:, :], ALU.add)
        (K.dma(fm_outs["kropeT"].k(ch)[:, tsl], s_[0:32, :], q="poolq"))


NEG = -1.0e9


def load_tm(K, Vt, src, c0, dv, nper=4):
    n = S // 128
    for i in range(0, n, nper):
        K.dma(Vt[:, i:i + nper, 0:dv], rv(src[:, :], src.t[i * 128:(i + nper) * 128, c0:c0 + dv].rearrange("(n p) d -> p n d", p=128)))


class AttnCtx:
    def __init__(self, K, c):
        self.K, self.c = K, c
        self.NS = 3
        self.NP = 4
        self.S_ps = [K.ps([128, 512]) for _ in range(self.NS)]
        self.O_ps = [K.ps([128, 512]) for _ in range(3)]
        self.Sum_ps = [K.ps([128, 512])]
        self.misc_ps = K.ps([128, 512])
        self.si = 0
        self.oi = 0
        self.P = [K.sb([128, 512], BF16) for _ in range(self.NP)]
        self.Sm = [K.sb([128, 512], F32) for _ in range(3)]
        self.pi = 0
        self.mi = 0
        self.R = [K.sb([128, 512], F32) for _ in range(3)]
        self.pending = []
        self.depth = 2
        self.deferred = []

    def nextS(self):
        self.si = (self.si + 1) % self.NS
        return self.S_ps[self.si]

    def nextO(self):
        self.oi = (self.oi + 1) % 3
        j = -1
        for idx, d in enumerate(self.deferred):
            if self.oi in d[3]:
                j = idx
        if j >= 0:
            self.flush()
        for _ in range(j + 1):
            self.deferred.pop(0)[0]()
        return self.O_ps[self.oi], self.Sum_ps[0], self.R[self.oi]

    def defer(self, fn, thresh, banks, tag=None):
        self.deferred.append([fn, 0, thresh, banks, tag])

    def force_tag(self, tag):
        j = -1
        for idx, d in enumerate(self.deferred):
            if d[4] == tag:
                j = idx
        if j >= 0:
            self.flush()
        for _ in range(j + 1):
            self.deferred.pop(0)[0]()

    def tick(self):
        for d in self.deferred:
            d[1] += 1
        while self.deferred and self.deferred[0][1] >= self.deferred[0][2]:
            self.deferred.pop(0)[0]()

    def drain(self):
        self.flush()
        while self.deferred:
            self.deferred.pop(0)[0]()

    def step(self, qk_list, N, scale, mask, v_lhsT, dv, O, Sum, M, first, last, extra=None, ones=None, Pt=None, mg=1, merged=False):
        K = self.K
        Sp = self.nextS()
        for (l, r, c0, n) in qk_list:
            K.mm(Sp[:, c0:c0 + n], l, r, start=True, stop=(extra is None))
        if extra is not None:
            K.mm(Sp[:, :N], extra[0], extra[1], start=False, stop=True)
        src = Sp[:, :N]
        if mask is not None:
            self.mi = (self.mi + 1) % 3
            sm = self.Sm[self.mi]
            if mg == 1:
                K.tt(sm[:, :N], Sp[:, :N], mask, ALU.add)
            else:
                mb = rv(mask, mask.ap.unsqueeze(1).to_broadcast([128, mg, N // mg]))
                K.tt(rv(sm[:, :], sm.t[:, :N].rearrange("p (m q) -> p m q", m=mg)),
                     rv(Sp[:, :], Sp.t[:, :N].rearrange("p (m q) -> p m q", m=mg)), mb, ALU.add)
            src = sm[:, :N]
        if Pt is None:
            self.pi = (self.pi + 1) % self.NP
            Pt = self.P[self.pi][:, :N]
        K.act(Pt, src, AF.Exp, scale=scale)
        onesv = ones if ones is not None else self.c["onesb"][:, :M]

        def pv():
            if merged:
                K.mm(O[:, :N], v_lhsT, Pt, start=first, stop=last)
            else:
                K.mm(O[:dv, :N], v_lhsT, Pt, start=first, stop=last)
                K.mm(Sum[:M, :N], onesv, Pt, start=first, stop=last)
        self.pending.append(pv)
        if len(self.pending) > self.depth:
            self.pending.pop(0)()
        self.tick()
        return Pt

    def flush(self):
        while self.pending:
            self.pending.pop(0)()

    def recip_m1(self, R, O, N):
        K = self.K
        K.ts(R[64:128, :N], O[64:128, :N], 1e-30, ALU.max)
        K.op("dve", "reciprocal", {"out": R[64:128, :N]}, {"in_": R[64:128, :N]})

    def recip_m2(self, R, N):
        K = self.K
        mp = self.misc_ps
        K.mm(mp[0:64, :N], self.c["ident32"][64:128, 64:128], R[64:128, :N])
        K.copy(R[0:64, :N], mp[0:64, :N], q="act")

    def recip(self, R, Sum, M, N):
        K = self.K
        self.flush()
        K.ts(R[:M, :N], Sum[:M, :N], 1e-30, ALU.max)
        K.op("dve", "reciprocal", {"out": R[:M, :N]}, {"in_": R[:M, :N]})


def build_B(K, c, lam_init, io, do=("diff", "mla", "nsa")):
    A = AttnCtx(K, c)
    NQB = S // 128
    cm512 = io["cm512"]
    cm_s = K.sb([128, 4, 512], F32)
    for j in range(4):
        K.dma(cm_s[:, j, :], cm512[j, :, :])
    wlow = io["wlow"]
    wlow_s = K.sb([128, 128], F32)
    K.dma(wlow_s[:, :], wlow[:, :])
    kaug_tok = io["kaug_tok"]

    if "diff" in do:
        dqT = io["dqT"]; dkT = io["dkT"]; dvv = io["dv"]; qaug_d = io["qaug_diff"]
        lam_p = io["lam_p"]; subg = io["subg"]; o_diff = io["o_diffT"]
        with K.scope():
            lp = K.sb([128, 256]); K.dma(lp[:, :], lam_p[:, :])
            pr = K.sb([128, 128]); l2 = K.sb([128, 2]); lam = K.sb([128, 1]); nlam = K.sb([128, 1])
            K.tt(pr[:, 0:64], lp[:, 0:64], lp[:, 64:128], ALU.mult)
            K.tt(pr[:, 64:128], lp[:, 128:192], lp[:, 192:256], ALU.mult)
            K.op("dve", "tensor_reduce", {"out": l2[:, 0:1]}, {"in_": pr[:, 0:64]}, axis=AX.X, op=ALU.add)
            K.op("dve", "tensor_reduce", {"out": l2[:, 1:2]}, {"in_": pr[:, 64:128]}, axis=AX.X, op=ALU.add)
            K.act(l2[:, :], l2[:, :], AF.Exp)
            K.tt(lam[:, :], l2[:, 0:1], l2[:, 1:2], ALU.subtract)
            K.ts(nlam[:, :], lam[:, :], lam_init, ALU.add, -1.0, ALU.mult)
            sg = K.sb([128, 1]); K.dma(sg[:, :], subg[:, :])
            K.ts(sg[:, :], sg[:, :], 1.0 - lam_init, ALU.mult)
            QA = K.sb([73, 2, S], BF16)
            KAt = K.sb([73, 2, S], BF16)
            Vt = K.sb([128, NQB, 128], BF16)
            o1 = K.sb([128, 256], F32); o2 = K.sb([128, 256], F32); sq = K.sb([128, 256], F32)
            ob = [K.sb([128, 256], BF16) for _ in range(2)]
            for h in range(4):
                for m in range(2):
                    r0 = h * 128 + m * 64
                    K.dma(QA[0:64, m, :], dqT[r0:r0 + 64, :])
                    K.dma(QA[64:73, m, :], qaug_d[h, :, :])
                    K.dma(KAt[0:64, m, :], dkT[r0:r0 + 64, :])
                    K.dma(KAt[64:73, m, :], kaug_tok[:, :])
                load_tm(K, Vt, dvv, h * 128, 128)
                for qc in range(S // 256):
                    q0 = qc * 256
                    O, Sum, R = A.nextO()
                    nkt = 2 * qc + 2
                    for kt in range(nkt):
                        j = kt - 2 * qc
                        mask = cm_s[:, j, 0:256] if j >= 0 else None
                        qk = [(KAt[:, m, kt * 128:(kt + 1) * 128], QA[:, m, q0:q0 + 256], m * 256, 256) for m in range(2)]
                        A.step(qk, 512, 0.125, mask, Vt[:, kt, :], 128, O, Sum, 128, kt == 0, kt == nkt - 1, mg=2)
                    A.recip(R, Sum, 128, 512)

                    def fin(O=O, R=R, h=h, qc=qc, q0=q0):
                        K.tt(o1[:, :], O[:, 0:256], R[:, 0:256], ALU.mult)
                        K.tt(o2[:, :], O[:, 256:512], R[:, 256:512], ALU.mult)
                        K.op("dve", "scalar_tensor_tensor", {"out": o1[:, :]}, {"in0": o2[:, :], "in1": o1[:, :], "scalar": nlam[:, 0:1]},
                             op0=ALU.mult, op1=ALU.add)
                        K.act(sq[:, :], o1[:, :], AF.Square)
                        mp = A.misc_ps
                        K.mm(mp[:, :256], c["ones32"][:, :], sq[:, :])
                        K.ts(sq[:, :], mp[:, :256], 1.0 / 128, ALU.mult, EPS_RMS, ALU.add)
                        K.act(sq[:, :], sq[:, :], AF.Sqrt)
                        K.op("dve", "reciprocal", {"out": sq[:, :]}, {"in_": sq[:, :]})
                        K.tt(o1[:, :], o1[:, :], sq[:, :], ALU.mult)
                        obb = ob[qc % 2]
                        K.act(obb[:, :], o1[:, :], AF.Identity, scale=sg[:, 0:1])
                        K.dma(o_diff.k((h, qc))[h * 128:(h + 1) * 128, q0:q0 + 256], obb[:, :], q="poolq")
                    A.defer(fin, 5, {A.oi})
            A.drain()

    if "mla" in do:
        qmT = io["qmT"]; kropeT = io["kropeT"]; knopeT = io["knopeT"]; mv = io["vm"]; o_mla = io["o_mlaT"]
        msc = 96 ** -0.5
        with K.scope():
            QA = K.sb([96, S], BF16)
            KAt = K.sb([96, S], BF16)
            Vt = K.sb([128, NQB, 128], BF16)
            K.memset(Vt[:, :, :], 1.0)
            ob = [K.sb([64, 512], BF16) for _ in range(2)]
            for h in range(8):
                K.dma(QA[:, :], qmT[h * 96:(h + 1) * 96, :])
                K.dma(KAt[0:32, :], kropeT[:, :])
                K.dma(KAt[32:96, :], knopeT[h * 64:(h + 1) * 64, :])
                load_tm(K, Vt, mv, h * 64, 64)
                for qc in range(S // 512):
                    q0 = qc * 512
                    O, Sum, R = A.nextO()
                    nkt = 4 * qc + 4
                    for kt in range(nkt):
                        j = kt - 4 * qc
                        mask = cm_s[:, j, :] if j >= 0 else None
                        A.step([(KAt[:, kt * 128:(kt + 1) * 128], QA[:, q0:q0 + 512], 0, 512)], 512, msc, mask,
                               Vt[:, kt, :], 64, O, Sum, 64, kt == 0, kt == nkt - 1, merged=True)
                    def fin1(O=O, R=R):
                        A.recip_m1(R, O, 512)

                    def fin2(O=O, R=R, h=h, qc=qc, q0=q0):
                        A.recip_m2(R, 512)
                        obb = ob[qc % 2]
                        K.tt(obb[:, :], O[:64, :], R[:64, :], ALU.mult)
                        K.dma(o_mla.k((h, qc))[h * 64:(h + 1) * 64, q0:q0 + 512], obb[:, :], q="poolq")
                    A.defer(fin1, 3, {A.oi})
                    A.defer(fin2, 7, {A.oi})
            A.drain()
    if "nsa" in do:
        for g in range(2):
            build_nsa(K, c, A, cm_s, wlow_s, kaug_tok, io, g)


def build_nsa(K, c, A, cm_s, wlow_s, kaug_tok, io, grp):
    NQB = S // 128
    nqT = io["nqT"]; qaug_n = io["qaug_nsa"]
    kcT = rv(io["kcT"][:, :], io["kcT"].t[grp * 64:(grp + 1) * 64, :])
    vcT = rv(io["vcT"][:, :], io["vcT"].t[grp * 64:(grp + 1) * 64, :])
    ksa = rv(io["ksT"][:, :], io["ksT"].t[grp * 64:(grp + 1) * 64, :])
    kwa = rv(io["kwT"][:, :], io["kwT"].t[grp * 64:(grp + 1) * 64, :])
    vs_in = io["vs"]; vw_in = io["vw"]
    ngT = rv(io["ngT"][:, :], io["ngT"].t[grp * 12:(grp + 1) * 12, :])
    w1 = {"k": io["cmp_w1_k"], "v": io["cmp_w1_v"]}
    w2 = {"k": io["cmp_w2_k"], "v": io["cmp_w2_v"]}
    pos = {"k": io["posk"], "v": io["posv"]}
    kaug_cmp = io["kaug_cmp"]; cmask = io["cmask"]; cprev = io["cprev"]; amat = io["amat"]
    addmask = io["addmask"]; E_in = io["E"]; oh_in = io["oh"]; o_nsa = io["o_nsaT"]

    KAc = K.sb([73, 512], BF16)
    Vc = K.sb([128, 4, 64], F32)
    K.memset(KAc[:, :], 0.0)
    K.dma(KAc[64:73, :], kaug_cmp[:, :])
    with K.scope():
        xT = K.sb([64, S], BF16)
        w1s = K.sb([64, 32, 256], BF16)
        w2s = K.sb([128, 2, 64], BF16)
        pos32 = K.sb([64, 32], F32); posb = K.sb([64, 32], BF16)
        g = K.sb([128, 2, 512], BF16)
        bias = K.sb([128, 1], F32)
        x = K.sb([128, 512], F32); x2 = K.sb([128, 512], F32)
        K.memset(g[:, :, :], 0.0)
        for which, src in (("k", kcT), ("v", vcT)):
            K.dma(xT[:, :], src)
            for l0 in range(0, 32, 8):
                K.dma(w1s[:, l0:l0 + 8, :], rv(w1[which][:, :], w1[which].t[l0 * 64:(l0 + 8) * 64, :].rearrange("(l d) j -> d l j", d=64)))
            K.dma(w2s[:, :, :], rv(w2[which][:, :], w2[which].t[:, :].rearrange("(jh p) d -> p jh d", p=128)))
            K.dma(pos32[:, :], pos[which][:, :])
            K.copy(posb[:, :], pos32[:, :])
            xv = xT.t[:, :].rearrange("d (c r) -> d c r", r=16)
            for jh in range(2):
                hp = A.nextS()
                for l in range(32):
                    rhs = rv(xT[:, :], xv[:, (l // 16):(l // 16) + 511, l % 16])
                    K.mm(hp[:, :511], w1s[:, l, jh * 128:(jh + 1) * 128], rhs, start=(l == 0), stop=(l == 31))
                bp = A.misc_ps
                for l in range(32):
                    K.mm(bp[:, 0:1], w1s[:, l, jh * 128:(jh + 1) * 128], posb[:, l:l + 1], start=(l == 0), stop=(l == 31))
                K.copy(bias[:, :], bp[:, 0:1])
                K.ts(x[:, :511], hp[:, :511], bias[:, 0:1], ALU.add)
                K.tt(x2[:, :511], x[:, :511], x[:, :511], ALU.mult)
                K.ts(x2[:, :511], x2[:, :511], 0.044715, ALU.mult, 1.0, ALU.add)
                K.tt(x2[:, :511], x2[:, :511], x[:, :511], ALU.mult)
                K.act(x2[:, :511], x2[:, :511], AF.Sigmoid, scale=1.5957691216057308)
                K.tt(g[:, jh, :511], x[:, :511], x2[:, :511], ALU.mult)
            if which == "k":
                kp = A.nextS()
                for jh in range(2):
                    K.mm(kp[:64, :511], w2s[:, jh, :], g[:, jh, :511], start=(jh == 0), stop=(jh == 1))
                K.copy(KAc[0:64, :511], kp[:64, :511])
            else:
                for ct in range(4):
                    vp = A.nextS()
                    for jh in range(2):
                        K.mm(vp[:, :64], g[:, jh, ct * 128:(ct + 1) * 128], w2s[:, jh, :], start=(jh == 0), stop=(jh == 1))
                    K.copy(Vc[:, ct, :], vp[:, :64])
    with K.scope():
        KAs = K.sb([73, S], BF16); KAw = K.sb([73, S], BF16)
        K.dma(KAs[0:64, :], ksa); K.dma(KAs[64:73, :], kaug_tok[:, :])
        K.dma(KAw[0:64, :], kwa); K.dma(KAw[64:73, :], kaug_tok[:, :])
        Vs = K.sb([128, NQB, 128], BF16); Vw = K.sb([128, NQB, 128], BF16)
        K.memset(Vs[:, :, :], 1.0); K.memset(Vw[:, :, :], 1.0)
        load_tm(K, Vs, vs_in, grp * 64, 64)
        load_tm(K, Vw, vw_in, grp * 64, 64)
        E_s = K.sb([128, S], BF16); K.dma(E_s[:, :], E_in[:, :])
        ng_s = K.sb([12, S], BF16); K.dma(ng_s[:, :], ngT)
        oh_s = K.sb([12, 768], BF16); K.dma(oh_s[:, :], oh_in[:, :])
        cprev_s = K.sb([128, 128], F32); K.dma(cprev_s[:, :], cprev[:, :])
        am_s = K.sb([128, 4, 128], F32)
        for ct in range(4):
            K.dma(am_s[:, ct, :], amat[ct, :, :])
        Qt = [K.sb([73, 4, 128], BF16) for _ in range(2)]
        cmk = [K.sb([128, 128], F32) for _ in range(2)]
        adm = [K.sb([128, 128], F32) for _ in range(2)]
        Pc = [K.sb([128, 512], F32) for _ in range(4)]
        pg = K.sb([128, 4, 128], F32)
        ob = [K.sb([64, 512], F32) for _ in range(3)]
        obc = [K.sb([64, 512], F32) for _ in range(2)]
        gs = K.sb([64, 512], F32)
        sc = K.sb([128, 128], F32); sc2 = K.sb([128, 128], F32)
        m8a = K.sb([128, 8], F32); m8b = K.sb([128, 8], F32)
        negsel = K.sb([128, 128], F32)
        nsT = K.sb([128, 128], BF16)
        outb = [K.sb([64, 512], BF16) for _ in range(2)]
        ident = c["ident32"]
        nsTs = [nsT, K.sb([128, 128], BF16)]
        obc3 = [obc[0], obc[1], K.sb([64, 512], F32)]

        def cmpA(qb):
            q0 = qb * 128
            Q = Qt[qb % 2]
            K.dma(Q[0:64, :, :], rv(nqT[:, :], nqT.t[grp * 256:(grp + 1) * 256, q0:q0 + 128].rearrange("(h d) s -> d h s", h=4)))
            K.dma(Q[64:73, :, :], qaug_n[:, grp * 4:(grp + 1) * 4, q0:q0 + 128])
            Qv = Q[:, :, :]
            nct = qb // 16 + 1
            ck = cmk[qb % 2]
            K.dma(ck[:, :], cmask[1 if nct == 4 else 0, qb % 16, :, :])
            ad = adm[qb % 2]
            K.dma(ad[:, :], addmask[qb, :, :])
            O, Sum, R = A.nextO()
            for ct in range(nct):
                last = (ct == nct - 1)
                cmsk = ck[:, :] if last else (cprev_s[:, :] if (qb % 16 == 0 and ct == nct - 2) else None)
                A.step([(KAc[:, ct * 128:(ct + 1) * 128], Qv, 0, 512)], 512, 0.125, cmsk,
                       Vc[:, ct, :], 64, O, Sum, 128, ct == 0, last, ones=c["ones32"][:, :], Pt=Pc[ct][:, :], mg=4)
            A.recip(R, Sum, 128, 512)
            K.tt(obc3[qb % 3][:, :], O[:64, :], R[:64, :], ALU.mult)
            for ct in range(nct):
                K.tt(Pc[ct][:, :], Pc[ct][:, :], R[:, :], ALU.mult)
                K.tt(pg[:, ct, :], Pc[ct][:, 0:128], Pc[ct][:, 128:256], ALU.add, q="pool")
                K.tt(pg[:, ct, :], pg[:, ct, :], Pc[ct][:, 256:384], ALU.add, q="pool")
                K.tt(pg[:, ct, :], pg[:, ct, :], Pc[ct][:, 384:512], ALU.add, q="pool")

        def cmpB(qb):
            nct = qb // 16 + 1
            ad = adm[qb % 2]
            mp = A.misc_ps
            for ct in range(nct):
                K.mm(mp[:, 0:128], pg[:, ct, :], am_s[:, ct, :], start=(ct == 0), stop=(ct == nct - 1))
            K.tt(sc[:, :], mp[:, 0:128], ad[:, :], ALU.add)
            K.op("dve", "max", {"out": m8a[:, :]}, {"in_": sc[:, :]})
            K.op("dve", "match_replace", {"out": sc2[:, :]}, {"in_to_replace": m8a[:, :], "in_values": sc[:, :]}, imm_value=-3.0e4)
            K.op("dve", "max", {"out": m8b[:, :]}, {"in_": sc2[:, :]})
            K.ts(negsel[:, :], sc[:, :], m8b[:, 7:8], ALU.is_lt, -30000.0, ALU.mult)

        def cmpC(qb):
            tp = A.nextS()
            K.tr(tp[:, 0:128], negsel[:, :], ident[:, :])
            K.copy(nsTs[qb % 2][:, :], tp[:, 0:128])

        cmpA(0); cmpB(0); cmpC(0)
        for qb in range(NQB):
            q0 = qb * 128
            Qv = Qt[qb % 2][:, :, :]
            A.force_tag(("C", qb))
            if qb + 1 < NQB:
                cmpA(qb + 1)
                A.defer((lambda qn=qb + 1: cmpB(qn)), 8, set(), tag=("B", qb + 1))
                A.defer((lambda qn=qb + 1: cmpC(qn)), 16, set(), tag=("C", qb + 1))
            nsq = nsTs[qb % 2]
            nsv = rv(nsq[:, :], nsq.t[:, :].unsqueeze(1).to_broadcast([128, 4, 128]))
            O, Sum, R = A.nextO()
            for kt in range(qb + 1):
                last = (kt == qb)
                A.step([(KAs[:, kt * 128:(kt + 1) * 128], Qv, 0, 512)], 512, 0.125, cm_s[:, 0, 0:128] if last else None,
                       Vs[:, kt, :], 64, O, Sum, 64, kt == 0, last, extra=(E_s[:, kt * 128:(kt + 1) * 128], nsv), mg=4, merged=True)
            Osel, Rsel, bsel = O, R, A.oi

            def sel1(O=O, R=R):
                A.recip_m1(R, O, 512)
            A.defer(sel1, 3, {A.oi})
            O, Sum, R = A.nextO()
            k0 = max(0, qb - 4)
            for kt in range(k0, qb + 1):
                last = (kt == qb)
                mask = cm_s[:, 0, 0:128] if last else (wlow_s[:, :] if kt == qb - 4 else None)
                A.step([(KAw[:, kt * 128:(kt + 1) * 128], Qv, 0, 512)], 512, 0.125, mask,
                       Vw[:, kt, :], 64, O, Sum, 64, kt == k0, last, mg=4, merged=True)
            Owin, Rwin, bwin = O, R, A.oi

            def win1(O=O, R=R):
                A.recip_m1(R, O, 512)
            A.defer(win1, 3, {A.oi})

            def post(Osel=Osel, Rsel=Rsel, Owin=Owin, Rwin=Rwin, qb=qb, q0=q0):
                A.recip_m2(Rsel, 512)
                K.tt(ob[1][:, :], Osel[:64, :], Rsel[:64, :], ALU.mult)
                A.recip_m2(Rwin, 512)
                K.tt(ob[2][:, :], Owin[:64, :], Rwin[:64, :], ALU.mult)
                obs = [obc3[qb % 3], ob[1], ob[2]]
                for gi in range(3):
                    gp = A.nextS()
                    for h in range(4):
                        j = h * 3 + gi
                        K.mm(gp[:64, h * 128:(h + 1) * 128], oh_s[:, j * 64:(j + 1) * 64], ng_s[:, q0:q0 + 128])
                    if gi == 0:
                        K.tt(gs[:, :], obs[0][:, :], gp[:64, :], ALU.mult)
                    else:
                        K.tt(obs[gi][:, :], obs[gi][:, :], gp[:64, :], ALU.mult)
                        K.tt(gs[:, :], gs[:, :], obs[gi][:, :], ALU.add, q="pool")
                oo = outb[qb % 2]
                K.copy(oo[:, :], gs[:, :], q="act")
                for h in range(4):
                    K.dma(o_nsa.k((grp, qb, h))[grp * 256 + h * 64:grp * 256 + (h + 1) * 64, q0:q0 + 128], oo[:, h * 128:(h + 1) * 128], q="poolq")
            A.defer(post, 7, {bsel, bwin})
        A.drain()


def build_C(K, c, last_layer, io, l):
    hT_in = io["hT32"]
    oT = {n: io[n] for n in ("o_nsaT", "o_diffT", "o_mlaT")}
    w_mg = io["w_in"]
    w_br = {n: io[n] for n in ("w_br_nsa", "w_br_diff", "w_br_mla")}
    w_out = io["w_out"]
    ln1g = io["ln1_g"]; ln1b = io["ln1_b"]; ln2g = io["ln2_g"]; ln2b = io["ln2_b"]
    rw = io["router_w"]; rb = io["router_b"]
    w1 = io["moe_w1"]; w3 = io["moe_w3"]; w2 = io["moe_w2"]
    h1_scr = K.dram("h1_scr%d" % l, [D, T], F32, "Internal")
    if last_layer:
        out = io["out"]
    else:
        hT_out = io["hT32_next"]

    ps = [K.ps([128, 512]) for _ in range(8)]
    pi = [0]

    def nps():
        pi[0] = (pi[0] + 1) % 6
        return ps[pi[0]]
    psA, psB = ps[6], ps[7]
    names = ("o_nsaT", "o_diffT", "o_mlaT")
    wnames = ("w_br_nsa", "w_br_diff", "w_br_mla")

    with K.scope():
        Wmg = K.sb([128, 8, 3072], BF16)
        for kt in range(8):
            K.dma(Wmg.k(kt)[:, kt, :], w_mg[kt * 128:(kt + 1) * 128, NA:NA + 3072])
        Wbr = {}
        for n in w_br:
            Wbr[n] = K.sb([128, 4, D], BF16)
            for kt in range(4):
                K.dma(Wbr[n][:, kt, :], w_br[n][kt * 128:(kt + 1) * 128, :])
        Wo = K.sb([128, 8, D], BF16)
        for kt in range(8):
            K.dma(Wo[:, kt, :], w_out[kt * 128:(kt + 1) * 128, :])
        g1 = K.sb([128, 8]); K.dma(g1[:, :], ln1g[:, :])
        b1 = K.sb([128, 8]); K.dma(b1[:, :], ln1b[:, :])
        h32 = K.sb([128, 8, 512], F32)
        hb = K.sb([128, 8, 512], BF16)
        ob = {n: K.sb([128, 4, 512], BF16) for n in oT}
        G = K.sb([128, 512], BF16)
        yb = K.sb([128, 8, 512], BF16)
        y32 = K.sb([128, 512], F32); tB = K.sb([128, 512], F32)
        r32 = K.sb([128, 8, 512], F32)
        tmp = {"sq": K.sb([128, 8, 512], F32), "mean": K.sb([128, 512]), "msq": K.sb([128, 512]), "rstd": K.sb([128, 512])}
        h1 = K.sb([128, 8, 512], F32)
        for ch in range(NCH):
            t0 = ch * 512
            tsl = slice(t0, t0 + 512)
            for ft in range(8):
                K.dma(h32[:, ft, :], hT_in[ft * 128:(ft + 1) * 128, tsl])
            for ft in range(8):
                K.copy(hb[:, ft, :], h32[:, ft, :], q=("act" if ft % 2 else "dve"))
            for n in names:
                for kt in range(4):
                    K.dma(ob[n][:, kt, :], oT[n][kt * 128:(kt + 1) * 128, tsl])
            for mt in range(8):
                for bi, (n, wn) in enumerate(zip(names, wnames)):
                    gp = nps()
                    for kt in range(8):
                        K.mm(gp[:, :], Wmg.k(kt)[:, kt, bi * 1024 + mt * 128: bi * 1024 + (mt + 1) * 128], hb[:, kt, :], start=(kt == 0), stop=(kt == 7))
                    K.act(G[:, :], gp[:, :], AF.Sigmoid)
                    yp = nps()
                    for kt in range(4):
                        K.mm(yp[:, :], Wbr[wn][:, kt, mt * 128:(mt + 1) * 128], ob[n][:, kt, :], start=(kt == 0), stop=(kt == 3))
                    if bi == 0:
                        K.tt(y32[:, :], yp[:, :], G[:, :], ALU.mult)
                    else:
                        K.tt(tB[:, :], yp[:, :], G[:, :], ALU.mult)
                        K.tt(y32[:, :], y32[:, :], tB[:, :], ALU.add, q="pool")
                K.copy(yb[:, mt, :], y32[:, :], q="act")
            for mt in range(8):
                mp = nps()
                for kt in range(8):
                    K.mm(mp[:, :], Wo[:, kt, mt * 128:(mt + 1) * 128], yb[:, kt, :], start=(kt == 0), stop=(kt == 7))
                K.op("dve", "scalar_tensor_tensor", {"out": r32[:, mt, :]}, {"in0": h32[:, mt, :], "in1": mp[:, :]},
                     scalar=ALPHA, op0=ALU.mult, op1=ALU.add)
            ln_fm(K, c, r32, g1, b1, h1, None, psA, psB, tmp)
            for ft in range(8):
                K.dma(h1_scr.k((ch, ft))[ft * 128:(ft + 1) * 128, tsl], h1[:, ft, :], q="poolq")

    with K.scope():
        g2 = K.sb([128, 8]); K.dma(g2[:, :], ln2g[:, :])
        b2 = K.sb([128, 8]); K.dma(b2[:, :], ln2b[:, :])
        rw_s = K.sb([128, 8, 16]); K.dma(rw_s[:, :, :], rw[:, :, :])
        rb_s = K.sb([128, 16]); K.dma(rb_s[:, :], rb[:, :])
        h1 = K.sb([128, 8, 512], F32)
        h1b = K.sb([128, 8, 512], BF16)
        h32 = K.sb([128, 8, 512], F32)
        r32 = K.sb([128, 8, 512], F32)
        tmp = {"sq": K.sb([128, 8, 512], F32), "mean": K.sb([128, 512]), "msq": K.sb([128, 512]), "rstd": K.sb([128, 512])}
        comb = K.sb([128, 16, 512], F32)
        hid = K.sb([128, 4, 512], BF16)
        sA = K.sb([128, 512], F32); tB = K.sb([128, 512], F32)
        ffn = K.sb([128, 8, 512], F32)
        W1 = [K.sb([128, 8, 512], BF16) for _ in range(2)]
        W3 = [K.sb([128, 8, 512], BF16) for _ in range(2)]
        W2 = [K.sb([128, 4, D], BF16) for _ in range(2)]
        aff = K.sb([128, 16]); sel = K.sb([128, 16]); pair = K.sb([128, 4, 6]); gsc = K.sb([128, 4]); gmx = K.sb([128, 1])
        goh = K.sb([128, 4]); selm = K.sb([128, 16]); m8 = K.sb([128, 8]); cho = K.sb([128, 16]); gsum = K.sb([128, 1])
        cmb = K.sb([128, 16])
        otile = [K.sb([128, D], F32) for _ in range(2)]
        for ch in range(NCH):
            t0 = ch * 512
            tsl = slice(t0, t0 + 512)
            for ft in range(8):
                K.dma(h1[:, ft, :], h1_scr.k((ch, ft))[ft * 128:(ft + 1) * 128, tsl])
            for ft in range(8):
                K.copy(h1b[:, ft, :], h1[:, ft, :], q=("act" if ft % 2 else "dve"))
            for sub in range(4):
                lp = nps()
                for kt in range(8):
                    K.mm(lp[:, 0:16], h1[:, kt, sub * 128:(sub + 1) * 128], rw_s[:, kt, :], start=(kt == 0), stop=(kt == 7))
                K.act(aff[:, :], lp[:, 0:16], AF.Sigmoid)
                K.tt(sel[:, :], aff[:, :], rb_s[:, :], ALU.add)
                sv = sel.t[:, :].rearrange("p (g e) -> p g e", e=4)
                pairs = [(0, 1), (0, 2), (0, 3), (1, 2), (1, 3), (2, 3)]
                for pi_, (a_, b_) in enumerate(pairs):
                    K.tt(rv(pair[:, :, :], pair.t[:, :, pi_]), rv(sel[:, :], sv[:, :, a_]), rv(sel[:, :], sv[:, :, b_]), ALU.add)
                K.op("dve", "tensor_reduce", {"out": gsc[:, :]}, {"in_": pair[:, :, :]}, axis=AX.X, op=ALU.max)
                K.op("dve", "tensor_reduce", {"out": gmx[:, :]}, {"in_": gsc[:, :]}, axis=AX.X, op=ALU.max)
                K.ts(goh[:, :], gsc[:, :], gmx[:, 0:1], ALU.is_ge, 1.0, ALU.subtract)
                K.ts(goh[:, :], goh[:, :], 1.0e4, ALU.mult)
                K.tt(rv(selm[:, :], selm.t[:, :].rearrange("p (g e) -> p g e", e=4)), rv(sel[:, :], sv),
                     rv(goh[:, :], goh.t[:, :].unsqueeze(2).to_broadcast([128, 4, 4])), ALU.add)
                K.op("dve", "max", {"out": m8[:, :]}, {"in_": selm[:, :]})
                K.ts(cho[:, :], selm[:, :], m8[:, 1:2], ALU.is_ge)
                K.tt(cho[:, :], cho[:, :], aff[:, :], ALU.mult)
                K.op("dve", "tensor_reduce", {"out": gsum[:, :]}, {"in_": cho[:, :]}, axis=AX.X, op=ALU.add)
                K.op("dve", "reciprocal", {"out": gsum[:, :]}, {"in_": gsum[:, :]})
                K.ts(cmb[:, :], cho[:, :], gsum[:, 0:1], ALU.mult)
                for e4 in range(4):
                    bp = nps()
                    for e in range(4):
                        ee = e4 * 4 + e
                        K.mm(bp[:, e * 128:(e + 1) * 128], rv(cmb[:, :], cmb.t[:, ee:ee + 1].to_broadcast([128, 128])), c["ident32"][:, :])
                    for e in range(4):
                        ee = e4 * 4 + e
                        K.copy(comb[:, ee, sub * 128:(sub + 1) * 128], bp[:, e * 128:(e + 1) * 128], q=("act" if e % 2 else "dve"))
            for e in range(16):
                W1e, W3e, W2e = W1[e % 2], W3[e % 2], W2[e % 2]
                for kt in range(8):
                    K.dma(W1e.k(kt)[:, kt, :], w1[e * D + kt * 128:e * D + (kt + 1) * 128, :], q="sp")
                    K.dma(W3e.k(kt)[:, kt, :], w3[e * D + kt * 128:e * D + (kt + 1) * 128, :], q="sp")
                for kt in range(4):
                    K.dma(W2e.k(kt)[:, kt, :], w2[e * 512 + kt * 128:e * 512 + (kt + 1) * 128, :], q="sp")
                for ft in range(4):
                    ap_ = nps()
                    for kt in range(8):
                        K.mm(ap_[:, :], W1e.k(kt)[:, kt, ft * 128:(ft + 1) * 128], h1b[:, kt, :], start=(kt == 0), stop=(kt == 7))
                    bp_ = nps()
                    for kt in range(8):
                        K.mm(bp_[:, :], W3e.k(kt)[:, kt, ft * 128:(ft + 1) * 128], h1b[:, kt, :], start=(kt == 0), stop=(kt == 7))
                    K.act(sA[:, :], ap_[:, :], AF.Silu)
                    K.tt(tB[:, :], bp_[:, :], comb[:, e, :], ALU.mult)
                    K.tt(hid[:, ft, :], sA[:, :], tB[:, :], ALU.mult, q="pool")
                for mt in range(8):
                    fp = nps()
                    for kt in range(4):
                        K.mm(fp[:, :], W2e.k(kt)[:, kt, mt * 128:(mt + 1) * 128], hid[:, kt, :], start=(kt == 0), stop=(kt == 3))
                    if e == 0:
                        K.copy(ffn[:, mt, :], fp[:, :], q="act")
                    else:
                        K.tt(ffn[:, mt, :], ffn[:, mt, :], fp[:, :], ALU.add)
            for mt in range(8):
                K.op("dve", "scalar_tensor_tensor", {"out": r32[:, mt, :]}, {"in0": h1[:, mt, :], "in1": ffn[:, mt, :]},
                     scalar=ALPHA, op0=ALU.mult, op1=ALU.add)
            ln_fm(K, c, r32, g2, b2, h32, None, psA, psB, tmp)
            if last_layer:
                for sub in range(4):
                    ot = otile[sub % 2]
                    for ft in range(8):
                        tp = nps()
                        K.tr(tp[:, 0:128], h32[:, ft, sub * 128:(sub + 1) * 128], c["ident32"][:, :])
                        K.copy(ot[:, ft * 128:(ft + 1) * 128], tp[:, 0:128], q=("act" if ft % 2 else "dve"))
                    K.outs.append(K.dma(out.k((ch, sub))[t0 + sub * 128:t0 + (sub + 1) * 128, :], ot[:, :], q="poolq"))
            else:
                for ft in range(8):
                    K.dma(hT_out.k((ch, ft))[ft * 128:(ft + 1) * 128, tsl], h32[:, ft, :], q="poolq")


LAYER_W = [("w_in", [D, 6328]), ("w_uq", [256, 768]), ("w_ukv", [128, 1024]),
           ("w_br_nsa", [512, D]), ("w_br_diff", [512, D]), ("w_br_mla", [512, D]), ("w_out", [D, D]),
           ("moe_w1", [16 * D, 512]), ("moe_w3", [16 * D, 512]), ("moe_w2", [16 * 512, D]),
           ("cmp_w1_k", [2048, 256]), ("cmp_w1_v", [2048, 256]), ("cmp_w2_k", [256, 64]), ("cmp_w2_v", [256, 64])]
LAYER_P = [("qg", [128, 2]), ("kvg", [128, 1]), ("lam_p", [128, 256]), ("subg", [128, 1]), ("posk", [64, 32]), ("posv", [64, 32]),
           ("ln1_g", [128, 8]), ("ln1_b", [128, 8]), ("ln2_g", [128, 8]), ("ln2_b", [128, 8])]
CONSTS = [("cosT", [32, S], F32), ("sinT", [32, S], F32), ("protT", [32, 32], F32), ("cm512", [4, 128, 512], F32),
          ("wlow", [128, 128], F32), ("kaug_tok", [9, S], BF16), ("qaug_diff", [4, 9, S], BF16), ("qaug_nsa", [9, 8, S], BF16),
          ("kaug_cmp", [9, 512], BF16), ("cmask", [2, 16, 128, 128], F32), ("cprev", [128, 128], F32), ("amat", [4, 128, 128], F32),
          ("addmask", [S // 128, 128, 128], F32), ("E", [128, S], BF16), ("oh", [12, 768], BF16),
          ("router_w", [128, 8, 16], F32), ("router_b", [128, 16], F32), ("ln_g", [128, 8], F32), ("ln_b", [128, 8], F32)]
SCR_FM = [("nqT", 512), ("kcT", 128), ("vcT", 128), ("ksT", 128), ("kwT", 128), ("ngT", 24), ("dqT", 512), ("dkT", 512),
          ("qmT", 768), ("kropeT", 32), ("knopeT", 512), ("o_nsaT", 512), ("o_diffT", 512), ("o_mlaT", 512)]
SCR_TM = [("vs", 128), ("vw", 128), ("dv", 512), ("vm", 512)]


def conv_w(K, src, dst, rows, cols, bufs):
    a = rows // 128
    sv = src.t[:, :].rearrange("(p a) c -> p (a c)", p=128)
    dv = dst.t[:, :].rearrange("(p a) c -> p (a c)", p=128)
    n = a * cols
    CH = 4096
    for i, c0 in enumerate(range(0, n, CH)):
        w = min(CH, n - c0)
        fa, fb = bufs[0][bufs[2][0] % 3], bufs[1][bufs[2][0] % 3]
        bufs[2][0] += 1
        K.dma(fa[:, :w], rv(src.k(i)[:, :], sv[:, c0:c0 + w]), q="sp")
        if i % 2 == 0:
            K.copy(fb[:, :w], fa[:, :w], q="dve")
        else:
            K.copy(fb[:, :w], fa[:, :w], q="act")
        K.dma(rv(dst.k(i)[:, :], dv[:, c0:c0 + w]), fb[:, :w], q="poolq")


def build_fused(debug=False, nlayers=2, stages="wABC"):
    K = KB()
    c = consts(K)
    x = K.inp("x", [S, D], F32)
    out = K.out("out", [S, D], F32)
    cst = {n: K.inp(n, sh, dt) for n, sh, dt in CONSTS}
    win = [{n: K.inp("%s_%d" % (n, l), sh, F32) for n, sh in LAYER_W} for l in range(nlayers)]
    prm = [{n: K.inp("%s_%d" % (n, l), sh, F32) for n, sh in LAYER_P} for l in range(nlayers)]
    wsc = {n: K.dram("b_" + n, sh, BF16, "Internal") for n, sh in LAYER_W}
    scr = {n: K.dram("s_" + n, [r, S], BF16, "Internal") for n, r in SCR_FM}
    scr.update({n: K.dram("s_" + n, [S, w], BF16, "Internal") for n, w in SCR_TM})
    hT = [K.dram("s_hT32_%d" % i, [D, S], F32, "Internal") for i in range(2)]
    dbg = {}
    if debug:
        dbg["hT32_dbg"] = K.out("hT32_dbg", [D, S], F32)
    for l in range(nlayers):
        with K.scope():
          if "w" in stages:
            fa = [K.sb([128, 4096], F32) for _ in range(3)]
            fb = [K.sb([128, 4096], BF16) for _ in range(3)]
            cnt = [0]
            for n, sh in LAYER_W:
                conv_w(K, win[l][n], wsc[n], sh[0], sh[1], (fa, fb, cnt))
        io = dict(cst)
        io.update(prm[l])
        io.update(wsc)
        io.update(scr)
        io["x"] = x
        io["out"] = out
        io["hT32"] = hT[l % 2]
        io["hT32_next"] = hT[(l + 1) % 2]
        with K.scope():
          if "A" in stages:
            build_A(K, c, l == 0, io)
        lam_init = 0.8 - 0.6 * math.exp(-0.3 * l)
        with K.scope():
          if "B" in stages:
            build_B(K, c, lam_init, io, do=tuple(x for x, f in (("diff", "d"), ("mla", "m"), ("nsa", "n")) if f in stages) if any(f in stages for f in "dmn") else ("diff", "mla", "nsa"))
        with K.scope():
          if "C" in stages:
            build_C(K, c, (l == nlayers - 1) and not debug, io, l)
    if debug:
        with K.scope():
            t = K.sb([128, 4096], F32)
            src = hT[nlayers % 2]
            for ft in range(8):
                for hh in range(2):
                    K.dma(t[:, :], src[ft * 128:(ft + 1) * 128, hh * 4096:(hh + 1) * 4096])
                    K.outs.append(K.dma(dbg["hT32_dbg"].k((ft, hh))[ft * 128:(ft + 1) * 128, hh * 4096:(hh + 1) * 4096], t[:, :]))
    return K.finish()


def split3(v):
    v = np.asarray(v, np.float32)
    a = v.astype(NPBF); r = v - a.astype(np.float32)
    b = r.astype(NPBF); r2 = r - b.astype(np.float32)
    c = r2.astype(NPBF)
    return [a, b, c]

def slopes_all():
    n = 12
    return (2.0 ** (-8.0 * np.arange(1, n + 1, dtype=np.float32) / n)).astype(np.float32)

def qaug(slope, scale, n=S):
    t = np.arange(n, dtype=np.float32)
    rows = split3(-(np.float32(slope) * t) / np.float32(scale))
    s3 = split3(np.full(n, np.float32(slope) / np.float32(scale), np.float32))
    return np.stack(rows + s3 + s3, 0)

def kaug(pos):
    pos = np.asarray(pos)
    one = np.ones(len(pos), np.float32).astype(NPBF)
    a = (64 * (pos // 64)).astype(np.float32).astype(NPBF)
    b = (pos % 64).astype(np.float32).astype(NPBF)
    return np.stack([one] * 3 + [a] * 3 + [b] * 3, 0)

def perm_uq():
    idx = []
    for h in range(8):
        idx += list(range(96 * h + 64, 96 * h + 96)) + list(range(96 * h, 96 * h + 64))
    return np.array(idx)

def perm_ukv():
    idx = []
    for h in range(8):
        idx += list(range(128 * h, 128 * h + 64))
    for h in range(8):
        idx += list(range(128 * h + 64, 128 * h + 128))
    return np.array(idx)

def pvec(v, nft):
    return np.ascontiguousarray(np.asarray(v, np.float32).reshape(nft, 128).T)

def const_inputs(inp):
    m = {}
    half = 16
    freqs = (10000.0 ** (-np.arange(half, dtype=np.float32) / half)).astype(np.float32)
    ang = np.arange(S, dtype=np.float32)[:, None] * freqs[None, :]
    cos = np.cos(ang).astype(np.float32); sin = np.sin(ang).astype(np.float32)
    m["cosT"] = np.ascontiguousarray(np.concatenate([cos.T, cos.T], 0))
    m["sinT"] = np.ascontiguousarray(np.concatenate([sin.T, sin.T], 0))
    P = np.zeros((32, 32), np.float32)
    for i in range(16):
        P[i, i + 16] = -1.0
        P[i + 16, i] = 1.0
    m["protT"] = np.ascontiguousarray(P.T)
    k = np.arange(128)[:, None]; q = np.arange(512)[None, :]
    m["cm512"] = np.stack([np.where(128 * j + k <= q, 0.0, -1e9).astype(np.float32) for j in range(4)], 0)
    q1 = np.arange(128)[None, :]
    m["wlow"] = np.where(k > q1, 0.0, -1e9).astype(np.float32)
    m["kaug_tok"] = kaug(np.arange(S))
    sl = slopes_all()
    m["qaug_diff"] = np.stack([qaug(sl[8 + h], 0.125) for h in range(4)], 0)
    m["qaug_nsa"] = np.ascontiguousarray(np.stack([qaug(sl[h], 0.125) for h in range(8)], 1))
    m["kaug_cmp"] = kaug(np.arange(512) * 16 + 31)
    cm = np.zeros((2, 16, 128, 128), np.float32)
    cc = np.arange(128)[:, None]; qq = np.arange(128)[None, :]
    for r in range(16):
        vis = (16 * cc + 31 <= 128 * r + qq)
        cm[0, r] = np.where(vis, 0.0, -1e9)
        cm[1, r] = np.where(vis & (cc < 127), 0.0, -1e9)
    m["cmask"] = cm
    cp = np.zeros((128, 128), np.float32); cp[127, :15] = -1e9
    m["cprev"] = cp
    Am = np.zeros((512, 128), np.float32)
    for cidx in range(511):
        for i in (cidx, cidx + 1):
            Am[cidx, i // 4] += 1.0
    m["amat"] = np.ascontiguousarray(Am.reshape(4, 128, 128))
    adm = np.zeros((S // 128, 128, 128), np.float32)
    jj = np.arange(128)[None, :]
    for qb in range(S // 128):
        t = qb * 128 + np.arange(128)[:, None]
        cur = t // 64
        forced = (jj == 0) | (jj == cur) | (jj == cur - 1)
        started = jj * 64 <= t
        adm[qb] = np.where(started, np.where(forced, 1e4, 0.0), -1e4)
    m["addmask"] = adm
    m["E"] = np.ascontiguousarray((np.arange(S)[None, :] // 64 == np.arange(128)[:, None]).astype(np.float32).astype(NPBF))
    oh = np.zeros((12, 12, 64), np.float32)
    for j in range(12):
        oh[j, j, :] = 1.0
    m["oh"] = np.ascontiguousarray(oh.reshape(12, 768).astype(NPBF))
    m["router_w"] = np.ascontiguousarray(inp["router_w"].reshape(8, 128, 16).transpose(1, 0, 2))
    m["router_b"] = np.ascontiguousarray(np.broadcast_to(inp["router_b"].reshape(1, 16), (128, 16)))
    m["ln_g"] = pvec(inp["ln_in_g"], 8); m["ln_b"] = pvec(inp["ln_in_b"], 8)
    return m

def layer_inputs(inp, l):
    m = {}
    m["w_in_%d" % l] = np.ascontiguousarray(inp["w_in"][l])
    m["w_uq_%d" % l] = np.ascontiguousarray(inp["mla_w_uq"][l][:, perm_uq()])
    m["w_ukv_%d" % l] = np.ascontiguousarray(inp["mla_w_ukv"][l][:, perm_ukv()])
    for n in ("w_br_nsa", "w_br_diff", "w_br_mla", "w_out"):
        m["%s_%d" % (n, l)] = np.ascontiguousarray(inp[n][l])
    m["moe_w1_%d" % l] = np.ascontiguousarray(inp["moe_w1"][l].reshape(16 * D, 512))
    m["moe_w3_%d" % l] = np.ascontiguousarray(inp["moe_w3"][l].reshape(16 * D, 512))
    m["moe_w2_%d" % l] = np.ascontiguousarray(inp["moe_w2"][l].reshape(16 * 512, D))
    for n in ("cmp_w1_k", "cmp_w1_v", "cmp_w2_k", "cmp_w2_v"):
        m["%s_%d" % (n, l)] = np.ascontiguousarray(inp[n][l])
    m["qg_%d" % l] = pvec(inp["mla_q_norm_g"][l], 2)
    m["kvg_%d" % l] = pvec(inp["mla_kv_norm_g"][l], 1)
    m["lam_p_%d" % l] = np.ascontiguousarray(np.broadcast_to(inp["diff_lambda"][l].reshape(1, 256), (128, 256)))
    m["subg_%d" % l] = np.ascontiguousarray(inp["diff_subln_g"][l].reshape(128, 1))
    m["posk_%d" % l] = np.ascontiguousarray(inp["cmp_pos_k"][l].T)
    m["posv_%d" % l] = np.ascontiguousarray(inp["cmp_pos_v"][l].T)
    for n in ("ln1_g", "ln1_b", "ln2_g", "ln2_b"):
        m["%s_%d" % (n, l)] = pvec(inp[n][l], 8)
    return m

def all_inputs(inp, nlayers, ncores):
    base = const_inputs(inp)
    for l in range(nlayers):
        base.update(layer_inputs(inp, l))
    maps = []
    for b in range(ncores):
        m = dict(base)
        m["x"] = np.ascontiguousarray(inp["x"][b])
        maps.append(m)
    return maps


def kernel(**inputs):
    inp = {k: np.asarray(v, np.float32) for k, v in inputs.items()}
    nc = build_fused(debug=False, nlayers=2)
    maps = all_inputs(inp, 2, 4)
    res = run_bass_kernel_spmd(nc, maps, core_ids=[0, 1, 2, 3]).results
    out = np.stack([np.asarray(res[b]["out"], np.float32) for b in range(4)], 0)
    return out
```

```python
import numpy as np
from contextlib import ExitStack
import concourse.bass as bass
import concourse.mybir as mybir

F32 = mybir.dt.float32
BF16 = mybir.dt.bfloat16
AF = mybir.ActivationFunctionType
ALU = mybir.AluOpType
AX = mybir.AxisListType

COMPUTE = ("pe", "act", "dve", "pool")
DMAQ = ("sp", "poolq", "actq")
NSLOT = 32
NSLOT_HW = 18


class Buf:
    __slots__ = ("w", "r")

    def __init__(self):
        self.w = None
        self.r = []


class Op:
    __slots__ = ("q", "fn", "deps", "inc", "sem", "val", "is_dma", "slot", "incv")

    def __init__(self, q, fn, is_dma):
        self.q = q
        self.fn = fn
        self.deps = []
        self.inc = False
        self.sem = None
        self.val = 0
        self.is_dma = is_dma
        self.slot = -1
        self.incv = 16


class V:
    __slots__ = ("ap", "bufs")

    def __init__(self, ap, bufs):
        self.ap = ap
        self.bufs = bufs


class Tl:
    def __init__(self, t):
        self.t = t
        self._b = {}

    def buf(self, key=None):
        b = self._b.get(key)
        if b is None:
            b = self._b[key] = Buf()
        return b

    def __getitem__(self, idx):
        return V(self.t[idx], [self.buf(None)])

    def k(self, key):
        return _Keyed(self, key)

    def ks(self, keys, idx):
        return V(self.t[idx], [self.buf(k) for k in keys])


class _Keyed:
    def __init__(self, tl, key):
        self.tl = tl
        self.key = key

    def __getitem__(self, idx):
        return V(self.tl.t[idx], [self.tl.buf(self.key)])


def rv(v, ap):
    return V(ap, v.bufs)


class KB:
    def __init__(self):
        self.nc = bass.Bass("TRN2", target_bir_lowering=False, num_devices=8)
        self.es = ExitStack()
        self.ops = {q: [] for q in ("pe", "act", "dve", "pool", "sp")}
        self.allops = []
        self.ndma = 0
        self.ndma_sw = 0
        self.slot_last = [None] * NSLOT
        self.n = 0
        self.outs = []

    def sb(self, shape, dt=F32, name=None):
        self.n += 1
        return Tl(self.es.enter_context(self.nc.sbuf_tensor(name or "sb%d" % self.n, list(shape), dt)))

    def ps(self, shape, dt=F32, name=None):
        self.n += 1
        return Tl(self.es.enter_context(self.nc.psum_tensor(name or "ps%d" % self.n, list(shape), dt)))

    def dram(self, name, shape, dt, kind):
        return Tl(self.nc.dram_tensor(name, list(shape), dt, kind=kind).ap())

    def inp(self, name, shape, dt):
        return self.dram(name, shape, dt, "ExternalInput")

    def out(self, name, shape, dt):
        return self.dram(name, shape, dt, "ExternalOutput")

    def rec(self, q, fn, reads, writes):
        is_dma = q in DMAQ
        o = Op(q, fn, is_dma)
        deps = set()
        for b in reads:
            if b.w is not None:
                deps.add(b.w)
        for b in writes:
            if b.w is not None:
                deps.add(b.w)
            for r in b.r:
                deps.add(r)
        if is_dma:
            if q == "poolq":
                o.slot = NSLOT_HW + (self.ndma_sw % (NSLOT - NSLOT_HW))
                self.ndma_sw += 1
            else:
                o.slot = self.ndma % NSLOT_HW
                self.ndma += 1
            prev = self.slot_last[o.slot]
            if prev is not None:
                deps.add(prev)
            self.slot_last[o.slot] = o
            o.inc = True
        for d in deps:
            if d is o:
                continue
            if (not d.is_dma) and (not is_dma) and d.q == "pe" and q == "pe":
                continue
            d.inc = True
            o.deps.append(d)
        for b in reads:
            b.r.append(o)
        for b in writes:
            b.w = o
            b.r = []
        st = {"poolq": "pool", "actq": "act"}.get(q, q)
        self.ops[st].append(o)
        self.allops.append(o)
        return o

    def op(self, q, meth, outs, ins, *args, **kw):
        reads = [b for v in ins.values() for b in v.bufs]
        writes = [b for v in outs.values() for b in v.bufs]
        kwargs = dict(kw)
        for k, v in outs.items():
            kwargs[k] = v.ap
        for k, v in ins.items():
            kwargs[k] = v.ap

        def fn(e):
            return getattr(e, meth)(*args, **kwargs)
        return self.rec(q, fn, reads, writes)

    def dma(self, out, in_, q="sp"):
        o = self.op(q, "dma_start", {"out": out}, {"in_": in_})
        return o

    def mm(self, out, lhsT, rhs, start=True, stop=True):
        ins = {"lhsT": lhsT, "rhs": rhs}
        reads = [b for v in ins.values() for b in v.bufs]
        writes = list(out.bufs)
        oa, la, ra = out.ap, lhsT.ap, rhs.ap

        def fn(e):
            return e.matmul(oa, la, ra, start=start, stop=stop)
        return self.rec("pe", fn, reads + (writes if not start else []), writes)

    def tr(self, out, in_, ident):
        oa, ia, da = out.ap, in_.ap, ident.ap

        def fn(e):
            return e.transpose(oa, ia, da)
        return self.rec("pe", fn, in_.bufs + ident.bufs, out.bufs)

    def act(self, out, in_, func, bias=None, scale=None, q="act"):
        ins = {"in_": in_}
        kw = {}
        if isinstance(bias, V):
            ins["bias"] = bias
        elif bias is not None:
            kw["bias"] = bias
        if isinstance(scale, V):
            ins["scale"] = scale
        elif scale is not None:
            kw["scale"] = scale
        return self.op(q, "activation", {"out": out}, ins, func=func, **kw)

    def tt(self, out, in0, in1, op, q="dve"):
        return self.op(q, "tensor_tensor", {"out": out}, {"in0": in0, "in1": in1}, op=op)

    def ts(self, out, in0, s1, op0, s2=None, op1=None, q="dve"):
        ins = {"in0": in0}
        kw = {}
        if isinstance(s1, V):
            ins["scalar1"] = s1
        else:
            kw["scalar1"] = s1
        if isinstance(s2, V):
            ins["scalar2"] = s2
        else:
            kw["scalar2"] = s2
        if op1 is not None:
            kw["op1"] = op1
        return self.op(q, "tensor_scalar", {"out": out}, ins, op0=op0, **kw)

    def copy(self, out, in_, q="dve"):
        if q == "act":
            return self.op("act", "copy", {"out": out}, {"in_": in_})
        return self.op(q, "tensor_copy", {"out": out}, {"in_": in_})

    def memset(self, out, val, q="pool"):
        return self.op(q, "memset", {}, {}, out.ap, val) if False else self._memset(out, val, q)

    def _memset(self, out, val, q):
        oa = out.ap

        def fn(e):
            return e.memset(oa, val)
        return self.rec(q, fn, [], out.bufs)

    def cc(self, kind, rg, in_, out, incv=1):
        ia, oa = in_.ap.opt(), out.ap.opt()

        def fn(e):
            return e.collective_compute(kind, ALU.bypass, replica_groups=rg, ins=[ia], outs=[oa])
        o = self.rec("poolq", fn, in_.bufs, out.bufs)
        o.incv = incv
        return o

    def barrier(self):
        lasts = [self.ops[st][-1] for st in self.ops if self.ops[st]] + [o for o in self.slot_last if o is not None]
        for st in ("pe", "act", "dve", "pool", "sp"):
            o = Op(st, (lambda e: e.nop()), False)
            for d in lasts:
                d.inc = True
                o.deps.append(d)
            self.ops[st].append(o)
            self.allops.append(o)

    class _Scope:
        def __init__(self, kb):
            self.kb = kb

        def __enter__(self):
            self.old = self.kb.es
            self.kb.es = ExitStack()
            return self

        def __exit__(self, *a):
            self.kb.barrier()
            self.kb.es.close()
            self.kb.es = self.old

    def scope(self):
        return KB._Scope(self)

    def finish(self):
        nc = self.nc
        final = [o for o in self.outs]
        with ExitStack() as es:
            esem = {q: es.enter_context(nc.semaphore("s_" + q)) for q in COMPUTE + ("sp",)}
            dsem = [es.enter_context(nc.semaphore("d_%d" % i)) for i in range(NSLOT)]
            cnt = {q: 0 for q in COMPUTE + ("sp",)}
            dcnt = [0] * NSLOT
            for o in self.allops:
                if not o.inc:
                    continue
                if o.is_dma:
                    dcnt[o.slot] += o.incv
                    o.sem = dsem[o.slot]
                    o.val = dcnt[o.slot]
                else:
                    cnt[o.q] += 1
                    o.sem = esem[o.q]
                    o.val = cnt[o.q]
            block = es.enter_context(nc.Block())
            streams = self.ops

            def run(st, eng):
                waited = {}
                for o in streams[st]:
                    for d in o.deps:
                        key = id(d.sem)
                        if waited.get(key, 0) >= d.val:
                            continue
                        eng.wait_ge(d.sem, d.val)
                        waited[key] = d.val
                    ins = o.fn(eng)
                    if o.inc:
                        ins.then_inc(o.sem, o.incv if o.is_dma else 1)
                if st == "sp":
                    for o in final:
                        if waited.get(id(o.sem), 0) < o.val:
                            eng.wait_ge(o.sem, o.val)
                            waited[id(o.sem)] = o.val

            block.tensor(lambda e: run("pe", e))
            block.scalar(lambda e: run("act", e))
            block.vector(lambda e: run("dve", e))
            block.gpsimd(lambda e: run("pool", e))
            block.sync(lambda e: run("sp", e))
        self.es.close()
        return nc

import math
import numpy as np
import ml_dtypes
from concourse.bass_utils import run_bass_kernel_spmd

NPBF = ml_dtypes.bfloat16
D = 1024
T = 8192
S = 8192
NCH = T // 512
EPS_LN = 1e-5
EPS_RMS = 1e-6
ALPHA = 4 ** 0.25
OFF = {}
_o = 0
for _n, _w in [("nq", 512), ("kc", 128), ("vc", 128), ("ks", 128), ("vs", 128), ("kw", 128), ("vw", 128), ("ng", 24),
               ("dq", 512), ("dk", 512), ("dv", 512), ("cq", 256), ("ckv", 128), ("kr", 32), ("mg", 3072)]:
    OFF[_n] = (_o, _w)
    _o += _w
NA = OFF["mg"][0]


def run(nc, in_maps):
    res = run_bass_kernel_spmd(nc, in_maps, core_ids=list(range(8)))
    return res.results


def consts(K):
    c = {}
    c["ones32"] = K.sb([128, 128], F32)
    K.memset(c["ones32"][:, :], 1.0)
    c["onesb"] = K.sb([128, 128], BF16)
    K.memset(c["onesb"][:, :], 1.0)
    ident = K.sb([128, 128], F32)
    K.memset(ident[:, :], 0.0)
    K.op("pool", "affine_select", {"out": ident[:, :]}, {"in_": ident[:, :]}, pattern=[[-1, 128]],
         compare_op=ALU.not_equal, fill=1.0, base=0, channel_multiplier=1)
    c["ident32"] = ident
    return c


def ln_fm(K, c, src, g, b, out32, outb, psA, psB, tmp, N=512, eps=EPS_LN, nft=8, pre=None):
    sq = tmp["sq"]
    Dn = nft * 128
    for ft in range(nft):
        K.mm(psA[:, :N], c["ones32"][:, :], src[:, ft, :], start=(ft == 0), stop=(ft == nft - 1))
    for ft in range(nft):
        K.act(sq[:, ft, :], src[:, ft, :], AF.Square)
    for ft in range(nft):
        K.mm(psB[:, :N], c["ones32"][:, :], sq[:, ft, :], start=(ft == 0), stop=(ft == nft - 1))
    mean, msq, rstd = tmp["mean"], tmp["msq"], tmp["rstd"]
    K.ts(mean[:, :], psA[:, :N], 1.0 / Dn, ALU.mult)
    K.tt(msq[:, :], mean[:, :], mean[:, :], ALU.mult)
    K.op("dve", "scalar_tensor_tensor", {"out": rstd[:, :]}, {"in0": psB[:, :N], "in1": msq[:, :]},
         scalar=1.0 / Dn, op0=ALU.mult, op1=ALU.subtract)
    K.ts(rstd[:, :], rstd[:, :], eps, ALU.add)
    K.act(rstd[:, :], rstd[:, :], AF.Sqrt)
    K.op("dve", "reciprocal", {"out": rstd[:, :]}, {"in_": rstd[:, :]})
    for ft in range(nft):
        t = sq
        K.tt(t[:, ft, :], src[:, ft, :], mean[:, :], ALU.subtract)
        K.tt(t[:, ft, :], t[:, ft, :], rstd[:, :], ALU.mult, q="pool")
        K.act(out32[:, ft, :], t[:, ft, :], AF.Identity, bias=b[:, ft:ft + 1], scale=g[:, ft:ft + 1])
        if outb is not None:
            K.copy(outb[:, ft, :], out32[:, ft, :], q="act")


def rms_fm(K, c, src, g, outb, psA, tmp, nft, N=512):
    sq = tmp["sq"]
    Dn = nft * 128
    for ft in range(nft):
        K.act(sq[:, ft, :], src[:, ft, :], AF.Square)
    for ft in range(nft):
        K.mm(psA[:, :N], c["ones32"][:, :], sq[:, ft, :], start=(ft == 0), stop=(ft == nft - 1))
    rstd = tmp["rstd"]
    K.ts(rstd[:, :], psA[:, :N], 1.0 / Dn, ALU.mult, EPS_RMS, ALU.add)
    K.act(rstd[:, :], rstd[:, :], AF.Sqrt)
    K.op("dve", "reciprocal", {"out": rstd[:, :]}, {"in_": rstd[:, :]})
    for ft in range(nft):
        K.tt(sq[:, ft, :], src[:, ft, :], rstd[:, :], ALU.mult)
        K.act(outb[:, ft, :], sq[:, ft, :], AF.Copy if False else AF.Identity, scale=g[:, ft:ft + 1])


def build_A(K, c, layer0, io):
    if layer0:
        x = io["x"]; lng = io["ln_g"]; lnb = io["ln_b"]
        hT_out = io["hT32"]
    else:
        hT_in = io["hT32"]
    w_in = io["w_in"]; w_uq = io["w_uq"]; w_ukv = io["w_ukv"]
    qg = io["qg"]; kvg = io["kvg"]; cosT = io["cosT"]; sinT = io["sinT"]; protT = io["protT"]
    fm_outs = {n: io[n] for n in ("nqT", "kcT", "vcT", "ksT", "kwT", "ngT", "dqT", "dkT", "qmT", "kropeT", "knopeT")}
    tm_outs = {n: io[n] for n in ("vs", "vw", "dv", "vm")}

    W = K.sb([128, 8, NA], BF16)
    for kt in range(8):
        K.dma(W.k(kt)[:, kt, :], w_in[kt * 128:(kt + 1) * 128, 0:NA], q="sp")
    Wuq = K.sb([128, 2, 768], BF16)
    for kt in range(2):
        K.dma(Wuq[:, kt, :], w_uq[kt * 128:(kt + 1) * 128, :])
    Wukv = K.sb([128, 1024], BF16)
    K.dma(Wukv[:, :], w_ukv[:, :])
    qg_s = K.sb([128, 2]); K.dma(qg_s[:, :], qg[:, :])
    kvg_s = K.sb([128, 1]); K.dma(kvg_s[:, :], kvg[:, :])
    prot_s = K.sb([32, 32]); K.dma(prot_s[:, :], protT[:, :])
    if layer0:
        g_s = K.sb([128, 8]); K.dma(g_s[:, :], lng[:, :])
        b_s = K.sb([128, 8]); K.dma(b_s[:, :], lnb[:, :])

    def Wv(kt, c0, n):
        return W.k(kt)[:, kt, c0:c0 + n]

    ps = [K.ps([128, 512]) for _ in range(8)]
    pi = [0]

    def nps():
        pi[0] = (pi[0] + 1) % 6
        return ps[pi[0]]
    psA, psB = ps[6], ps[7]

    xT = K.sb([128, 8, 512], F32)
    tmp = {"sq": K.sb([128, 8, 512], F32), "mean": K.sb([128, 512]), "msq": K.sb([128, 512]), "rstd": K.sb([128, 512])}
    h32 = K.sb([128, 8, 512], F32)
    hb = K.sb([128, 8, 512], BF16)
    xc = [K.sb([128, 4, D], F32) for _ in range(2)] if layer0 else None
    stg = [K.sb([128, 512], BF16) for _ in range(4)]
    si = [0]

    def nstg():
        si[0] = (si[0] + 1) % 4
        return stg[si[0]]
    cq32 = K.sb([128, 2, 512], F32)
    cqb = K.sb([128, 2, 512], BF16)
    ckv32 = K.sb([128, 1, 512], F32)
    ckvb = K.sb([128, 1, 512], BF16)
    cos_s = K.sb([32, 512]); sin_s = K.sb([32, 512])
    xs32 = K.sb([96, 512], F32)
    r1 = K.sb([32, 512], F32); r2 = K.sb([32, 512], F32)
    qstg = [K.sb([96, 512], BF16) for _ in range(2)]

    evq = [0]

    def evac(out, in_, func=None):
        evq[0] += 1
        if func is not None:
            K.act(out, in_, func)
        elif evq[0] % 2:
            K.copy(out, in_, q="act")
        else:
            K.copy(out, in_, q="dve")

    for ch in range(NCH):
        t0 = ch * 512
        tsl = slice(t0, t0 + 512)
        if layer0:
            xcc = xc[ch % 2]
            for sub in range(4):
                K.dma(xcc.k(sub)[:, sub, :], x[t0 + sub * 128:t0 + (sub + 1) * 128, :], q="sp")
            for ft in range(8):
                p = nps()
                for sub in range(4):
                    K.tr(p[:, sub * 128:(sub + 1) * 128], xcc.k(sub)[:, sub, ft * 128:(ft + 1) * 128], c["ident32"][:, :])
                evac(xT[:, ft, :], p[:, :])
            ln_fm(K, c, xT, g_s, b_s, h32, hb, psA, psB, tmp)
            for ft in range(8):
                (K.dma(hT_out.k((ch, ft))[ft * 128:(ft + 1) * 128, tsl], h32[:, ft, :], q="poolq"))
        else:
            for ft in range(8):
                K.dma(h32[:, ft, :], hT_in[ft * 128:(ft + 1) * 128, tsl], q="sp")
            for ft in range(8):
                K.copy(hb[:, ft, :], h32[:, ft, :], q=("act" if ft % 2 else "dve"))
        K.dma(cos_s[:, :], cosT[:, tsl]); K.dma(sin_s[:, :], sinT[:, tsl])

        def proj_fm(c0, M):
            p = nps()
            for kt in range(8):
                K.mm(p[:M, :], Wv(kt, c0, M), hb[:, kt, :], start=(kt == 0), stop=(kt == 7))
            return p

        def fm_to(name, r0, M, p, func=None):
            s_ = nstg()
            evac(s_[:M, :], p[:M, :], func)
            (K.dma(fm_outs[name].k((ch, r0))[r0:r0 + M, tsl], s_[:M, :], q="poolq"))

        for name in ("nq", "kc", "vc", "ks", "kw", "dq", "dk"):
            c0, w = OFF[name]
            for m0 in range(0, w, 128):
                fm_to(name + "T", m0, 128, proj_fm(c0 + m0, 128))
        fm_to("ngT", 0, 24, proj_fm(OFF["ng"][0], 24), AF.Sigmoid)
        for name in ("vs", "vw", "dv"):
            c0, w = OFF[name]
            for sub in range(4):
                p = nps()
                for kt in range(8):
                    K.mm(p[:, :w], hb[:, kt, sub * 128:(sub + 1) * 128], Wv(kt, c0, w), start=(kt == 0), stop=(kt == 7))
                s_ = nstg()
                evac(s_[:, :w], p[:, :w])
                (K.dma(tm_outs[name].k((ch, sub))[t0 + sub * 128:t0 + (sub + 1) * 128, :], s_[:, :w], q="poolq"))
        for m in range(2):
            p = proj_fm(OFF["cq"][0] + m * 128, 128)
            evac(cq32[:, m, :], p[:, :])
        rms_fm(K, c, cq32, qg_s, cqb, psA, tmp, 2)
        for h in range(8):
            p = nps()
            for kt in range(2):
                K.mm(p[:96, :], Wuq[:, kt, h * 96:(h + 1) * 96], cqb[:, kt, :], start=(kt == 0), stop=(kt == 1))
            K.copy(xs32[:, :], p[:96, :], q="act")
            p2 = nps()
            K.mm(p2[:32, :], prot_s[:, :], xs32[0:32, :])
            K.tt(r1[:, :], xs32[0:32, :], cos_s[:, :], ALU.mult, q="pool")
            K.tt(r2[:, :], p2[:32, :], sin_s[:, :], ALU.mult)
            qs = qstg[h % 2]
            K.tt(qs[0:32, :], r1[:, :], r2[:, :], ALU.add)
            K.copy(qs[32:64, :], xs32[32:64, :], q="pool")
            K.copy(qs[64:96, :], xs32[64:96, :], q="pool")
            (K.dma(fm_outs["qmT"].k((ch, h))[h * 96:(h + 1) * 96, tsl], qs[:, :], q="poolq"))
        p = proj_fm(OFF["ckv"][0], 128)
        evac(ckv32[:, 0, :], p[:, :])
        rms_fm(K, c, ckv32, kvg_s, ckvb, psA, tmp, 1)
        for m in range(4):
            p = nps()
            K.mm(p[:, :], Wukv[:, m * 128:(m + 1) * 128], ckvb[:, 0, :])
            fm_to("knopeT", m * 128, 128, p)
        for sub in range(4):
            p = nps()
            K.mm(p[:, :], ckvb[:, 0, sub * 128:(sub + 1) * 128], Wukv[:, 512:1024])
            s_ = nstg()
            evac(s_[:, :], p[:, :])
            (K.dma(tm_outs["vm"].k((ch, sub))[t0 + sub * 128:t0 + (sub + 1) * 128, :], s_[:, :], q="poolq"))
        p = proj_fm(OFF["kr"][0], 32)
        K.copy(xs32[0:32, :], p[:32, :], q="act")
        p2 = nps()
        K.mm(p2[:32, :], prot_s[:, :], xs32[0:32, :])
        K.tt(r1[:, :], xs32[0:32, :], cos_s[:, :], ALU.mult, q="pool")
        K.tt(r2[:, :], p2[:32, :], sin_s[:, :], ALU.mult)
        s_ = nstg()
        K.tt(s_[0:32, :], r1[:, :], r2[:, :], ALU.add)
        (K.dma(fm_outs["kropeT"].k(ch)[:, tsl], s_[0:32, :], q="poolq"))


NEG = -1.0e9


def load_tm(K, Vt, src, c0, dv, nper=4):
    n = S // 128
    for i in range(0, n, nper):
        K.dma(Vt[:, i:i + nper, 0:dv], rv(src[:, :], src.t[i * 128:(i + nper) * 128, c0:c0 + dv].rearrange("(n p) d -> p n d", p=128)))


class AttnCtx:
    def __init__(self, K, c):
        self.K, self.c = K, c
        self.NS = 3
        self.NP = 4
        self.S_ps = [K.ps([128, 512]) for _ in range(self.NS)]
        self.O_ps = [K.ps([128, 512]) for _ in range(3)]
        self.Sum_ps = [K.ps([128, 512])]
        self.misc_ps = K.ps([128, 512])
        self.si = 0
        self.oi = 0
        self.P = [K.sb([128, 512], BF16) for _ in range(self.NP)]
        self.Sm = [K.sb([128, 512], F32) for _ in range(3)]
        self.pi = 0
        self.mi = 0
        self.R = [K.sb([128, 512], F32) for _ in range(3)]
        self.pending = []
        self.depth = 2
        self.deferred = []

    def nextS(self):
        self.si = (self.si + 1) % self.NS
        return self.S_ps[self.si]

    def nextO(self):
        self.oi = (self.oi + 1) % 3
        j = -1
        for idx, d in enumerate(self.deferred):
            if self.oi in d[3]:
                j = idx
        if j >= 0:
            self.flush()
        for _ in range(j + 1):
            self.deferred.pop(0)[0]()
        return self.O_ps[self.oi], self.Sum_ps[0], self.R[self.oi]

    def defer(self, fn, thresh, banks, tag=None):
        self.deferred.append([fn, 0, thresh, banks, tag])

    def force_tag(self, tag):
        j = -1
        for idx, d in enumerate(self.deferred):
            if d[4] == tag:
                j = idx
        if j >= 0:
            self.flush()
        for _ in range(j + 1):
            self.deferred.pop(0)[0]()

    def tick(self):
        for d in self.deferred:
            d[1] += 1
        while self.deferred and self.deferred[0][1] >= self.deferred[0][2]:
            self.deferred.pop(0)[0]()

    def drain(self):
        self.flush()
        while self.deferred:
            self.deferred.pop(0)[0]()

    def step(self, qk_list, N, scale, mask, v_lhsT, dv, O, Sum, M, first, last, extra=None, ones=None, Pt=None, mg=1, merged=False):
        K = self.K
        Sp = self.nextS()
        for (l, r, c0, n) in qk_list:
            K.mm(Sp[:, c0:c0 + n], l, r, start=True, stop=(extra is None))
        if extra is not None:
            K.mm(Sp[:, :N], extra[0], extra[1], start=False, stop=True)
        src = Sp[:, :N]
        if mask is not None:
            self.mi = (self.mi + 1) % 3
            sm = self.Sm[self.mi]
            if mg == 1:
                K.tt(sm[:, :N], Sp[:, :N], mask, ALU.add)
            else:
                mb = rv(mask, mask.ap.unsqueeze(1).to_broadcast([128, mg, N // mg]))
                K.tt(rv(sm[:, :], sm.t[:, :N].rearrange("p (m q) -> p m q", m=mg)),
                     rv(Sp[:, :], Sp.t[:, :N].rearrange("p (m q) -> p m q", m=mg)), mb, ALU.add)
            src = sm[:, :N]
        if Pt is None:
            self.pi = (self.pi + 1) % self.NP
            Pt = self.P[self.pi][:, :N]
        K.act(Pt, src, AF.Exp, scale=scale)
        onesv = ones if ones is not None else self.c["onesb"][:, :M]

        def pv():
            if merged:
                K.mm(O[:, :N], v_lhsT, Pt, start=first, stop=last)
            else:
                K.mm(O[:dv, :N], v_lhsT, Pt, start=first, stop=last)
                K.mm(Sum[:M, :N], onesv, Pt, start=first, stop=last)
        self.pending.append(pv)
        if len(self.pending) > self.depth:
            self.pending.pop(0)()
        self.tick()
        return Pt

    def flush(self):
        while self.pending:
            self.pending.pop(0)()

    def recip_m1(self, R, O, N):
        K = self.K
        K.ts(R[64:128, :N], O[64:128, :N], 1e-30, ALU.max)
        K.op("dve", "reciprocal", {"out": R[64:128, :N]}, {"in_": R[64:128, :N]})

    def recip_m2(self, R, N):
        K = self.K
        mp = self.misc_ps
        K.mm(mp[0:64, :N], self.c["ident32"][64:128, 64:128], R[64:128, :N])
        K.copy(R[0:64, :N], mp[0:64, :N], q="act")

    def recip(self, R, Sum, M, N):
        K = self.K
        self.flush()
        K.ts(R[:M, :N], Sum[:M, :N], 1e-30, ALU.max)
        K.op("dve", "reciprocal", {"out": R[:M, :N]}, {"in_": R[:M, :N]})


def build_B(K, c, lam_init, io, do=("diff", "mla", "nsa")):
    A = AttnCtx(K, c)
    NQB = S // 128
    cm512 = io["cm512"]
    cm_s = K.sb([128, 4, 512], F32)
    for j in range(4):
        K.dma(cm_s[:, j, :], cm512[j, :, :])
    wlow = io["wlow"]
    wlow_s = K.sb([128, 128], F32)
    K.dma(wlow_s[:, :], wlow[:, :])
    kaug_tok = io["kaug_tok"]

    if "diff" in do:
        dqT = io["dqT"]; dkT = io["dkT"]; dvv = io["dv"]; qaug_d = io["qaug_diff"]
        lam_p = io["lam_p"]; subg = io["subg"]; o_diff = io["o_diffT"]
        with K.scope():
            lp = K.sb([128, 256]); K.dma(lp[:, :], lam_p[:, :])
            pr = K.sb([128, 128]); l2 = K.sb([128, 2]); lam = K.sb([128, 1]); nlam = K.sb([128, 1])
            K.tt(pr[:, 0:64], lp[:, 0:64], lp[:, 64:128], ALU.mult)
            K.tt(pr[:, 64:128], lp[:, 128:192], lp[:, 192:256], ALU.mult)
            K.op("dve", "tensor_reduce", {"out": l2[:, 0:1]}, {"in_": pr[:, 0:64]}, axis=AX.X, op=ALU.add)
            K.op("dve", "tensor_reduce", {"out": l2[:, 1:2]}, {"in_": pr[:, 64:128]}, axis=AX.X, op=ALU.add)
            K.act(l2[:, :], l2[:, :], AF.Exp)
            K.tt(lam[:, :], l2[:, 0:1], l2[:, 1:2], ALU.subtract)
            K.ts(nlam[:, :], lam[:, :], lam_init, ALU.add, -1.0, ALU.mult)
            sg = K.sb([128, 1]); K.dma(sg[:, :], subg[:, :])
            K.ts(sg[:, :], sg[:, :], 1.0 - lam_init, ALU.mult)
            QA = K.sb([73, 2, S], BF16)
            KAt = K.sb([73, 2, S], BF16)
            Vt = K.sb([128, NQB, 128], BF16)
            o1 = K.sb([128, 256], F32); o2 = K.sb([128, 256], F32); sq = K.sb([128, 256], F32)
            ob = [K.sb([128, 256], BF16) for _ in range(2)]
            for h in range(4):
                for m in range(2):
                    r0 = h * 128 + m * 64
                    K.dma(QA[0:64, m, :], dqT[r0:r0 + 64, :])
                    K.dma(QA[64:73, m, :], qaug_d[h, :, :])
                    K.dma(KAt[0:64, m, :], dkT[r0:r0 + 64, :])
                    K.dma(KAt[64:73, m, :], kaug_tok[:, :])
                load_tm(K, Vt, dvv, h * 128, 128)
                for qc in range(S // 256):
                    q0 = qc * 256
                    O, Sum, R = A.nextO()
                    nkt = 2 * qc + 2
                    for kt in range(nkt):
                        j = kt - 2 * qc
                        mask = cm_s[:, j, 0:256] if j >= 0 else None
                        qk = [(KAt[:, m, kt * 128:(kt + 1) * 128], QA[:, m, q0:q0 + 256], m * 256, 256) for m in range(2)]
                        A.step(qk, 512, 0.125, mask, Vt[:, kt, :], 128, O, Sum, 128, kt == 0, kt == nkt - 1, mg=2)
                    A.recip(R, Sum, 128, 512)

                    def fin(O=O, R=R, h=h, qc=qc, q0=q0):
                        K.tt(o1[:, :], O[:, 0:256], R[:, 0:256], ALU.mult)
                        K.tt(o2[:, :], O[:, 256:512], R[:, 256:512], ALU.mult)
                        K.op("dve", "scalar_tensor_tensor", {"out": o1[:, :]}, {"in0": o2[:, :], "in1": o1[:, :], "scalar": nlam[:, 0:1]},
                             op0=ALU.mult, op1=ALU.add)
                        K.act(sq[:, :], o1[:, :], AF.Square)
                        mp = A.misc_ps
                        K.mm(mp[:, :256], c["ones32"][:, :], sq[:, :])
                        K.ts(sq[:, :], mp[:, :256], 1.0 / 128, ALU.mult, EPS_RMS, ALU.add)
                        K.act(sq[:, :], sq[:, :], AF.Sqrt)
                        K.op("dve", "reciprocal", {"out": sq[:, :]}, {"in_": sq[:, :]})
                        K.tt(o1[:, :], o1[:, :], sq[:, :], ALU.mult)
                        obb = ob[qc % 2]
                        K.act(obb[:, :], o1[:, :], AF.Identity, scale=sg[:, 0:1])
                        K.dma(o_diff.k((h, qc))[h * 128:(h + 1) * 128, q0:q0 + 256], obb[:, :], q="poolq")
                    A.defer(fin, 5, {A.oi})
            A.drain()

    if "mla" in do:
        qmT = io["qmT"]; kropeT = io["kropeT"]; knopeT = io["knopeT"]; mv = io["vm"]; o_mla = io["o_mlaT"]
        msc = 96 ** -0.5
        with K.scope():
            QA = K.sb([96, S], BF16)
            KAt = K.sb([96, S], BF16)
            Vt = K.sb([128, NQB, 128], BF16)
            K.memset(Vt[:, :, :], 1.0)
            ob = [K.sb([64, 512], BF16) for _ in range(2)]
            for h in range(8):
                K.dma(QA[:, :], qmT[h * 96:(h + 1) * 96, :])
                K.dma(KAt[0:32, :], kropeT[:, :])
                K.dma(KAt[32:96, :], knopeT[h * 64:(h + 1) * 64, :])
                load_tm(K, Vt, mv, h * 64, 64)
                for qc in range(S // 512):
                    q0 = qc * 512
                    O, Sum, R = A.nextO()
                    nkt = 4 * qc + 4
                    for kt in range(nkt):
                        j = kt - 4 * qc
                        mask = cm_s[:, j, :] if j >= 0 else None
                        A.step([(KAt[:, kt * 128:(kt + 1) * 128], QA[:, q0:q0 + 512], 0, 512)], 512, msc, mask,
                               Vt[:, kt, :], 64, O, Sum, 64, kt == 0, kt == nkt - 1, merged=True)
                    def fin1(O=O, R=R):
                        A.recip_m1(R, O, 512)

                    def fin2(O=O, R=R, h=h, qc=qc, q0=q0):
                        A.recip_m2(R, 512)
                        obb = ob[qc % 2]
                        K.tt(obb[:, :], O[:64, :], R[:64, :], ALU.mult)
                        K.dma(o_mla.k((h, qc))[h * 64:(h + 1) * 64, q0:q0 + 512], obb[:, :], q="poolq")
                    A.defer(fin1, 3, {A.oi})
                    A.defer(fin2, 7, {A.oi})
            A.drain()
    if "nsa" in do:
        for g in range(2):
            build_nsa(K, c, A, cm_s, wlow_s, kaug_tok, io, g)


def build_nsa(K, c, A, cm_s, wlow_s, kaug_tok, io, grp):
    NQB = S // 128
    nqT = io["nqT"]; qaug_n = io["qaug_nsa"]
    kcT = rv(io["kcT"][:, :], io["kcT"].t[grp * 64:(grp + 1) * 64, :])
    vcT = rv(io["vcT"][:, :], io["vcT"].t[grp * 64:(grp + 1) * 64, :])
    ksa = rv(io["ksT"][:, :], io["ksT"].t[grp * 64:(grp + 1) * 64, :])
    kwa = rv(io["kwT"][:, :], io["kwT"].t[grp * 64:(grp + 1) * 64, :])
    vs_in = io["vs"]; vw_in = io["vw"]
    ngT = rv(io["ngT"][:, :], io["ngT"].t[grp * 12:(grp + 1) * 12, :])
    w1 = {"k": io["cmp_w1_k"], "v": io["cmp_w1_v"]}
    w2 = {"k": io["cmp_w2_k"], "v": io["cmp_w2_v"]}
    pos = {"k": io["posk"], "v": io["posv"]}
    kaug_cmp = io["kaug_cmp"]; cmask = io["cmask"]; cprev = io["cprev"]; amat = io["amat"]
    addmask = io["addmask"]; E_in = io["E"]; oh_in = io["oh"]; o_nsa = io["o_nsaT"]

    KAc = K.sb([73, 512], BF16)
    Vc = K.sb([128, 4, 64], F32)
    K.memset(KAc[:, :], 0.0)
    K.dma(KAc[64:73, :], kaug_cmp[:, :])
    with K.scope():
        xT = K.sb([64, S], BF16)
        w1s = K.sb([64, 32, 256], BF16)
        w2s = K.sb([128, 2, 64], BF16)
        pos32 = K.sb([64, 32], F32); posb = K.sb([64, 32], BF16)
        g = K.sb([128, 2, 512], BF16)
        bias = K.sb([128, 1], F32)
        x = K.sb([128, 512], F32); x2 = K.sb([128, 512], F32)
        K.memset(g[:, :, :], 0.0)
        for which, src in (("k", kcT), ("v", vcT)):
            K.dma(xT[:, :], src)
            for l0 in range(0, 32, 8):
                K.dma(w1s[:, l0:l0 + 8, :], rv(w1[which][:, :], w1[which].t[l0 * 64:(l0 + 8) * 64, :].rearrange("(l d) j -> d l j", d=64)))
            K.dma(w2s[:, :, :], rv(w2[which][:, :], w2[which].t[:, :].rearrange("(jh p) d -> p jh d", p=128)))
            K.dma(pos32[:, :], pos[which][:, :])
            K.copy(posb[:, :], pos32[:, :])
            xv = xT.t[:, :].rearrange("d (c r) -> d c r", r=16)
            for jh in range(2):
                hp = A.nextS()
                for l in range(32):
                    rhs = rv(xT[:, :], xv[:, (l // 16):(l // 16) + 511, l % 16])
                    K.mm(hp[:, :511], w1s[:, l, jh * 128:(jh + 1) * 128], rhs, start=(l == 0), stop=(l == 31))
                bp = A.misc_ps
                for l in range(32):
                    K.mm(bp[:, 0:1], w1s[:, l, jh * 128:(jh + 1) * 128], posb[:, l:l + 1], start=(l == 0), stop=(l == 31))
                K.copy(bias[:, :], bp[:, 0:1])
                K.ts(x[:, :511], hp[:, :511], bias[:, 0:1], ALU.add)
                K.tt(x2[:, :511], x[:, :511], x[:, :511], ALU.mult)
                K.ts(x2[:, :511], x2[:, :511], 0.044715, ALU.mult, 1.0, ALU.add)
                K.tt(x2[:, :511], x2[:, :511], x[:, :511], ALU.mult)
                K.act(x2[:, :511], x2[:, :511], AF.Sigmoid, scale=1.5957691216057308)
                K.tt(g[:, jh, :511], x[:, :511], x2[:, :511], ALU.mult)
            if which == "k":
                kp = A.nextS()
                for jh in range(2):
                    K.mm(kp[:64, :511], w2s[:, jh, :], g[:, jh, :511], start=(jh == 0), stop=(jh == 1))
                K.copy(KAc[0:64, :511], kp[:64, :511])
            else:
                for ct in range(4):
                    vp = A.nextS()
                    for jh in range(2):
                        K.mm(vp[:, :64], g[:, jh, ct * 128:(ct + 1) * 128], w2s[:, jh, :], start=(jh == 0), stop=(jh == 1))
                    K.copy(Vc[:, ct, :], vp[:, :64])
    with K.scope():
        KAs = K.sb([73, S], BF16); KAw = K.sb([73, S], BF16)
        K.dma(KAs[0:64, :], ksa); K.dma(KAs[64:73, :], kaug_tok[:, :])
        K.dma(KAw[0:64, :], kwa); K.dma(KAw[64:73, :], kaug_tok[:, :])
        Vs = K.sb([128, NQB, 128], BF16); Vw = K.sb([128, NQB, 128], BF16)
        K.memset(Vs[:, :, :], 1.0); K.memset(Vw[:, :, :], 1.0)
        load_tm(K, Vs, vs_in, grp * 64, 64)
        load_tm(K, Vw, vw_in, grp * 64, 64)
        E_s = K.sb([128, S], BF16); K.dma(E_s[:, :], E_in[:, :])
        ng_s = K.sb([12, S], BF16); K.dma(ng_s[:, :], ngT)
        oh_s = K.sb([12, 768], BF16); K.dma(oh_s[:, :], oh_in[:, :])
        cprev_s = K.sb([128, 128], F32); K.dma(cprev_s[:, :], cprev[:, :])
        am_s = K.sb([128, 4, 128], F32)
        for ct in range(4):
            K.dma(am_s[:, ct, :], amat[ct, :, :])
        Qt = [K.sb([73, 4, 128], BF16) for _ in range(2)]
        cmk = [K.sb([128, 128], F32) for _ in range(2)]
        adm = [K.sb([128, 128], F32) for _ in range(2)]
        Pc = [K.sb([128, 512], F32) for _ in range(4)]
        pg = K.sb([128, 4, 128], F32)
        ob = [K.sb([64, 512], F32) for _ in range(3)]
        obc = [K.sb([64, 512], F32) for _ in range(2)]
        gs = K.sb([64, 512], F32)
        sc = K.sb([128, 128], F32); sc2 = K.sb([128, 128], F32)
        m8a = K.sb([128, 8], F32); m8b = K.sb([128, 8], F32)
        negsel = K.sb([128, 128], F32)
        nsT = K.sb([128, 128], BF16)
        outb = [K.sb([64, 512], BF16) for _ in range(2)]
        ident = c["ident32"]
        nsTs = [nsT, K.sb([128, 128], BF16)]
        obc3 = [obc[0], obc[1], K.sb([64, 512], F32)]

        def cmpA(qb):
            q0 = qb * 128
            Q = Qt[qb % 2]
            K.dma(Q[0:64, :, :], rv(nqT[:, :], nqT.t[grp * 256:(grp + 1) * 256, q0:q0 + 128].rearrange("(h d) s -> d h s", h=4)))
            K.dma(Q[64:73, :, :], qaug_n[:, grp * 4:(grp + 1) * 4, q0:q0 + 128])
            Qv = Q[:, :, :]
            nct = qb // 16 + 1
            ck = cmk[qb % 2]
            K.dma(ck[:, :], cmask[1 if nct == 4 else 0, qb % 16, :, :])
            ad = adm[qb % 2]
            K.dma(ad[:, :], addmask[qb, :, :])
            O, Sum, R = A.nextO()
            for ct in range(nct):
                last = (ct == nct - 1)
                cmsk = ck[:, :] if last else (cprev_s[:, :] if (qb % 16 == 0 and ct == nct - 2) else None)
                A.step([(KAc[:, ct * 128:(ct + 1) * 128], Qv, 0, 512)], 512, 0.125, cmsk,
                       Vc[:, ct, :], 64, O, Sum, 128, ct == 0, last, ones=c["ones32"][:, :], Pt=Pc[ct][:, :], mg=4)
            A.recip(R, Sum, 128, 512)
            K.tt(obc3[qb % 3][:, :], O[:64, :], R[:64, :], ALU.mult)
            for ct in range(nct):
                K.tt(Pc[ct][:, :], Pc[ct][:, :], R[:, :], ALU.mult)
                K.tt(pg[:, ct, :], Pc[ct][:, 0:128], Pc[ct][:, 128:256], ALU.add, q="pool")
                K.tt(pg[:, ct, :], pg[:, ct, :], Pc[ct][:, 256:384], ALU.add, q="pool")
                K.tt(pg[:, ct, :], pg[:, ct, :], Pc[ct][:, 384:512], ALU.add, q="pool")

        def cmpB(qb):
            nct = qb // 16 + 1
            ad = adm[qb % 2]
            mp = A.misc_ps
            for ct in range(nct):
                K.mm(mp[:, 0:128], pg[:, ct, :], am_s[:, ct, :], start=(ct == 0), stop=(ct == nct - 1))
            K.tt(sc[:, :], mp[:, 0:128], ad[:, :], ALU.add)
            K.op("dve", "max", {"out": m8a[:, :]}, {"in_": sc[:, :]})
            K.op("dve", "match_replace", {"out": sc2[:, :]}, {"in_to_replace": m8a[:, :], "in_values": sc[:, :]}, imm_value=-3.0e4)
            K.op("dve", "max", {"out": m8b[:, :]}, {"in_": sc2[:, :]})
            K.ts(negsel[:, :], sc[:, :], m8b[:, 7:8], ALU.is_lt, -30000.0, ALU.mult)

        def cmpC(qb):
            tp = A.nextS()
            K.tr(tp[:, 0:128], negsel[:, :], ident[:, :])
            K.copy(nsTs[qb % 2][:, :], tp[:, 0:128])

        cmpA(0); cmpB(0); cmpC(0)
        for qb in range(NQB):
            q0 = qb * 128
            Qv = Qt[qb % 2][:, :, :]
            A.force_tag(("C", qb))
            if qb + 1 < NQB:
                cmpA(qb + 1)
                A.defer((lambda qn=qb + 1: cmpB(qn)), 8, set(), tag=("B", qb + 1))
                A.defer((lambda qn=qb + 1: cmpC(qn)), 16, set(), tag=("C", qb + 1))
            nsq = nsTs[qb % 2]
            nsv = rv(nsq[:, :], nsq.t[:, :].unsqueeze(1).to_broadcast([128, 4, 128]))
            O, Sum, R = A.nextO()
            for kt in range(qb + 1):
                last = (kt == qb)
                A.step([(KAs[:, kt * 128:(kt + 1) * 128], Qv, 0, 512)], 512, 0.125, cm_s[:, 0, 0:128] if last else None,
                       Vs[:, kt, :], 64, O, Sum, 64, kt == 0, last, extra=(E_s[:, kt * 128:(kt + 1) * 128], nsv), mg=4, merged=True)
            Osel, Rsel, bsel = O, R, A.oi

            def sel1(O=O, R=R):
                A.recip_m1(R, O, 512)
            A.defer(sel1, 3, {A.oi})
            O, Sum, R = A.nextO()
            k0 = max(0, qb - 4)
            for kt in range(k0, qb + 1):
                last = (kt == qb)
                mask = cm_s[:, 0, 0:128] if last else (wlow_s[:, :] if kt == qb - 4 else None)
                A.step([(KAw[:, kt * 128:(kt + 1) * 128], Qv, 0, 512)], 512, 0.125, mask,
                       Vw[:, kt, :], 64, O, Sum, 64, kt == k0, last, mg=4, merged=True)
            Owin, Rwin, bwin = O, R, A.oi

            def win1(O=O, R=R):
                A.recip_m1(R, O, 512)
            A.defer(win1, 3, {A.oi})

            def post(Osel=Osel, Rsel=Rsel, Owin=Owin, Rwin=Rwin, qb=qb, q0=q0):
                A.recip_m2(Rsel, 512)
                K.tt(ob[1][:, :], Osel[:64, :], Rsel[:64, :], ALU.mult)
                A.recip_m2(Rwin, 512)
                K.tt(ob[2][:, :], Owin[:64, :], Rwin[:64, :], ALU.mult)
                obs = [obc3[qb % 3], ob[1], ob[2]]
                for gi in range(3):
                    gp = A.nextS()
                    for h in range(4):
                        j = h * 3 + gi
                        K.mm(gp[:64, h * 128:(h + 1) * 128], oh_s[:, j * 64:(j + 1) * 64], ng_s[:, q0:q0 + 128])
                    if gi == 0:
                        K.tt(gs[:, :], obs[0][:, :], gp[:64, :], ALU.mult)
                    else:
                        K.tt(obs[gi][:, :], obs[gi][:, :], gp[:64, :], ALU.mult)
                        K.tt(gs[:, :], gs[:, :], obs[gi][:, :], ALU.add, q="pool")
                oo = outb[qb % 2]
                K.copy(oo[:, :], gs[:, :], q="act")
                for h in range(4):
                    K.dma(o_nsa.k((grp, qb, h))[grp * 256 + h * 64:grp * 256 + (h + 1) * 64, q0:q0 + 128], oo[:, h * 128:(h + 1) * 128], q="poolq")
            A.defer(post, 7, {bsel, bwin})
        A.drain()


def build_C(K, c, last_layer, io, l):
    hT_in = io["hT32"]
    oT = {n: io[n] for n in ("o_nsaT", "o_diffT", "o_mlaT")}
    w_mg = io["w_in"]
    w_br = {n: io[n] for n in ("w_br_nsa", "w_br_diff", "w_br_mla")}
    w_out = io["w_out"]
    ln1g = io["ln1_g"]; ln1b = io["ln1_b"]; ln2g = io["ln2_g"]; ln2b = io["ln2_b"]
    rw = io["router_w"]; rb = io["router_b"]
    w1 = io["moe_w1"]; w3 = io["moe_w3"]; w2 = io["moe_w2"]
    h1_scr = K.dram("h1_scr%d" % l, [D, T], F32, "Internal")
    if last_layer:
        out = io["out"]
    else:
        hT_out = io["hT32_next"]

    ps = [K.ps([128, 512]) for _ in range(8)]
    pi = [0]

    def nps():
        pi[0] = (pi[0] + 1) % 6
        return ps[pi[0]]
    psA, psB = ps[6], ps[7]
    names = ("o_nsaT", "o_diffT", "o_mlaT")
    wnames = ("w_br_nsa", "w_br_diff", "w_br_mla")

    cmb_all = K.sb([128, NCH * 4, 16], F32)
    with K.scope():
        rw_s = K.sb([128, 8, 16]); K.dma(rw_s[:, :, :], rw[:, :, :])
        rb_s = K.sb([128, 16]); K.dma(rb_s[:, :], rb[:, :])
        aff = K.sb([128, 16]); sel = K.sb([128, 16]); pair = K.sb([128, 4, 6]); gsc = K.sb([128, 4]); gmx = K.sb([128, 1])
        goh = K.sb([128, 4]); selm = K.sb([128, 16]); m8 = K.sb([128, 8]); cho = K.sb([128, 16]); gsum = K.sb([128, 1])
        Wmg = K.sb([128, 8, 3072], BF16)
        for kt in range(8):
            K.dma(Wmg.k(kt)[:, kt, :], w_mg[kt * 128:(kt + 1) * 128, NA:NA + 3072])
        Wbr = {}
        for n in w_br:
            Wbr[n] = K.sb([128, 4, D], BF16)
            for kt in range(4):
                K.dma(Wbr[n][:, kt, :], w_br[n][kt * 128:(kt + 1) * 128, :])
        Wo = K.sb([128, 8, D], BF16)
        for kt in range(8):
            K.dma(Wo[:, kt, :], w_out[kt * 128:(kt + 1) * 128, :])
        g1 = K.sb([128, 8]); K.dma(g1[:, :], ln1g[:, :])
        b1 = K.sb([128, 8]); K.dma(b1[:, :], ln1b[:, :])
        h32 = K.sb([128, 8, 512], F32)
        hb = K.sb([128, 8, 512], BF16)
        ob = {n: K.sb([128, 4, 512], BF16) for n in oT}
        G = K.sb([128, 512], BF16)
        yb = K.sb([128, 8, 512], BF16)
        y32 = K.sb([128, 512], F32); tB = K.sb([128, 512], F32)
        r32 = K.sb([128, 8, 512], F32)
        tmp = {"sq": K.sb([128, 8, 512], F32), "mean": K.sb([128, 512]), "msq": K.sb([128, 512]), "rstd": K.sb([128, 512])}
        h1 = K.sb([128, 8, 512], F32)
        for ch in range(NCH):
            t0 = ch * 512
            tsl = slice(t0, t0 + 512)
            for ft in range(8):
                K.dma(h32[:, ft, :], hT_in[ft * 128:(ft + 1) * 128, tsl])
            for ft in range(8):
                K.copy(hb[:, ft, :], h32[:, ft, :], q=("act" if ft % 2 else "dve"))
            for n in names:
                for kt in range(4):
                    K.dma(ob[n][:, kt, :], oT[n][kt * 128:(kt + 1) * 128, tsl])
            for mt in range(8):
                for bi, (n, wn) in enumerate(zip(names, wnames)):
                    gp = nps()
                    for kt in range(8):
                        K.mm(gp[:, :], Wmg.k(kt)[:, kt, bi * 1024 + mt * 128: bi * 1024 + (mt + 1) * 128], hb[:, kt, :], start=(kt == 0), stop=(kt == 7))
                    K.act(G[:, :], gp[:, :], AF.Sigmoid)
                    yp = nps()
                    for kt in range(4):
                        K.mm(yp[:, :], Wbr[wn][:, kt, mt * 128:(mt + 1) * 128], ob[n][:, kt, :], start=(kt == 0), stop=(kt == 3))
                    if bi == 0:
                        K.tt(y32[:, :], yp[:, :], G[:, :], ALU.mult)
                    else:
                        K.tt(tB[:, :], yp[:, :], G[:, :], ALU.mult)
                        K.tt(y32[:, :], y32[:, :], tB[:, :], ALU.add, q="pool")
                K.copy(yb[:, mt, :], y32[:, :], q="act")
            for mt in range(8):
                mp = nps()
                for kt in range(8):
                    K.mm(mp[:, :], Wo[:, kt, mt * 128:(mt + 1) * 128], yb[:, kt, :], start=(kt == 0), stop=(kt == 7))
                K.op("dve", "scalar_tensor_tensor", {"out": r32[:, mt, :]}, {"in0": h32[:, mt, :], "in1": mp[:, :]},
                     scalar=ALPHA, op0=ALU.mult, op1=ALU.add)
            ln_fm(K, c, r32, g1, b1, h1, None, psA, psB, tmp)
            for sub in range(4):
                lp = nps()
                for kt in range(8):
                    K.mm(lp[:, 0:16], h1[:, kt, sub * 128:(sub + 1) * 128], rw_s[:, kt, :], start=(kt == 0), stop=(kt == 7))
                K.act(aff[:, :], lp[:, 0:16], AF.Sigmoid)
                K.tt(sel[:, :], aff[:, :], rb_s[:, :], ALU.add)
                sv = sel.t[:, :].rearrange("p (g e) -> p g e", e=4)
                pairs = [(0, 1), (0, 2), (0, 3), (1, 2), (1, 3), (2, 3)]
                for pi_, (a_, b_) in enumerate(pairs):
                    K.tt(rv(pair[:, :, :], pair.t[:, :, pi_]), rv(sel[:, :], sv[:, :, a_]), rv(sel[:, :], sv[:, :, b_]), ALU.add)
                K.op("dve", "tensor_reduce", {"out": gsc[:, :]}, {"in_": pair[:, :, :]}, axis=AX.X, op=ALU.max)
                K.op("dve", "tensor_reduce", {"out": gmx[:, :]}, {"in_": gsc[:, :]}, axis=AX.X, op=ALU.max)
                K.ts(goh[:, :], gsc[:, :], gmx[:, 0:1], ALU.is_ge, 1.0, ALU.subtract)
                K.ts(goh[:, :], goh[:, :], 1.0e4, ALU.mult)
                K.tt(rv(selm[:, :], selm.t[:, :].rearrange("p (g e) -> p g e", e=4)), rv(sel[:, :], sv),
                     rv(goh[:, :], goh.t[:, :].unsqueeze(2).to_broadcast([128, 4, 4])), ALU.add)
                K.op("dve", "max", {"out": m8[:, :]}, {"in_": selm[:, :]})
                K.ts(cho[:, :], selm[:, :], m8[:, 1:2], ALU.is_ge)
                K.tt(cho[:, :], cho[:, :], aff[:, :], ALU.mult)
                K.op("dve", "tensor_reduce", {"out": gsum[:, :]}, {"in_": cho[:, :]}, axis=AX.X, op=ALU.add)
                K.op("dve", "reciprocal", {"out": gsum[:, :]}, {"in_": gsum[:, :]})
                K.ts(cmb_all.k(ch * 4 + sub)[:, ch * 4 + sub, :], cho[:, :], gsum[:, 0:1], ALU.mult)
            for ft in range(8):
                K.dma(h1_scr.k((ch, ft))[ft * 128:(ft + 1) * 128, tsl], h1[:, ft, :], q="poolq")

    with K.scope():
        g2 = K.sb([128, 8]); K.dma(g2[:, :], ln2g[:, :])
        b2 = K.sb([128, 8]); K.dma(b2[:, :], ln2b[:, :])
        h1 = K.sb([128, 8, 512], F32)
        h1b = K.sb([128, 8, 512], BF16)
        h32 = K.sb([128, 8, 512], F32)
        r32 = K.sb([128, 8, 512], F32)
        tmp = {"sq": K.sb([128, 8, 512], F32), "mean": K.sb([128, 512]), "msq": K.sb([128, 512]), "rstd": K.sb([128, 512])}
        comb = K.sb([128, 16, 512], F32)
        hid = K.sb([128, 4, 512], BF16)
        sA = K.sb([128, 512], F32); tB = K.sb([128, 512], F32)
        ffn = K.sb([128, 8, 512], F32)
        W1 = [K.sb([128, 8, 512], BF16) for _ in range(2)]
        W3 = [K.sb([128, 8, 512], BF16) for _ in range(2)]
        W2 = [K.sb([128, 4, D], BF16) for _ in range(2)]
        otile = [K.sb([128, D], F32) for _ in range(2)]
        for ch in range(NCH):
            t0 = ch * 512
            tsl = slice(t0, t0 + 512)
            for ft in range(8):
                K.dma(h1[:, ft, :], h1_scr.k((ch, ft))[ft * 128:(ft + 1) * 128, tsl])
            for ft in range(8):
                K.copy(h1b[:, ft, :], h1[:, ft, :], q=("act" if ft % 2 else "dve"))
            for sub in range(4):
                cmbv = cmb_all.k(ch * 4 + sub)
                for e4 in range(4):
                    bp = nps()
                    for e in range(4):
                        ee = e4 * 4 + e
                        K.mm(bp[:, e * 128:(e + 1) * 128], rv(cmbv[:, ch * 4 + sub, :], cmb_all.t[:, ch * 4 + sub, ee:ee + 1].to_broadcast([128, 128])), c["ident32"][:, :])
                    for e in range(4):
                        ee = e4 * 4 + e
                        K.copy(comb[:, ee, sub * 128:(sub + 1) * 128], bp[:, e * 128:(e + 1) * 128], q=("act" if e % 2 else "dve"))
            for e in range(16):
                W1e, W3e, W2e = W1[e % 2], W3[e % 2], W2[e % 2]
                for kt in range(8):
                    K.dma(W1e.k(kt)[:, kt, :], w1[e * D + kt * 128:e * D + (kt + 1) * 128, :], q="sp")
                    K.dma(W3e.k(kt)[:, kt, :], w3[e * D + kt * 128:e * D + (kt + 1) * 128, :], q="sp")
                for kt in range(4):
                    K.dma(W2e.k(kt)[:, kt, :], w2[e * 512 + kt * 128:e * 512 + (kt + 1) * 128, :], q="sp")
                for ft in range(4):
                    ap_ = nps()
                    for kt in range(8):
                        K.mm(ap_[:, :], W1e.k(kt)[:, kt, ft * 128:(ft + 1) * 128], h1b[:, kt, :], start=(kt == 0), stop=(kt == 7))
                    bp_ = nps()
                    for kt in range(8):
                        K.mm(bp_[:, :], W3e.k(kt)[:, kt, ft * 128:(ft + 1) * 128], h1b[:, kt, :], start=(kt == 0), stop=(kt == 7))
                    K.act(sA[:, :], ap_[:, :], AF.Silu)
                    K.tt(tB[:, :], bp_[:, :], comb[:, e, :], ALU.mult)
                    K.tt(hid[:, ft, :], sA[:, :], tB[:, :], ALU.mult, q="pool")
                for mt in range(8):
                    fp = nps()
                    for kt in range(4):
                        K.mm(fp[:, :], W2e.k(kt)[:, kt, mt * 128:(mt + 1) * 128], hid[:, kt, :], start=(kt == 0), stop=(kt == 3))
                    if e == 0:
                        K.copy(ffn[:, mt, :], fp[:, :], q="act")
                    else:
                        K.tt(ffn[:, mt, :], ffn[:, mt, :], fp[:, :], ALU.add)
            for mt in range(8):
                K.op("dve", "scalar_tensor_tensor", {"out": r32[:, mt, :]}, {"in0": h1[:, mt, :], "in1": ffn[:, mt, :]},
                     scalar=ALPHA, op0=ALU.mult, op1=ALU.add)
            ln_fm(K, c, r32, g2, b2, h32, None, psA, psB, tmp)
            if last_layer:
                for sub in range(4):
                    ot = otile[sub % 2]
                    for ft in range(8):
                        tp = nps()
                        K.tr(tp[:, 0:128], h32[:, ft, sub * 128:(sub + 1) * 128], c["ident32"][:, :])
                        K.copy(ot[:, ft * 128:(ft + 1) * 128], tp[:, 0:128], q=("act" if ft % 2 else "dve"))
                    K.outs.append(K.dma(out.k((ch, sub))[t0 + sub * 128:t0 + (sub + 1) * 128, :], ot[:, :], q="poolq"))
            else:
                for ft in range(8):
                    K.dma(hT_out.k((ch, ft))[ft * 128:(ft + 1) * 128, tsl], h32[:, ft, :], q="poolq")


LAYER_W = [("w_in", [D, 6328]), ("w_uq", [256, 768]), ("w_ukv", [128, 1024]),
           ("w_br_nsa", [512, D]), ("w_br_diff", [512, D]), ("w_br_mla", [512, D]), ("w_out", [D, D]),
           ("moe_w1", [16 * D, 512]), ("moe_w3", [16 * D, 512]), ("moe_w2", [16 * 512, D]),
           ("cmp_w1_k", [2048, 256]), ("cmp_w1_v", [2048, 256]), ("cmp_w2_k", [256, 64]), ("cmp_w2_v", [256, 64])]
LAYER_P = [("qg", [128, 2]), ("kvg", [128, 1]), ("lam_p", [128, 256]), ("subg", [128, 1]), ("posk", [64, 32]), ("posv", [64, 32]),
           ("ln1_g", [128, 8]), ("ln1_b", [128, 8]), ("ln2_g", [128, 8]), ("ln2_b", [128, 8])]
CONSTS = [("cosT", [32, S], F32), ("sinT", [32, S], F32), ("protT", [32, 32], F32), ("cm512", [4, 128, 512], F32),
          ("wlow", [128, 128], F32), ("kaug_tok", [9, S], BF16), ("qaug_diff", [4, 9, S], BF16), ("qaug_nsa", [9, 8, S], BF16),
          ("kaug_cmp", [9, 512], BF16), ("cmask", [2, 16, 128, 128], F32), ("cprev", [128, 128], F32), ("amat", [4, 128, 128], F32),
          ("addmask", [S // 128, 128, 128], F32), ("E", [128, S], BF16), ("oh", [12, 768], BF16),
          ("router_w", [128, 8, 16], F32), ("router_b", [128, 16], F32), ("ln_g", [128, 8], F32), ("ln_b", [128, 8], F32)]
SCR_FM = [("nqT", 512), ("kcT", 128), ("vcT", 128), ("ksT", 128), ("kwT", 128), ("ngT", 24), ("dqT", 512), ("dkT", 512),
          ("qmT", 768), ("kropeT", 32), ("knopeT", 512), ("o_nsaT", 512), ("o_diffT", 512), ("o_mlaT", 512)]
SCR_TM = [("vs", 128), ("vw", 128), ("dv", 512), ("vm", 512)]


def conv_w(K, src, dst, rows, cols, bufs):
    a = rows // 128
    sv = src.t[:, :].rearrange("(p a) c -> p (a c)", p=128)
    dv = dst.t[:, :].rearrange("(p a) c -> p (a c)", p=128)
    n = a * cols
    CH = 4096
    for i, c0 in enumerate(range(0, n, CH)):
        w = min(CH, n - c0)
        fa, fb = bufs[0][bufs[2][0] % 3], bufs[1][bufs[2][0] % 3]
        bufs[2][0] += 1
        K.dma(fa[:, :w], rv(src.k(i)[:, :], sv[:, c0:c0 + w]), q="sp")
        if i % 2 == 0:
            K.copy(fb[:, :w], fa[:, :w], q="dve")
        else:
            K.copy(fb[:, :w], fa[:, :w], q="act")
        K.dma(rv(dst.k(i)[:, :], dv[:, c0:c0 + w]), fb[:, :w], q="poolq")


def build_fused(debug=False, nlayers=2, stages="wABC"):
    K = KB()
    c = consts(K)
    x = K.inp("x", [S, D], F32)
    out = K.out("out", [S, D], F32)
    cst = {n: K.inp(n, sh, dt) for n, sh, dt in CONSTS}
    win = [{n: K.inp("%s_%d" % (n, l), sh, F32) for n, sh in LAYER_W} for l in range(nlayers)]
    prm = [{n: K.inp("%s_%d" % (n, l), sh, F32) for n, sh in LAYER_P} for l in range(nlayers)]
    wsc = {n: K.dram("b_" + n, sh, BF16, "Internal") for n, sh in LAYER_W}
    scr = {n: K.dram("s_" + n, [r, S], BF16, "Internal") for n, r in SCR_FM}
    scr.update({n: K.dram("s_" + n, [S, w], BF16, "Internal") for n, w in SCR_TM})
    hT = [K.dram("s_hT32_%d" % i, [D, S], F32, "Internal") for i in range(2)]
    dbg = {}
    if debug:
        dbg["hT32_dbg"] = K.out("hT32_dbg", [D, S], F32)
    for l in range(nlayers):
        with K.scope():
          if "w" in stages:
            fa = [K.sb([128, 4096], F32) for _ in range(3)]
            fb = [K.sb([128, 4096], BF16) for _ in range(3)]
            cnt = [0]
            for n, sh in LAYER_W:
                conv_w(K, win[l][n], wsc[n], sh[0], sh[1], (fa, fb, cnt))
        io = dict(cst)
        io.update(prm[l])
        io.update(wsc)
        io.update(scr)
        io["x"] = x
        io["out"] = out
        io["hT32"] = hT[l % 2]
        io["hT32_next"] = hT[(l + 1) % 2]
        with K.scope():
          if "A" in stages:
            build_A(K, c, l == 0, io)
        lam_init = 0.8 - 0.6 * math.exp(-0.3 * l)
        with K.scope():
          if "B" in stages:
            build_B(K, c, lam_init, io, do=tuple(x for x, f in (("diff", "d"), ("mla", "m"), ("nsa", "n")) if f in stages) if any(f in stages for f in "dmn") else ("diff", "mla", "nsa"))
        with K.scope():
          if "C" in stages:
            build_C(K, c, (l == nlayers - 1) and not debug, io, l)
    if debug:
        with K.scope():
            t = K.sb([128, 4096], F32)
            src = hT[nlayers % 2]
            for ft in range(8):
                for hh in range(2):
                    K.dma(t[:, :], src[ft * 128:(ft + 1) * 128, hh * 4096:(hh + 1) * 4096])
                    K.outs.append(K.dma(dbg["hT32_dbg"].k((ft, hh))[ft * 128:(ft + 1) * 128, hh * 4096:(hh + 1) * 4096], t[:, :]))
    return K.finish()


def split3(v):
    v = np.asarray(v, np.float32)
    a = v.astype(NPBF); r = v - a.astype(np.float32)
    b = r.astype(NPBF); r2 = r - b.astype(np.float32)
    c = r2.astype(NPBF)
    return [a, b, c]

def slopes_all():
    n = 12
    return (2.0 ** (-8.0 * np.arange(1, n + 1, dtype=np.float32) / n)).astype(np.float32)

def qaug(slope, scale, n=S):
    t = np.arange(n, dtype=np.float32)
    rows = split3(-(np.float32(slope) * t) / np.float32(scale))
    s3 = split3(np.full(n, np.float32(slope) / np.float32(scale), np.float32))
    return np.stack(rows + s3 + s3, 0)

def kaug(pos):
    pos = np.asarray(pos)
    one = np.ones(len(pos), np.float32).astype(NPBF)
    a = (64 * (pos // 64)).astype(np.float32).astype(NPBF)
    b = (pos % 64).astype(np.float32).astype(NPBF)
    return np.stack([one] * 3 + [a] * 3 + [b] * 3, 0)

def perm_uq():
    idx = []
    for h in range(8):
        idx += list(range(96 * h + 64, 96 * h + 96)) + list(range(96 * h, 96 * h + 64))
    return np.array(idx)

def perm_ukv():
    idx = []
    for h in range(8):
        idx += list(range(128 * h, 128 * h + 64))
    for h in range(8):
        idx += list(range(128 * h + 64, 128 * h + 128))
    return np.array(idx)

def pvec(v, nft):
    return np.ascontiguousarray(np.asarray(v, np.float32).reshape(nft, 128).T)

def const_inputs(inp):
    m = {}
    half = 16
    freqs = (10000.0 ** (-np.arange(half, dtype=np.float32) / half)).astype(np.float32)
    ang = np.arange(S, dtype=np.float32)[:, None] * freqs[None, :]
    cos = np.cos(ang).astype(np.float32); sin = np.sin(ang).astype(np.float32)
    m["cosT"] = np.ascontiguousarray(np.concatenate([cos.T, cos.T], 0))
    m["sinT"] = np.ascontiguousarray(np.concatenate([sin.T, sin.T], 0))
    P = np.zeros((32, 32), np.float32)
    for i in range(16):
        P[i, i + 16] = -1.0
        P[i + 16, i] = 1.0
    m["protT"] = np.ascontiguousarray(P.T)
    k = np.arange(128)[:, None]; q = np.arange(512)[None, :]
    m["cm512"] = np.stack([np.where(128 * j + k <= q, 0.0, -1e9).astype(np.float32) for j in range(4)], 0)
    q1 = np.arange(128)[None, :]
    m["wlow"] = np.where(k > q1, 0.0, -1e9).astype(np.float32)
    m["kaug_tok"] = kaug(np.arange(S))
    sl = slopes_all()
    m["qaug_diff"] = np.stack([qaug(sl[8 + h], 0.125) for h in range(4)], 0)
    m["qaug_nsa"] = np.ascontiguousarray(np.stack([qaug(sl[h], 0.125) for h in range(8)], 1))
    m["kaug_cmp"] = kaug(np.arange(512) * 16 + 31)
    cm = np.zeros((2, 16, 128, 128), np.float32)
    cc = np.arange(128)[:, None]; qq = np.arange(128)[None, :]
    for r in range(16):
        vis = (16 * cc + 31 <= 128 * r + qq)
        cm[0, r] = np.where(vis, 0.0, -1e9)
        cm[1, r] = np.where(vis & (cc < 127), 0.0, -1e9)
    m["cmask"] = cm
    cp = np.zeros((128, 128), np.float32); cp[127, :15] = -1e9
    m["cprev"] = cp
    Am = np.zeros((512, 128), np.float32)
    for cidx in range(511):
        for i in (cidx, cidx + 1):
            Am[cidx, i // 4] += 1.0
    m["amat"] = np.ascontiguousarray(Am.reshape(4, 128, 128))
    adm = np.zeros((S // 128, 128, 128), np.float32)
    jj = np.arange(128)[None, :]
    for qb in range(S // 128):
        t = qb * 128 + np.arange(128)[:, None]
        cur = t // 64
        forced = (jj == 0) | (jj == cur) | (jj == cur - 1)
        started = jj * 64 <= t
        adm[qb] = np.where(started, np.where(forced, 1e4, 0.0), -1e4)
    m["addmask"] = adm
    m["E"] = np.ascontiguousarray((np.arange(S)[None, :] // 64 == np.arange(128)[:, None]).astype(np.float32).astype(NPBF))
    oh = np.zeros((12, 12, 64), np.float32)
    for j in range(12):
        oh[j, j, :] = 1.0
    m["oh"] = np.ascontiguousarray(oh.reshape(12, 768).astype(NPBF))
    m["router_w"] = np.ascontiguousarray(inp["router_w"].reshape(8, 128, 16).transpose(1, 0, 2))
    m["router_b"] = np.ascontiguousarray(np.broadcast_to(inp["router_b"].reshape(1, 16), (128, 16)))
    m["ln_g"] = pvec(inp["ln_in_g"], 8); m["ln_b"] = pvec(inp["ln_in_b"], 8)
    return m

def layer_inputs(inp, l):
    m = {}
    m["w_in_%d" % l] = np.ascontiguousarray(inp["w_in"][l])
    m["w_uq_%d" % l] = np.ascontiguousarray(inp["mla_w_uq"][l][:, perm_uq()])
    m["w_ukv_%d" % l] = np.ascontiguousarray(inp["mla_w_ukv"][l][:, perm_ukv()])
    for n in ("w_br_nsa", "w_br_diff", "w_br_mla", "w_out"):
        m["%s_%d" % (n, l)] = np.ascontiguousarray(inp[n][l])
    m["moe_w1_%d" % l] = np.ascontiguousarray(inp["moe_w1"][l].reshape(16 * D, 512))
    m["moe_w3_%d" % l] = np.ascontiguousarray(inp["moe_w3"][l].reshape(16 * D, 512))
    m["moe_w2_%d" % l] = np.ascontiguousarray(inp["moe_w2"][l].reshape(16 * 512, D))
    for n in ("cmp_w1_k", "cmp_w1_v", "cmp_w2_k", "cmp_w2_v"):
        m["%s_%d" % (n, l)] = np.ascontiguousarray(inp[n][l])
    m["qg_%d" % l] = pvec(inp["mla_q_norm_g"][l], 2)
    m["kvg_%d" % l] = pvec(inp["mla_kv_norm_g"][l], 1)
    m["lam_p_%d" % l] = np.ascontiguousarray(np.broadcast_to(inp["diff_lambda"][l].reshape(1, 256), (128, 256)))
    m["subg_%d" % l] = np.ascontiguousarray(inp["diff_subln_g"][l].reshape(128, 1))
    m["posk_%d" % l] = np.ascontiguousarray(inp["cmp_pos_k"][l].T)
    m["posv_%d" % l] = np.ascontiguousarray(inp["cmp_pos_v"][l].T)
    for n in ("ln1_g", "ln1_b", "ln2_g", "ln2_b"):
        m["%s_%d" % (n, l)] = pvec(inp[n][l], 8)
    return m

def all_inputs(inp, nlayers, ncores):
    base = const_inputs(inp)
    for l in range(nlayers):
        base.update(layer_inputs(inp, l))
    maps = []
    for b in range(ncores):
        m = dict(base)
        m["x"] = np.ascontiguousarray(inp["x"][b])
        maps.append(m)
    return maps


def kernel(**inputs):
    inp = {k: np.asarray(v, np.float32) for k, v in inputs.items()}
    nc = build_fused(debug=False, nlayers=2)
    maps = all_inputs(inp, 2, 4)
    res = run_bass_kernel_spmd(nc, maps, core_ids=[0, 1, 2, 3]).results
    out = np.stack([np.asarray(res[b]["out"], np.float32) for b in range(4)], 0)
    return out
```

```python
import numpy as np
from contextlib import ExitStack
import concourse.bass as bass
import concourse.mybir as mybir

F32 = mybir.dt.float32
BF16 = mybir.dt.bfloat16
AF = mybir.ActivationFunctionType
ALU = mybir.AluOpType
AX = mybir.AxisListType

COMPUTE = ("pe", "act", "dve", "pool")
DMAQ = ("sp", "poolq", "actq")
NSLOT = 32
NSLOT_HW = 18


class Buf:
    __slots__ = ("w", "r")

    def __init__(self):
        self.w = None
        self.r = []


class Op:
    __slots__ = ("q", "fn", "deps", "inc", "sem", "val", "is_dma", "slot", "incv")

    def __init__(self, q, fn, is_dma):
        self.q = q
        self.fn = fn
        self.deps = []
        self.inc = False
        self.sem = None
        self.val = 0
        self.is_dma = is_dma
        self.slot = -1
        self.incv = 16


class V:
    __slots__ = ("ap", "bufs")

    def __init__(self, ap, bufs):
        self.ap = ap
        self.bufs = bufs


class Tl:
    def __init__(self, t):
        self.t = t
        self._b = {}

    def buf(self, key=None):
        b = self._b.get(key)
        if b is None:
            b = self._b[key] = Buf()
        return b

    def __getitem__(self, idx):
        return V(self.t[idx], [self.buf(None)])

    def k(self, key):
        return _Keyed(self, key)

    def ks(self, keys, idx):
        return V(self.t[idx], [self.buf(k) for k in keys])


class _Keyed:
    def __init__(self, tl, key):
        self.tl = tl
        self.key = key

    def __getitem__(self, idx):
        return V(self.tl.t[idx], [self.tl.buf(self.key)])


def rv(v, ap):
    return V(ap, v.bufs)


class KB:
    def __init__(self):
        self.nc = bass.Bass("TRN2", target_bir_lowering=False, num_devices=8)
        self.es = ExitStack()
        self.ops = {q: [] for q in ("pe", "act", "dve", "pool", "sp")}
        self.allops = []
        self.ndma = 0
        self.ndma_sw = 0
        self.slot_last = [None] * NSLOT
        self.n = 0
        self.outs = []

    def sb(self, shape, dt=F32, name=None):
        self.n += 1
        return Tl(self.es.enter_context(self.nc.sbuf_tensor(name or "sb%d" % self.n, list(shape), dt)))

    def ps(self, shape, dt=F32, name=None):
        self.n += 1
        return Tl(self.es.enter_context(self.nc.psum_tensor(name or "ps%d" % self.n, list(shape), dt)))

    def dram(self, name, shape, dt, kind):
        return Tl(self.nc.dram_tensor(name, list(shape), dt, kind=kind).ap())

    def inp(self, name, shape, dt):
        return self.dram(name, shape, dt, "ExternalInput")

    def out(self, name, shape, dt):
        return self.dram(name, shape, dt, "ExternalOutput")

    def rec(self, q, fn, reads, writes):
        is_dma = q in DMAQ
        o = Op(q, fn, is_dma)
        deps = set()
        for b in reads:
            if b.w is not None:
                deps.add(b.w)
        for b in writes:
            if b.w is not None:
                deps.add(b.w)
            for r in b.r:
                deps.add(r)
        if is_dma:
            if q == "poolq":
                o.slot = NSLOT_HW + (self.ndma_sw % (NSLOT - NSLOT_HW))
                self.ndma_sw += 1
            else:
                o.slot = self.ndma % NSLOT_HW
                self.ndma += 1
            prev = self.slot_last[o.slot]
            if prev is not None:
                deps.add(prev)
            self.slot_last[o.slot] = o
            o.inc = True
        for d in deps:
            if d is o:
                continue
            if (not d.is_dma) and (not is_dma) and d.q == "pe" and q == "pe":
                continue
            d.inc = True
            o.deps.append(d)
        for b in reads:
            b.r.append(o)
        for b in writes:
            b.w = o
            b.r = []
        st = {"poolq": "pool", "actq": "act"}.get(q, q)
        self.ops[st].append(o)
        self.allops.append(o)
        return o

    def op(self, q, meth, outs, ins, *args, **kw):
        reads = [b for v in ins.values() for b in v.bufs]
        writes = [b for v in outs.values() for b in v.bufs]
        kwargs = dict(kw)
        for k, v in outs.items():
            kwargs[k] = v.ap
        for k, v in ins.items():
            kwargs[k] = v.ap

        def fn(e):
            return getattr(e, meth)(*args, **kwargs)
        return self.rec(q, fn, reads, writes)

    def dma(self, out, in_, q="sp"):
        o = self.op(q, "dma_start", {"out": out}, {"in_": in_})
        return o

    def mm(self, out, lhsT, rhs, start=True, stop=True):
        ins = {"lhsT": lhsT, "rhs": rhs}
        reads = [b for v in ins.values() for b in v.bufs]
        writes = list(out.bufs)
        oa, la, ra = out.ap, lhsT.ap, rhs.ap

        def fn(e):
            return e.matmul(oa, la, ra, start=start, stop=stop)
        return self.rec("pe", fn, reads + (writes if not start else []), writes)

    def tr(self, out, in_, ident):
        oa, ia, da = out.ap, in_.ap, ident.ap

        def fn(e):
            return e.transpose(oa, ia, da)
        return self.rec("pe", fn, in_.bufs + ident.bufs, out.bufs)

    def act(self, out, in_, func, bias=None, scale=None, q="act"):
        ins = {"in_": in_}
        kw = {}
        if isinstance(bias, V):
            ins["bias"] = bias
        elif bias is not None:
            kw["bias"] = bias
        if isinstance(scale, V):
            ins["scale"] = scale
        elif scale is not None:
            kw["scale"] = scale
        return self.op(q, "activation", {"out": out}, ins, func=func, **kw)

    def tt(self, out, in0, in1, op, q="dve"):
        return self.op(q, "tensor_tensor", {"out": out}, {"in0": in0, "in1": in1}, op=op)

    def ts(self, out, in0, s1, op0, s2=None, op1=None, q="dve"):
        ins = {"in0": in0}
        kw = {}
        if isinstance(s1, V):
            ins["scalar1"] = s1
        else:
            kw["scalar1"] = s1
        if isinstance(s2, V):
            ins["scalar2"] = s2
        else:
            kw["scalar2"] = s2
        if op1 is not None:
            kw["op1"] = op1
        return self.op(q, "tensor_scalar", {"out": out}, ins, op0=op0, **kw)

    def copy(self, out, in_, q="dve"):
        if q == "act":
            return self.op("act", "copy", {"out": out}, {"in_": in_})
        return self.op(q, "tensor_copy", {"out": out}, {"in_": in_})

    def memset(self, out, val, q="pool"):
        return self.op(q, "memset", {}, {}, out.ap, val) if False else self._memset(out, val, q)

    def _memset(self, out, val, q):
        oa = out.ap

        def fn(e):
            return e.memset(oa, val)
        return self.rec(q, fn, [], out.bufs)

    def cc(self, kind, rg, in_, out, incv=1):
        ia, oa = in_.ap.opt(), out.ap.opt()

        def fn(e):
            return e.collective_compute(kind, ALU.bypass, replica_groups=rg, ins=[ia], outs=[oa])
        o = self.rec("poolq", fn, in_.bufs, out.bufs)
        o.incv = incv
        return o

    def barrier(self):
        lasts = [self.ops[st][-1] for st in self.ops if self.ops[st]] + [o for o in self.slot_last if o is not None]
        for st in ("pe", "act", "dve", "pool", "sp"):
            o = Op(st, (lambda e: e.nop()), False)
            for d in lasts:
                d.inc = True
                o.deps.append(d)
            self.ops[st].append(o)
            self.allops.append(o)

    class _Scope:
        def __init__(self, kb):
            self.kb = kb

        def __enter__(self):
            self.old = self.kb.es
            self.kb.es = ExitStack()
            return self

        def __exit__(self, *a):
            self.kb.barrier()
            self.kb.es.close()
            self.kb.es = self.old

    def scope(self):
        return KB._Scope(self)

    def finish(self):
        nc = self.nc
        final = [o for o in self.outs]
        with ExitStack() as es:
            esem = {q: es.enter_context(nc.semaphore("s_" + q)) for q in COMPUTE + ("sp",)}
            dsem = [es.enter_context(nc.semaphore("d_%d" % i)) for i in range(NSLOT)]
            cnt = {q: 0 for q in COMPUTE + ("sp",)}
            dcnt = [0] * NSLOT
            for o in self.allops:
                if not o.inc:
                    continue
                if o.is_dma:
                    dcnt[o.slot] += o.incv
                    o.sem = dsem[o.slot]
                    o.val = dcnt[o.slot]
                else:
                    cnt[o.q] += 1
                    o.sem = esem[o.q]
                    o.val = cnt[o.q]
            block = es.enter_context(nc.Block())
            streams = self.ops

            def run(st, eng):
                waited = {}
                for o in streams[st]:
                    for d in o.deps:
                        key = id(d.sem)
                        if waited.get(key, 0) >= d.val:
                            continue
                        eng.wait_ge(d.sem, d.val)
                        waited[key] = d.val
                    ins = o.fn(eng)
                    if o.inc:
                        ins.then_inc(o.sem, o.incv if o.is_dma else 1)
                if st == "sp":
                    for o in final:
                        if waited.get(id(o.sem), 0) < o.val:
                            eng.wait_ge(o.sem, o.val)
                            waited[id(o.sem)] = o.val

            block.tensor(lambda e: run("pe", e))
            block.scalar(lambda e: run("act", e))
            block.vector(lambda e: run("dve", e))
            block.gpsimd(lambda e: run("pool", e))
            block.sync(lambda e: run("sp", e))
        self.es.close()
        return nc

import math
import numpy as np
import ml_dtypes
from concourse.bass_utils import run_bass_kernel_spmd

NPBF = ml_dtypes.bfloat16
D = 1024
T = 8192
S = 8192
NCH = T // 512
EPS_LN = 1e-5
EPS_RMS = 1e-6
ALPHA = 4 ** 0.25
OFF = {}
_o = 0
for _n, _w in [("nq", 512), ("kc", 128), ("vc", 128), ("ks", 128), ("vs", 128), ("kw", 128), ("vw", 128), ("ng", 24),
               ("dq", 512), ("dk", 512), ("dv", 512), ("cq", 256), ("ckv", 128), ("kr", 32), ("mg", 3072)]:
    OFF[_n] = (_o, _w)
    _o += _w
NA = OFF["mg"][0]


def run(nc, in_maps):
    res = run_bass_kernel_spmd(nc, in_maps, core_ids=list(range(8)))
    return res.results


def consts(K):
    c = {}
    c["ones32"] = K.sb([128, 128], F32)
    K.memset(c["ones32"][:, :], 1.0)
    c["onesb"] = K.sb([128, 128], BF16)
    K.memset(c["onesb"][:, :], 1.0)
    ident = K.sb([128, 128], F32)
    K.memset(ident[:, :], 0.0)
    K.op("pool", "affine_select", {"out": ident[:, :]}, {"in_": ident[:, :]}, pattern=[[-1, 128]],
         compare_op=ALU.not_equal, fill=1.0, base=0, channel_multiplier=1)
    c["ident32"] = ident
    return c


def ln_fm(K, c, src, g, b, out32, outb, psA, psB, tmp, N=512, eps=EPS_LN, nft=8, pre=None):
    sq = tmp["sq"]
    Dn = nft * 128
    for ft in range(nft):
        K.mm(psA[:, :N], c["ones32"][:, :], src[:, ft, :], start=(ft == 0), stop=(ft == nft - 1))
    for ft in range(nft):
        K.act(sq[:, ft, :], src[:, ft, :], AF.Square)
    for ft in range(nft):
        K.mm(psB[:, :N], c["ones32"][:, :], sq[:, ft, :], start=(ft == 0), stop=(ft == nft - 1))
    mean, msq, rstd = tmp["mean"], tmp["msq"], tmp["rstd"]
    K.ts(mean[:, :], psA[:, :N], 1.0 / Dn, ALU.mult)
    K.tt(msq[:, :], mean[:, :], mean[:, :], ALU.mult)
    K.op("dve", "scalar_tensor_tensor", {"out": rstd[:, :]}, {"in0": psB[:, :N], "in1": msq[:, :]},
         scalar=1.0 / Dn, op0=ALU.mult, op1=ALU.subtract)
    K.ts(rstd[:, :], rstd[:, :], eps, ALU.add)
    K.act(rstd[:, :], rstd[:, :], AF.Sqrt)
    K.op("dve", "reciprocal", {"out": rstd[:, :]}, {"in_": rstd[:, :]})
    for ft in range(nft):
        t = sq
        K.tt(t[:, ft, :], src[:, ft, :], mean[:, :], ALU.subtract)
        K.tt(t[:, ft, :], t[:, ft, :], rstd[:, :], ALU.mult, q="pool")
        K.act(out32[:, ft, :], t[:, ft, :], AF.Identity, bias=b[:, ft:ft + 1], scale=g[:, ft:ft + 1])
        if outb is not None:
            K.copy(outb[:, ft, :], out32[:, ft, :], q="act")


def rms_fm(K, c, src, g, outb, psA, tmp, nft, N=512):
    sq = tmp["sq"]
    Dn = nft * 128
    for ft in range(nft):
        K.act(sq[:, ft, :], src[:, ft, :], AF.Square)
    for ft in range(nft):
        K.mm(psA[:, :N], c["ones32"][:, :], sq[:, ft, :], start=(ft == 0), stop=(ft == nft - 1))
    rstd = tmp["rstd"]
    K.ts(rstd[:, :], psA[:, :N], 1.0 / Dn, ALU.mult, EPS_RMS, ALU.add)
    K.act(rstd[:, :], rstd[:, :], AF.Sqrt)
    K.op("dve", "reciprocal", {"out": rstd[:, :]}, {"in_": rstd[:, :]})
    for ft in range(nft):
        K.tt(sq[:, ft, :], src[:, ft, :], rstd[:, :], ALU.mult)
        K.act(outb[:, ft, :], sq[:, ft, :], AF.Copy if False else AF.Identity, scale=g[:, ft:ft + 1])


def build_A(K, c, layer0, io):
    if layer0:
        x = io["x"]; lng = io["ln_g"]; lnb = io["ln_b"]
        hT_out = io["hT32"]
    else:
        hT_in = io["hT32"]
    w_in = io["w_in"]; w_uq = io["w_uq"]; w_ukv = io["w_ukv"]
    qg = io["qg"]; kvg = io["kvg"]; cosT = io["cosT"]; sinT = io["sinT"]; protT = io["protT"]
    fm_outs = {n: io[n] for n in ("nqT", "kcT", "vcT", "ksT", "kwT", "ngT", "dqT", "dkT", "qmT", "kropeT", "knopeT")}
    tm_outs = {n: io[n] for n in ("vs", "vw", "dv", "vm")}

    W = K.sb([128, 8, NA], BF16)
    for kt in range(8):
        K.dma(W.k(kt)[:, kt, :], w_in[kt * 128:(kt + 1) * 128, 0:NA], q="sp")
    Wuq = K.sb([128, 2, 768], BF16)
    for kt in range(2):
        K.dma(Wuq[:, kt, :], w_uq[kt * 128:(kt + 1) * 128, :])
    Wukv = K.sb([128, 1024], BF16)
    K.dma(Wukv[:, :], w_ukv[:, :])
    qg_s = K.sb([128, 2]); K.dma(qg_s[:, :], qg[:, :])
    kvg_s = K.sb([128, 1]); K.dma(kvg_s[:, :], kvg[:, :])
    prot_s = K.sb([32, 32]); K.dma(prot_s[:, :], protT[:, :])
    if layer0:
        g_s = K.sb([128, 8]); K.dma(g_s[:, :], lng[:, :])
        b_s = K.sb([128, 8]); K.dma(b_s[:, :], lnb[:, :])

    def Wv(kt, c0, n):
        return W.k(kt)[:, kt, c0:c0 + n]

    ps = [K.ps([128, 512]) for _ in range(8)]
    pi = [0]

    def nps():
        pi[0] = (pi[0] + 1) % 6
        return ps[pi[0]]
    psA, psB = ps[6], ps[7]

    xT = K.sb([128, 8, 512], F32)
    tmp = {"sq": K.sb([128, 8, 512], F32), "mean": K.sb([128, 512]), "msq": K.sb([128, 512]), "rstd": K.sb([128, 512])}
    h32 = K.sb([128, 8, 512], F32)
    hb = K.sb([128, 8, 512], BF16)
    xc = [K.sb([128, 4, D], F32) for _ in range(2)] if layer0 else None
    stg = [K.sb([128, 512], BF16) for _ in range(4)]
    si = [0]

    def nstg():
        si[0] = (si[0] + 1) % 4
        return stg[si[0]]
    cq32 = K.sb([128, 2, 512], F32)
    cqb = K.sb([128, 2, 512], BF16)
    ckv32 = K.sb([128, 1, 512], F32)
    ckvb = K.sb([128, 1, 512], BF16)
    cos_s = K.sb([32, 512]); sin_s = K.sb([32, 512])
    xs32 = K.sb([96, 512], F32)
    r1 = K.sb([32, 512], F32); r2 = K.sb([32, 512], F32)
    qstg = [K.sb([96, 512], BF16) for _ in range(2)]

    evq = [0]

    def evac(out, in_, func=None):
        evq[0] += 1
        if func is not None:
            K.act(out, in_, func)
        elif evq[0] % 2:
            K.copy(out, in_, q="act")
        else:
            K.copy(out, in_, q="dve")

    for ch in range(NCH):
        t0 = ch * 512
        tsl = slice(t0, t0 + 512)
        if layer0:
            xcc = xc[ch % 2]
            for sub in range(4):
                K.dma(xcc.k(sub)[:, sub, :], x[t0 + sub * 128:t0 + (sub + 1) * 128, :], q="sp")
            for ft in range(8):
                p = nps()
                for sub in range(4):
                    K.tr(p[:, sub * 128:(sub + 1) * 128], xcc.k(sub)[:, sub, ft * 128:(ft + 1) * 128], c["ident32"][:, :])
                evac(xT[:, ft, :], p[:, :])
            ln_fm(K, c, xT, g_s, b_s, h32, hb, psA, psB, tmp)
            for ft in range(8):
                (K.dma(hT_out.k((ch, ft))[ft * 128:(ft + 1) * 128, tsl], h32[:, ft, :], q="poolq"))
        else:
            for ft in range(8):
                K.dma(h32[:, ft, :], hT_in[ft * 128:(ft + 1) * 128, tsl], q="sp")
            for ft in range(8):
                K.copy(hb[:, ft, :], h32[:, ft, :], q=("act" if ft % 2 else "dve"))
        K.dma(cos_s[:, :], cosT[:, tsl]); K.dma(sin_s[:, :], sinT[:, tsl])

        def proj_fm(c0, M):
            p = nps()
            for kt in range(8):
                K.mm(p[:M, :], Wv(kt, c0, M), hb[:, kt, :], start=(kt == 0), stop=(kt == 7))
            return p

        def fm_to(name, r0, M, p, func=None):
            s_ = nstg()
            evac(s_[:M, :], p[:M, :], func)
            (K.dma(fm_outs[name].k((ch, r0))[r0:r0 + M, tsl], s_[:M, :], q="poolq"))

        for name in ("nq", "kc", "vc", "ks", "kw", "dq", "dk"):
            c0, w = OFF[name]
            for m0 in range(0, w, 128):
                fm_to(name + "T", m0, 128, proj_fm(c0 + m0, 128))
        fm_to("ngT", 0, 24, proj_fm(OFF["ng"][0], 24), AF.Sigmoid)
        for name in ("vs", "vw", "dv"):
            c0, w = OFF[name]
            for sub in range(4):
                p = nps()
                for kt in range(8):
                    K.mm(p[:, :w], hb[:, kt, sub * 128:(sub + 1) * 128], Wv(kt, c0, w), start=(kt == 0), stop=(kt == 7))
                s_ = nstg()
                evac(s_[:, :w], p[:, :w])
                (K.dma(tm_outs[name].k((ch, sub))[t0 + sub * 128:t0 + (sub + 1) * 128, :], s_[:, :w], q="poolq"))
        for m in range(2):
            p = proj_fm(OFF["cq"][0] + m * 128, 128)
            evac(cq32[:, m, :], p[:, :])
        rms_fm(K, c, cq32, qg_s, cqb, psA, tmp, 2)
        for h in range(8):
            p = nps()
            for kt in range(2):
                K.mm(p[:96, :], Wuq[:, kt, h * 96:(h + 1) * 96], cqb[:, kt, :], start=(kt == 0), stop=(kt == 1))
            K.copy(xs32[:, :], p[:96, :], q="act")
            p2 = nps()
            K.mm(p2[:32, :], prot_s[:, :], xs32[0:32, :])
            K.tt(r1[:, :], xs32[0:32, :], cos_s[:, :], ALU.mult, q="pool")
            K.tt(r2[:, :], p2[:32, :], sin_s[:, :], ALU.mult)
            qs = qstg[h % 2]
            K.tt(qs[0:32, :], r1[:, :], r2[:, :], ALU.add)
            K.copy(qs[32:64, :], xs32[32:64, :], q="pool")
            K.copy(qs[64:96, :], xs32[64:96, :], q="pool")
            (K.dma(fm_outs["qmT"].k((ch, h))[h * 96:(h + 1) * 96, tsl], qs[:, :], q="poolq"))
        p = proj_fm(OFF["ckv"][0], 128)
        evac(ckv32[:, 0, :], p[:, :])
        rms_fm(K, c, ckv32, kvg_s, ckvb, psA, tmp, 1)
        for m in range(4):
            p = nps()
            K.mm(p[:, :], Wukv[:, m * 128:(m + 1) * 128], ckvb[:, 0, :])
            fm_to("knopeT", m * 128, 128, p)
        for sub in range(4):
            p = nps()
            K.mm(p[:, :], ckvb[:, 0, sub * 128:(sub + 1) * 128], Wukv[:, 512:1024])
            s_ = nstg()
            evac(s_[:, :], p[:, :])
            (K.dma(tm_outs["vm"].k((ch, sub))[t0 + sub * 128:t0 + (sub + 1) * 128, :], s_[:, :], q="poolq"))
        p = proj_fm(OFF["kr"][0], 32)
        K.copy(xs32[0:32, :], p[:32, :], q="act")
        p2 = nps()
        K.mm(p2[:32, :], prot_s[:, :], xs32[0:32, :])
        K.tt(r1[:, :], xs32[0:32, :], cos_s[:, :], ALU.mult, q="pool")
        K.tt(r2[:, :], p2[:32, :], sin_s[:, :], ALU.mult)
        s_ = nstg()
        K.tt(s_[0:32, :], r1[:, :], r2[:, :], ALU.add)
        (K.dma(fm_outs["kropeT"].k(ch)[:, tsl], s_[0:32, :], q="poolq"))


NEG = -1.0e9


def load_tm(K, Vt, src, c0, dv, nper=4):
    n = S // 128
    for i in range(0, n, nper):
        K.dma(Vt[:, i:i + nper, 0:dv], rv(src[:, :], src.t[i * 128:(i + nper) * 128, c0:c0 + dv].rearrange("(n p) d -> p n d", p=128)))


class AttnCtx:
    def __init__(self, K, c):
        self.K, self.c = K, c
        self.NS = 3
        self.NP = 4
        self.S_ps = [K.ps([128, 512]) for _ in range(self.NS)]
        self.O_ps = [K.ps([128, 512]) for _ in range(3)]
        self.Sum_ps = [K.ps([128, 512])]
        self.misc_ps = K.ps([128, 512])
        self.si = 0
        self.oi = 0
        self.P = [K.sb([128, 512], BF16) for _ in range(self.NP)]
        self.Sm = [K.sb([128, 512], F32) for _ in range(3)]
        self.pi = 0
        self.mi = 0
        self.R = [K.sb([128, 512], F32) for _ in range(3)]
        self.pending = []
        self.depth = 2
        self.deferred = []

    def nextS(self):
        self.si = (self.si + 1) % self.NS
        return self.S_ps[self.si]

    def nextO(self):
        self.oi = (self.oi + 1) % 3
        j = -1
        for idx, d in enumerate(self.deferred):
            if self.oi in d[3]:
                j = idx
        if j >= 0:
            self.flush()
        for _ in range(j + 1):
            self.deferred.pop(0)[0]()
        return self.O_ps[self.oi], self.Sum_ps[0], self.R[self.oi]

    def defer(self, fn, thresh, banks, tag=None):
        self.deferred.append([fn, 0, thresh, banks, tag])

    def force_tag(self, tag):
        j = -1
        for idx, d in enumerate(self.deferred):
            if d[4] == tag:
                j = idx
        if j >= 0:
            self.flush()
        for _ in range(j + 1):
            self.deferred.pop(0)[0]()

    def tick(self):
        for d in self.deferred:
            d[1] += 1
        while self.deferred and self.deferred[0][1] >= self.deferred[0][2]:
            self.deferred.pop(0)[0]()

    def drain(self):
        self.flush()
        while self.deferred:
            self.deferred.pop(0)[0]()

    def step(self, qk_list, N, scale, mask, v_lhsT, dv, O, Sum, M, first, last, extra=None, ones=None, Pt=None, mg=1, merged=False):
        K = self.K
        Sp = self.nextS()
        for (l, r, c0, n) in qk_list:
            K.mm(Sp[:, c0:c0 + n], l, r, start=True, stop=(extra is None))
        if extra is not None:
            K.mm(Sp[:, :N], extra[0], extra[1], start=False, stop=True)
        src = Sp[:, :N]
        if mask is not None:
            self.mi = (self.mi + 1) % 3
            sm = self.Sm[self.mi]
            if mg == 1:
                K.tt(sm[:, :N], Sp[:, :N], mask, ALU.add)
            else:
                mb = rv(mask, mask.ap.unsqueeze(1).to_broadcast([128, mg, N // mg]))
                K.tt(rv(sm[:, :], sm.t[:, :N].rearrange("p (m q) -> p m q", m=mg)),
                     rv(Sp[:, :], Sp.t[:, :N].rearrange("p (m q) -> p m q", m=mg)), mb, ALU.add)
            src = sm[:, :N]
        if Pt is None:
            self.pi = (self.pi + 1) % self.NP
            Pt = self.P[self.pi][:, :N]
        K.act(Pt, src, AF.Exp, scale=scale)
        onesv = ones if ones is not None else self.c["onesb"][:, :M]

        def pv():
            if merged:
                K.mm(O[:, :N], v_lhsT, Pt, start=first, stop=last)
            else:
                K.mm(O[:dv, :N], v_lhsT, Pt, start=first, stop=last)
                K.mm(Sum[:M, :N], onesv, Pt, start=first, stop=last)
        self.pending.append(pv)
        if len(self.pending) > self.depth:
            self.pending.pop(0)()
        self.tick()
        return Pt

    def flush(self):
        while self.pending:
            self.pending.pop(0)()

    def recip_m1(self, R, O, N):
        K = self.K
        K.ts(R[64:128, :N], O[64:128, :N], 1e-30, ALU.max)
        K.op("dve", "reciprocal", {"out": R[64:128, :N]}, {"in_": R[64:128, :N]})

    def recip_m2(self, R, N):
        K = self.K
        mp = self.misc_ps
        K.mm(mp[0:64, :N], self.c["ident32"][64:128, 64:128], R[64:128, :N])
        K.copy(R[0:64, :N], mp[0:64, :N], q="act")

    def recip(self, R, Sum, M, N):
        K = self.K
        self.flush()
        K.ts(R[:M, :N], Sum[:M, :N], 1e-30, ALU.max)
        K.op("dve", "reciprocal", {"out": R[:M, :N]}, {"in_": R[:M, :N]})


def build_B(K, c, lam_init, io, do=("diff", "mla", "nsa")):
    A = AttnCtx(K, c)
    NQB = S // 128
    cm512 = io["cm512"]
    cm_s = K.sb([128, 4, 512], F32)
    for j in range(4):
        K.dma(cm_s[:, j, :], cm512[j, :, :])
    wlow = io["wlow"]
    wlow_s = K.sb([128, 128], F32)
    K.dma(wlow_s[:, :], wlow[:, :])
    kaug_tok = io["kaug_tok"]

    if "diff" in do:
        dqT = io["dqT"]; dkT = io["dkT"]; dvv = io["dv"]; qaug_d = io["qaug_diff"]
        lam_p = io["lam_p"]; subg = io["subg"]; o_diff = io["o_diffT"]
        with K.scope():
            lp = K.sb([128, 256]); K.dma(lp[:, :], lam_p[:, :])
            pr = K.sb([128, 128]); l2 = K.sb([128, 2]); lam = K.sb([128, 1]); nlam = K.sb([128, 1])
            K.tt(pr[:, 0:64], lp[:, 0:64], lp[:, 64:128], ALU.mult)
            K.tt(pr[:, 64:128], lp[:, 128:192], lp[:, 192:256], ALU.mult)
            K.op("dve", "tensor_reduce", {"out": l2[:, 0:1]}, {"in_": pr[:, 0:64]}, axis=AX.X, op=ALU.add)
            K.op("dve", "tensor_reduce", {"out": l2[:, 1:2]}, {"in_": pr[:, 64:128]}, axis=AX.X, op=ALU.add)
            K.act(l2[:, :], l2[:, :], AF.Exp)
            K.tt(lam[:, :], l2[:, 0:1], l2[:, 1:2], ALU.subtract)
            K.ts(nlam[:, :], lam[:, :], lam_init, ALU.add, -1.0, ALU.mult)
            sg = K.sb([128, 1]); K.dma(sg[:, :], subg[:, :])
            K.ts(sg[:, :], sg[:, :], 1.0 - lam_init, ALU.mult)
            QA = K.sb([73, 2, S], BF16)
            KAt = K.sb([73, 2, S], BF16)
            Vt = K.sb([128, NQB, 128], BF16)
            o1 = K.sb([128, 256], F32); o2 = K.sb([128, 256], F32); sq = K.sb([128, 256], F32)
            ob = [K.sb([128, 256], BF16) for _ in range(2)]
            for h in range(4):
                for m in range(2):
                    r0 = h * 128 + m * 64
                    K.dma(QA[0:64, m, :], dqT[r0:r0 + 64, :])
                    K.dma(QA[64:73, m, :], qaug_d[h, :, :])
                    K.dma(KAt[0:64, m, :], dkT[r0:r0 + 64, :])
                    K.dma(KAt[64:73, m, :], kaug_tok[:, :])
                load_tm(K, Vt, dvv, h * 128, 128)
                for qc in range(S // 256):
                    q0 = qc * 256
                    O, Sum, R = A.nextO()
                    nkt = 2 * qc + 2
                    for kt in range(nkt):
                        j = kt - 2 * qc
                        mask = cm_s[:, j, 0:256] if j >= 0 else None
                        qk = [(KAt[:, m, kt * 128:(kt + 1) * 128], QA[:, m, q0:q0 + 256], m * 256, 256) for m in range(2)]
                        A.step(qk, 512, 0.125, mask, Vt[:, kt, :], 128, O, Sum, 128, kt == 0, kt == nkt - 1, mg=2)
                    A.recip(R, Sum, 128, 512)

                    def fin(O=O, R=R, h=h, qc=qc, q0=q0):
                        K.tt(o1[:, :], O[:, 0:256], R[:, 0:256], ALU.mult)
                        K.tt(o2[:, :], O[:, 256:512], R[:, 256:512], ALU.mult)
                        K.op("dve", "scalar_tensor_tensor", {"out": o1[:, :]}, {"in0": o2[:, :], "in1": o1[:, :], "scalar": nlam[:, 0:1]},
                             op0=ALU.mult, op1=ALU.add)
                        K.act(sq[:, :], o1[:, :], AF.Square)
                        mp = A.misc_ps
                        K.mm(mp[:, :256], c["ones32"][:, :], sq[:, :])
                        K.ts(sq[:, :], mp[:, :256], 1.0 / 128, ALU.mult, EPS_RMS, ALU.add)
                        K.act(sq[:, :], sq[:, :], AF.Sqrt)
                        K.op("dve", "reciprocal", {"out": sq[:, :]}, {"in_": sq[:, :]})
                        K.tt(o1[:, :], o1[:, :], sq[:, :], ALU.mult)
                        obb = ob[qc % 2]
                        K.act(obb[:, :], o1[:, :], AF.Identity, scale=sg[:, 0:1])
                        K.dma(o_diff.k((h, qc))[h * 128:(h + 1) * 128, q0:q0 + 256], obb[:, :], q="poolq")
                    A.defer(fin, 5, {A.oi})
            A.drain()

    if "mla" in do:
        qmT = io["qmT"]; kropeT = io["kropeT"]; knopeT = io["knopeT"]; mv = io["vm"]; o_mla = io["o_mlaT"]
        msc = 96 ** -0.5
        with K.scope():
            QA = K.sb([96, S], BF16)
            KAt = K.sb([96, S], BF16)
            Vt = K.sb([128, NQB, 128], BF16)
            K.memset(Vt[:, :, :], 1.0)
            ob = [K.sb([64, 512], BF16) for _ in range(2)]
            for h in range(8):
                K.dma(QA[:, :], qmT[h * 96:(h + 1) * 96, :])
                K.dma(KAt[0:32, :], kropeT[:, :])
                K.dma(KAt[32:96, :], knopeT[h * 64:(h + 1) * 64, :])
                load_tm(K, Vt, mv, h * 64, 64)
                for qc in range(S // 512):
                    q0 = qc * 512
                    O, Sum, R = A.nextO()
                    nkt = 4 * qc + 4
                    for kt in range(nkt):
                        j = kt - 4 * qc
                        mask = cm_s[:, j, :] if j >= 0 else None
                        A.step([(KAt[:, kt * 128:(kt + 1) * 128], QA[:, q0:q0 + 512], 0, 512)], 512, msc, mask,
                               Vt[:, kt, :], 64, O, Sum, 64, kt == 0, kt == nkt - 1, merged=True)
                    def fin1(O=O, R=R):
                        A.recip_m1(R, O, 512)

                    def fin2(O=O, R=R, h=h, qc=qc, q0=q0):
                        A.recip_m2(R, 512)
                        obb = ob[qc % 2]
                        K.tt(obb[:, :], O[:64, :], R[:64, :], ALU.mult)
                        K.dma(o_mla.k((h, qc))[h * 64:(h + 1) * 64, q0:q0 + 512], obb[:, :], q="poolq")
                    A.defer(fin1, 3, {A.oi})
                    A.defer(fin2, 7, {A.oi})
            A.drain()
    if "nsa" in do:
        for g in range(2):
            build_nsa(K, c, A, cm_s, wlow_s, kaug_tok, io, g)


def build_nsa(K, c, A, cm_s, wlow_s, kaug_tok, io, grp):
    NQB = S // 128
    nqT = io["nqT"]; qaug_n = io["qaug_nsa"]
    kcT = rv(io["kcT"][:, :], io["kcT"].t[grp * 64:(grp + 1) * 64, :])
    vcT = rv(io["vcT"][:, :], io["vcT"].t[grp * 64:(grp + 1) * 64, :])
    ksa = rv(io["ksT"][:, :], io["ksT"].t[grp * 64:(grp + 1) * 64, :])
    kwa = rv(io["kwT"][:, :], io["kwT"].t[grp * 64:(grp + 1) * 64, :])
    vs_in = io["vs"]; vw_in = io["vw"]
    ngT = rv(io["ngT"][:, :], io["ngT"].t[grp * 12:(grp + 1) * 12, :])
    w1 = {"k": io["cmp_w1_k"], "v": io["cmp_w1_v"]}
    w2 = {"k": io["cmp_w2_k"], "v": io["cmp_w2_v"]}
    pos = {"k": io["posk"], "v": io["posv"]}
    kaug_cmp = io["kaug_cmp"]; cmask = io["cmask"]; cprev = io["cprev"]; amat = io["amat"]
    addmask = io["addmask"]; E_in = io["E"]; oh_in = io["oh"]; o_nsa = io["o_nsaT"]

    KAc = K.sb([73, 512], BF16)
    Vc = K.sb([128, 4, 64], F32)
    K.memset(KAc[:, :], 0.0)
    K.dma(KAc[64:73, :], kaug_cmp[:, :])
    with K.scope():
        xT = K.sb([64, S], BF16)
        w1s = K.sb([64, 32, 256], BF16)
        w2s = K.sb([128, 2, 64], BF16)
        pos32 = K.sb([64, 32], F32); posb = K.sb([64, 32], BF16)
        g = K.sb([128, 2, 512], BF16)
        bias = K.sb([128, 1], F32)
        x = K.sb([128, 512], F32); x2 = K.sb([128, 512], F32)
        K.memset(g[:, :, :], 0.0)
        for which, src in (("k", kcT), ("v", vcT)):
            K.dma(xT[:, :], src)
            for l0 in range(0, 32, 8):
                K.dma(w1s[:, l0:l0 + 8, :], rv(w1[which][:, :], w1[which].t[l0 * 64:(l0 + 8) * 64, :].rearrange("(l d) j -> d l j", d=64)))
            K.dma(w2s[:, :, :], rv(w2[which][:, :], w2[which].t[:, :].rearrange("(jh p) d -> p jh d", p=128)))
            K.dma(pos32[:, :], pos[which][:, :])
            K.copy(posb[:, :], pos32[:, :])
            xv = xT.t[:, :].rearrange("d (c r) -> d c r", r=16)
            for jh in range(2):
                hp = A.nextS()
                for l in range(32):
                    rhs = rv(xT[:, :], xv[:, (l // 16):(l // 16) + 511, l % 16])
                    K.mm(hp[:, :511], w1s[:, l, jh * 128:(jh + 1) * 128], rhs, start=(l == 0), stop=(l == 31))
                bp = A.misc_ps
                for l in range(32):
                    K.mm(bp[:, 0:1], w1s[:, l, jh * 128:(jh + 1) * 128], posb[:, l:l + 1], start=(l == 0), stop=(l == 31))
                K.copy(bias[:, :], bp[:, 0:1])
                K.ts(x[:, :511], hp[:, :511], bias[:, 0:1], ALU.add)
                K.tt(x2[:, :511], x[:, :511], x[:, :511], ALU.mult)
                K.ts(x2[:, :511], x2[:, :511], 0.044715, ALU.mult, 1.0, ALU.add)
                K.tt(x2[:, :511], x2[:, :511], x[:, :511], ALU.mult)
                K.act(x2[:, :511], x2[:, :511], AF.Sigmoid, scale=1.5957691216057308)
                K.tt(g[:, jh, :511], x[:, :511], x2[:, :511], ALU.mult)
            if which == "k":
                kp = A.nextS()
                for jh in range(2):
                    K.mm(kp[:64, :511], w2s[:, jh, :], g[:, jh, :511], start=(jh == 0), stop=(jh == 1))
                K.copy(KAc[0:64, :511], kp[:64, :511])
            else:
                for ct in range(4):
                    vp = A.nextS()
                    for jh in range(2):
                        K.mm(vp[:, :64], g[:, jh, ct * 128:(ct + 1) * 128], w2s[:, jh, :], start=(jh == 0), stop=(jh == 1))
                    K.copy(Vc[:, ct, :], vp[:, :64])
    with K.scope():
        KAs = K.sb([73, S], BF16); KAw = K.sb([73, S], BF16)
        K.dma(KAs[0:64, :], ksa); K.dma(KAs[64:73, :], kaug_tok[:, :])
        K.dma(KAw[0:64, :], kwa); K.dma(KAw[64:73, :], kaug_tok[:, :])
        Vs = K.sb([128, NQB, 128], BF16); Vw = K.sb([128, NQB, 128], BF16)
        K.memset(Vs[:, :, :], 1.0); K.memset(Vw[:, :, :], 1.0)
        load_tm(K, Vs, vs_in, grp * 64, 64)
        load_tm(K, Vw, vw_in, grp * 64, 64)
        E_s = K.sb([128, S], BF16); K.dma(E_s[:, :], E_in[:, :])
        ng_s = K.sb([12, S], BF16); K.dma(ng_s[:, :], ngT)
        oh_s = K.sb([12, 768], BF16); K.dma(oh_s[:, :], oh_in[:, :])
        cprev_s = K.sb([128, 128], F32); K.dma(cprev_s[:, :], cprev[:, :])
        am_s = K.sb([128, 4, 128], F32)
        for ct in range(4):
            K.dma(am_s[:, ct, :], amat[ct, :, :])
        Qt = [K.sb([73, 4, 128], BF16) for _ in range(2)]
        cmk = [K.sb([128, 128], F32) for _ in range(2)]
        adm = [K.sb([128, 128], F32) for _ in range(2)]
        Pc = [K.sb([128, 512], F32) for _ in range(4)]
        pg = K.sb([128, 4, 128], F32)
        ob = [K.sb([64, 512], F32) for _ in range(3)]
        obc = [K.sb([64, 512], F32) for _ in range(2)]
        gs = K.sb([64, 512], F32)
        sc = K.sb([128, 128], F32); sc2 = K.sb([128, 128], F32)
        m8a = K.sb([128, 8], F32); m8b = K.sb([128, 8], F32)
        negsel = K.sb([128, 128], F32)
        nsT = K.sb([128, 128], BF16)
        outb = [K.sb([64, 512], BF16) for _ in range(2)]
        ident = c["ident32"]
        nsTs = [nsT, K.sb([128, 128], BF16)]
        obc3 = [obc[0], obc[1], K.sb([64, 512], F32)]

        def cmpA(qb):
            q0 = qb * 128
            Q = Qt[qb % 2]
            K.dma(Q[0:64, :, :], rv(nqT[:, :], nqT.t[grp * 256:(grp + 1) * 256, q0:q0 + 128].rearrange("(h d) s -> d h s", h=4)))
            K.dma(Q[64:73, :, :], qaug_n[:, grp * 4:(grp + 1) * 4, q0:q0 + 128])
            Qv = Q[:, :, :]
            nct = qb // 16 + 1
            ck = cmk[qb % 2]
            K.dma(ck[:, :], cmask[1 if nct == 4 else 0, qb % 16, :, :])
            ad = adm[qb % 2]
            K.dma(ad[:, :], addmask[qb, :, :])
            O, Sum, R = A.nextO()
            for ct in range(nct):
                last = (ct == nct - 1)
                cmsk = ck[:, :] if last else (cprev_s[:, :] if (qb % 16 == 0 and ct == nct - 2) else None)
                A.step([(KAc[:, ct * 128:(ct + 1) * 128], Qv, 0, 512)], 512, 0.125, cmsk,
                       Vc[:, ct, :], 64, O, Sum, 128, ct == 0, last, ones=c["ones32"][:, :], Pt=Pc[ct][:, :], mg=4)
            A.recip(R, Sum, 128, 512)
            K.tt(obc3[qb % 3][:, :], O[:64, :], R[:64, :], ALU.mult)
            for ct in range(nct):
                K.tt(Pc[ct][:, :], Pc[ct][:, :], R[:, :], ALU.mult)
                K.tt(pg[:, ct, :], Pc[ct][:, 0:128], Pc[ct][:, 128:256], ALU.add, q="pool")
                K.tt(pg[:, ct, :], pg[:, ct, :], Pc[ct][:, 256:384], ALU.add, q="pool")
                K.tt(pg[:, ct, :], pg[:, ct, :], Pc[ct][:, 384:512], ALU.add, q="pool")

        def cmpB(qb):
            nct = qb // 16 + 1
            ad = adm[qb % 2]
            mp = A.misc_ps
            for ct in range(nct):
                K.mm(mp[:, 0:128], pg[:, ct, :], am_s[:, ct, :], start=(ct == 0), stop=(ct == nct - 1))
            K.tt(sc[:, :], mp[:, 0:128], ad[:, :], ALU.add)
            K.op("dve", "max", {"out": m8a[:, :]}, {"in_": sc[:, :]})
            K.op("dve", "match_replace", {"out": sc2[:, :]}, {"in_to_replace": m8a[:, :], "in_values": sc[:, :]}, imm_value=-3.0e4)
            K.op("dve", "max", {"out": m8b[:, :]}, {"in_": sc2[:, :]})
            K.ts(negsel[:, :], sc[:, :], m8b[:, 7:8], ALU.is_lt, -30000.0, ALU.mult)

        def cmpC(qb):
            tp = A.nextS()
            K.tr(tp[:, 0:128], negsel[:, :], ident[:, :])
            K.copy(nsTs[qb % 2][:, :], tp[:, 0:128])

        cmpA(0); cmpB(0); cmpC(0)
        for qb in range(NQB):
            q0 = qb * 128
            Qv = Qt[qb % 2][:, :, :]
            A.force_tag(("C", qb))
            if qb + 1 < NQB:
                cmpA(qb + 1)
                A.defer((lambda qn=qb + 1: cmpB(qn)), 8, set(), tag=("B", qb + 1))
                A.defer((lambda qn=qb + 1: cmpC(qn)), 16, set(), tag=("C", qb + 1))
            nsq = nsTs[qb % 2]
            nsv = rv(nsq[:, :], nsq.t[:, :].unsqueeze(1).to_broadcast([128, 4, 128]))
            O, Sum, R = A.nextO()
            for kt in range(qb + 1):
                last = (kt == qb)
                A.step([(KAs[:, kt * 128:(kt + 1) * 128], Qv, 0, 512)], 512, 0.125, cm_s[:, 0, 0:128] if last else None,
                       Vs[:, kt, :], 64, O, Sum, 64, kt == 0, last, extra=(E_s[:, kt * 128:(kt + 1) * 128], nsv), mg=4, merged=True)
            Osel, Rsel, bsel = O, R, A.oi

            def sel1(O=O, R=R):
                A.recip_m1(R, O, 512)
            A.defer(sel1, 3, {A.oi})
            O, Sum, R = A.nextO()
            k0 = max(0, qb - 4)
            for kt in range(k0, qb + 1):
                last = (kt == qb)
                mask = cm_s[:, 0, 0:128] if last else (wlow_s[:, :] if kt == qb - 4 else None)
                A.step([(KAw[:, kt * 128:(kt + 1) * 128], Qv, 0, 512)], 512, 0.125, mask,
                       Vw[:, kt, :], 64, O, Sum, 64, kt == k0, last, mg=4, merged=True)
            Owin, Rwin, bwin = O, R, A.oi

            def win1(O=O, R=R):
                A.recip_m1(R, O, 512)
            A.defer(win1, 3, {A.oi})

            def post(Osel=Osel, Rsel=Rsel, Owin=Owin, Rwin=Rwin, qb=qb, q0=q0):
                A.recip_m2(Rsel, 512)
                K.tt(ob[1][:, :], Osel[:64, :], Rsel[:64, :], ALU.mult)
                A.recip_m2(Rwin, 512)
                K.tt(ob[2][:, :], Owin[:64, :], Rwin[:64, :], ALU.mult)
                obs = [obc3[qb % 3], ob[1], ob[2]]
                for gi in range(3):
                    gp = A.nextS()
                    for h in range(4):
                        j = h * 3 + gi
                        K.mm(gp[:64, h * 128:(h + 1) * 128], oh_s[:, j * 64:(j + 1) * 64], ng_s[:, q0:q0 + 128])
                    if gi == 0:
                        K.tt(gs[:, :], obs[0][:, :], gp[:64, :], ALU.mult)
                    else:
                        K.tt(obs[gi][:, :], obs[gi][:, :], gp[:64, :], ALU.mult)
                        K.tt(gs[:, :], gs[:, :], obs[gi][:, :], ALU.add, q="pool")
                oo = outb[qb % 2]
                K.copy(oo[:, :], gs[:, :], q="act")
                for h in range(4):
                    K.dma(o_nsa.k((grp, qb, h))[grp * 256 + h * 64:grp * 256 + (h + 1) * 64, q0:q0 + 128], oo[:, h * 128:(h + 1) * 128], q="poolq")
            A.defer(post, 7, {bsel, bwin})
        A.drain()


def build_C(K, c, last_layer, io, l):
    hT_in = io["hT32"]
    oT = {n: io[n] for n in ("o_nsaT", "o_diffT", "o_mlaT")}
    w_mg = io["w_in"]
    w_br = {n: io[n] for n in ("w_br_nsa", "w_br_diff", "w_br_mla")}
    w_out = io["w_out"]
    ln1g = io["ln1_g"]; ln1b = io["ln1_b"]; ln2g = io["ln2_g"]; ln2b = io["ln2_b"]
    rw = io["router_w"]; rb = io["router_b"]
    w1 = io["moe_w1"]; w3 = io["moe_w3"]; w2 = io["moe_w2"]
    h1_scr = K.dram("h1_scr%d" % l, [D, T], F32, "Internal")
    if last_layer:
        out = io["out"]
    else:
        hT_out = io["hT32_next"]

    ps = [K.ps([128, 512]) for _ in range(8)]
    pi = [0]

    def nps():
        pi[0] = (pi[0] + 1) % 6
        return ps[pi[0]]
    psA, psB = ps[6], ps[7]
    names = ("o_nsaT", "o_diffT", "o_mlaT")
    wnames = ("w_br_nsa", "w_br_diff", "w_br_mla")

    cmb_all = K.sb([128, NCH * 4, 16], F32)
    with K.scope():
        rw_s = K.sb([128, 8, 16]); K.dma(rw_s[:, :, :], rw[:, :, :])
        rb_s = K.sb([128, 16]); K.dma(rb_s[:, :], rb[:, :])
        aff = K.sb([128, 16]); sel = K.sb([128, 16]); pair = K.sb([128, 4, 6]); gsc = K.sb([128, 4]); gmx = K.sb([128, 1])
        goh = K.sb([128, 4]); selm = K.sb([128, 16]); m8 = K.sb([128, 8]); cho = K.sb([128, 16]); gsum = K.sb([128, 1])
        Wmg = K.sb([128, 8, 3072], BF16)
        for kt in range(8):
            K.dma(Wmg.k(kt)[:, kt, :], w_mg[kt * 128:(kt + 1) * 128, NA:NA + 3072])
        Wbr = {}
        for n in w_br:
            Wbr[n] = K.sb([128, 4, D], BF16)
            for kt in range(4):
                K.dma(Wbr[n][:, kt, :], w_br[n][kt * 128:(kt + 1) * 128, :])
        Wo = K.sb([128, 8, D], BF16)
        for kt in range(8):
            K.dma(Wo[:, kt, :], w_out[kt * 128:(kt + 1) * 128, :])
        g1 = K.sb([128, 8]); K.dma(g1[:, :], ln1g[:, :])
        b1 = K.sb([128, 8]); K.dma(b1[:, :], ln1b[:, :])
        h32 = K.sb([128, 8, 512], F32)
        hb = K.sb([128, 8, 512], BF16)
        ob = {n: K.sb([128, 4, 512], BF16) for n in oT}
        G = K.sb([128, 512], BF16)
        yb = K.sb([128, 8, 512], BF16)
        y32 = K.sb([128, 512], F32); tB = K.sb([128, 512], F32)
        r32 = K.sb([128, 8, 512], F32)
        tmp = {"sq": K.sb([128, 8, 512], F32), "mean": K.sb([128, 512]), "msq": K.sb([128, 512]), "rstd": K.sb([128, 512])}
        h1 = K.sb([128, 8, 512], F32)
        for ch in range(NCH):
            t0 = ch * 512
            tsl = slice(t0, t0 + 512)
            for ft in range(8):
                K.dma(h32[:, ft, :], hT_in[ft * 128:(ft + 1) * 128, tsl])
            for ft in range(8):
                K.copy(hb[:, ft, :], h32[:, ft, :], q=("act" if ft % 2 else "dve"))
            for n in names:
                for kt in range(4):
                    K.dma(ob[n][:, kt, :], oT[n][kt * 128:(kt + 1) * 128, tsl])
            for mt in range(8):
                for bi, (n, wn) in enumerate(zip(names, wnames)):
                    gp = nps()
                    for kt in range(8):
                        K.mm(gp[:, :], Wmg.k(kt)[:, kt, bi * 1024 + mt * 128: bi * 1024 + (mt + 1) * 128], hb[:, kt, :], start=(kt == 0), stop=(kt == 7))
                    K.act(G[:, :], gp[:, :], AF.Sigmoid)
                    yp = nps()
                    for kt in range(4):
                        K.mm(yp[:, :], Wbr[wn][:, kt, mt * 128:(mt + 1) * 128], ob[n][:, kt, :], start=(kt == 0), stop=(kt == 3))
                    if bi == 0:
                        K.tt(y32[:, :], yp[:, :], G[:, :], ALU.mult)
                    else:
                        K.tt(tB[:, :], yp[:, :], G[:, :], ALU.mult)
                        K.tt(y32[:, :], y32[:, :], tB[:, :], ALU.add, q="pool")
                K.copy(yb[:, mt, :], y32[:, :], q="act")
            for mt in range(8):
                mp = nps()
                for kt in range(8):
                    K.mm(mp[:, :], Wo[:, kt, mt * 128:(mt + 1) * 128], yb[:, kt, :], start=(kt == 0), stop=(kt == 7))
                K.op("dve", "scalar_tensor_tensor", {"out": r32[:, mt, :]}, {"in0": h32[:, mt, :], "in1": mp[:, :]},
                     scalar=ALPHA, op0=ALU.mult, op1=ALU.add)
            ln_fm(K, c, r32, g1, b1, h1, None, psA, psB, tmp)
            for sub in range(4):
                lp = nps()
                for kt in range(8):
                    K.mm(lp[:, 0:16], h1[:, kt, sub * 128:(sub + 1) * 128], rw_s[:, kt, :], start=(kt == 0), stop=(kt == 7))
                K.act(aff[:, :], lp[:, 0:16], AF.Sigmoid)
                K.tt(sel[:, :], aff[:, :], rb_s[:, :], ALU.add)
                sv = sel.t[:, :].rearrange("p (g e) -> p g e", e=4)
                pairs = [(0, 1), (0, 2), (0, 3), (1, 2), (1, 3), (2, 3)]
                for pi_, (a_, b_) in enumerate(pairs):
                    K.tt(rv(pair[:, :, :], pair.t[:, :, pi_]), rv(sel[:, :], sv[:, :, a_]), rv(sel[:, :], sv[:, :, b_]), ALU.add)
                K.op("dve", "tensor_reduce", {"out": gsc[:, :]}, {"in_": pair[:, :, :]}, axis=AX.X, op=ALU.max)
                K.op("dve", "tensor_reduce", {"out": gmx[:, :]}, {"in_": gsc[:, :]}, axis=AX.X, op=ALU.max)
                K.ts(goh[:, :], gsc[:, :], gmx[:, 0:1], ALU.is_ge, 1.0, ALU.subtract)
                K.ts(goh[:, :], goh[:, :], 1.0e4, ALU.mult)
                K.tt(rv(selm[:, :], selm.t[:, :].rearrange("p (g e) -> p g e", e=4)), rv(sel[:, :], sv),
                     rv(goh[:, :], goh.t[:, :].unsqueeze(2).to_broadcast([128, 4, 4])), ALU.add)
                K.op("dve", "max", {"out": m8[:, :]}, {"in_": selm[:, :]})
                K.ts(cho[:, :], selm[:, :], m8[:, 1:2], ALU.is_ge)
                K.tt(cho[:, :], cho[:, :], aff[:, :], ALU.mult)
                K.op("dve", "tensor_reduce", {"out": gsum[:, :]}, {"in_": cho[:, :]}, axis=AX.X, op=ALU.add)
                K.op("dve", "reciprocal", {"out": gsum[:, :]}, {"in_": gsum[:, :]})
                K.ts(cmb_all.k(ch * 4 + sub)[:, ch * 4 + sub, :], cho[:, :], gsum[:, 0:1], ALU.mult)
            for ft in range(8):
                K.dma(h1_scr.k((ch, ft))[ft * 128:(ft + 1) * 128, tsl], h1[:, ft, :], q="poolq")

    with K.scope():
        g2 = K.sb([128, 8]); K.dma(g2[:, :], ln2g[:, :])
        b2 = K.sb([128, 8]); K.dma(b2[:, :], ln2b[:, :])
        h1 = K.sb([128, 8, 512], F32)
        h1b = K.sb([128, 8, 512], BF16)
        h32 = K.sb([128, 8, 512], F32)
        r32 = K.sb([128, 8, 512], F32)
        tmp = {"sq": K.sb([128, 8, 512], F32), "mean": K.sb([128, 512]), "msq": K.sb([128, 512]), "rstd": K.sb([128, 512])}
        comb = K.sb([128, 16, 512], F32)
        hid = K.sb([128, 4, 512], BF16)
        sA = K.sb([128, 512], F32); tB = K.sb([128, 512], F32)
        ffn = K.sb([128, 8, 512], F32)
        W1 = [K.sb([128, 8, 512], BF16) for _ in range(2)]
        W3 = [K.sb([128, 8, 512], BF16) for _ in range(2)]
        W2 = [K.sb([128, 4, D], BF16) for _ in range(2)]
        otile = [K.sb([128, D], F32) for _ in range(2)]
        for ch in range(NCH):
            t0 = ch * 512
            tsl = slice(t0, t0 + 512)
            for ft in range(8):
                K.dma(h1[:, ft, :], h1_scr.k((ch, ft))[ft * 128:(ft + 1) * 128, tsl])
            for ft in range(8):
                K.copy(h1b[:, ft, :], h1[:, ft, :], q=("act" if ft % 2 else "dve"))
            for sub in range(4):
                cmbv = cmb_all.k(ch * 4 + sub)
                for e4 in range(4):
                    bp = nps()
                    for e in range(4):
                        ee = e4 * 4 + e
                        K.mm(bp[:, e * 128:(e + 1) * 128], rv(cmbv[:, ch * 4 + sub, :], cmb_all.t[:, ch * 4 + sub, ee:ee + 1].to_broadcast([128, 128])), c["ident32"][:, :])
                    for e in range(4):
                        ee = e4 * 4 + e
                        K.copy(comb[:, ee, sub * 128:(sub + 1) * 128], bp[:, e * 128:(e + 1) * 128], q=("act" if e % 2 else "dve"))
            for e in range(16):
                W1e, W3e, W2e = W1[e % 2], W3[e % 2], W2[e % 2]
                for k0 in range(0, 8, 4):
                    K.dma(W1e.ks(range(k0, k0 + 4), (slice(None), slice(k0, k0 + 4), slice(None))),
                          rv(w1[:, :], w1.t[e * D + k0 * 128:e * D + (k0 + 4) * 128, :].rearrange("(kt p) f -> p kt f", p=128)), q="sp")
                    K.dma(W3e.ks(range(k0, k0 + 4), (slice(None), slice(k0, k0 + 4), slice(None))),
                          rv(w3[:, :], w3.t[e * D + k0 * 128:e * D + (k0 + 4) * 128, :].rearrange("(kt p) f -> p kt f", p=128)), q="sp")
                K.dma(W2e.ks(range(4), (slice(None), slice(None), slice(None))),
                      rv(w2[:, :], w2.t[e * 512:(e + 1) * 512, :].rearrange("(kt p) f -> p kt f", p=128)), q="sp")
                for ft in range(4):
                    ap_ = nps()
                    for kt in range(8):
                        K.mm(ap_[:, :], W1e.k(kt)[:, kt, ft * 128:(ft + 1) * 128], h1b[:, kt, :], start=(kt == 0), stop=(kt == 7))
                    bp_ = nps()
                    for kt in range(8):
                        K.mm(bp_[:, :], W3e.k(kt)[:, kt, ft * 128:(ft + 1) * 128], h1b[:, kt, :], start=(kt == 0), stop=(kt == 7))
                    K.act(sA[:, :], ap_[:, :], AF.Silu)
                    K.tt(tB[:, :], bp_[:, :], comb[:, e, :], ALU.mult)
                    K.tt(hid[:, ft, :], sA[:, :], tB[:, :], ALU.mult, q="pool")
                for mt in range(8):
                    fp = nps()
                    for kt in range(4):
                        K.mm(fp[:, :], W2e.k(kt)[:, kt, mt * 128:(mt + 1) * 128], hid[:, kt, :], start=(kt == 0), stop=(kt == 3))
                    if e == 0:
                        K.copy(ffn[:, mt, :], fp[:, :], q="act")
                    else:
                        K.tt(ffn[:, mt, :], ffn[:, mt, :], fp[:, :], ALU.add)
            for mt in range(8):
                K.op("dve", "scalar_tensor_tensor", {"out": r32[:, mt, :]}, {"in0": h1[:, mt, :], "in1": ffn[:, mt, :]},
                     scalar=ALPHA, op0=ALU.mult, op1=ALU.add)
            ln_fm(K, c, r32, g2, b2, h32, None, psA, psB, tmp)
            if last_layer:
                for sub in range(4):
                    ot = otile[sub % 2]
                    for ft in range(8):
                        tp = nps()
                        K.tr(tp[:, 0:128], h32[:, ft, sub * 128:(sub + 1) * 128], c["ident32"][:, :])
                        K.copy(ot[:, ft * 128:(ft + 1) * 128], tp[:, 0:128], q=("act" if ft % 2 else "dve"))
                    K.outs.append(K.dma(out.k((ch, sub))[t0 + sub * 128:t0 + (sub + 1) * 128, :], ot[:, :], q="poolq"))
            else:
                for ft in range(8):
                    K.dma(hT_out.k((ch, ft))[ft * 128:(ft + 1) * 128, tsl], h32[:, ft, :], q="poolq")


LAYER_W = [("w_in", [D, 6328]), ("w_uq", [256, 768]), ("w_ukv", [128, 1024]),
           ("w_br_nsa", [512, D]), ("w_br_diff", [512, D]), ("w_br_mla", [512, D]), ("w_out", [D, D]),
           ("moe_w1", [16 * D, 512]), ("moe_w3", [16 * D, 512]), ("moe_w2", [16 * 512, D]),
           ("cmp_w1_k", [2048, 256]), ("cmp_w1_v", [2048, 256]), ("cmp_w2_k", [256, 64]), ("cmp_w2_v", [256, 64])]
LAYER_P = [("qg", [128, 2]), ("kvg", [128, 1]), ("lam_p", [128, 256]), ("subg", [128, 1]), ("posk", [64, 32]), ("posv", [64, 32]),
           ("ln1_g", [128, 8]), ("ln1_b", [128, 8]), ("ln2_g", [128, 8]), ("ln2_b", [128, 8])]
CONSTS = [("cosT", [32, S], F32), ("sinT", [32, S], F32), ("protT", [32, 32], F32), ("cm512", [4, 128, 512], F32),
          ("wlow", [128, 128], F32), ("kaug_tok", [9, S], BF16), ("qaug_diff", [4, 9, S], BF16), ("qaug_nsa", [9, 8, S], BF16),
          ("kaug_cmp", [9, 512], BF16), ("cmask", [2, 16, 128, 128], F32), ("cprev", [128, 128], F32), ("amat", [4, 128, 128], F32),
          ("addmask", [S // 128, 128, 128], F32), ("E", [128, S], BF16), ("oh", [12, 768], BF16),
          ("router_w", [128, 8, 16], F32), ("router_b", [128, 16], F32), ("ln_g", [128, 8], F32), ("ln_b", [128, 8], F32)]
SCR_FM = [("nqT", 512), ("kcT", 128), ("vcT", 128), ("ksT", 128), ("kwT", 128), ("ngT", 24), ("dqT", 512), ("dkT", 512),
          ("qmT", 768), ("kropeT", 32), ("knopeT", 512), ("o_nsaT", 512), ("o_diffT", 512), ("o_mlaT", 512)]
SCR_TM = [("vs", 128), ("vw", 128), ("dv", 512), ("vm", 512)]


def conv_w(K, src, dst, rows, cols, bufs):
    a = rows // 128
    sv = src.t[:, :].rearrange("(p a) c -> p (a c)", p=128)
    dv = dst.t[:, :].rearrange("(p a) c -> p (a c)", p=128)
    n = a * cols
    CH = 4096
    for i, c0 in enumerate(range(0, n, CH)):
        w = min(CH, n - c0)
        fa, fb = bufs[0][bufs[2][0] % 3], bufs[1][bufs[2][0] % 3]
        bufs[2][0] += 1
        K.dma(fa[:, :w], rv(src.k(i)[:, :], sv[:, c0:c0 + w]), q="sp")
        if i % 2 == 0:
            K.copy(fb[:, :w], fa[:, :w], q="dve")
        else:
            K.copy(fb[:, :w], fa[:, :w], q="act")
        K.dma(rv(dst.k(i)[:, :], dv[:, c0:c0 + w]), fb[:, :w], q="poolq")


def build_fused(debug=False, nlayers=2, stages="wABC"):
    K = KB()
    c = consts(K)
    x = K.inp("x", [S, D], F32)
    out = K.out("out", [S, D], F32)
    cst = {n: K.inp(n, sh, dt) for n, sh, dt in CONSTS}
    win = [{n: K.inp("%s_%d" % (n, l), sh, F32) for n, sh in LAYER_W} for l in range(nlayers)]
    prm = [{n: K.inp("%s_%d" % (n, l), sh, F32) for n, sh in LAYER_P} for l in range(nlayers)]
    wsc = {n: K.dram("b_" + n, sh, BF16, "Internal") for n, sh in LAYER_W}
    scr = {n: K.dram("s_" + n, [r, S], BF16, "Internal") for n, r in SCR_FM}
    scr.update({n: K.dram("s_" + n, [S, w], BF16, "Internal") for n, w in SCR_TM})
    hT = [K.dram("s_hT32_%d" % i, [D, S], F32, "Internal") for i in range(2)]
    dbg = {}
    if debug:
        dbg["hT32_dbg"] = K.out("hT32_dbg", [D, S], F32)
    for l in range(nlayers):
        with K.scope():
          if "w" in stages:
            fa = [K.sb([128, 4096], F32) for _ in range(3)]
            fb = [K.sb([128, 4096], BF16) for _ in range(3)]
            cnt = [0]
            for n, sh in LAYER_W:
                conv_w(K, win[l][n], wsc[n], sh[0], sh[1], (fa, fb, cnt))
        io = dict(cst)
        io.update(prm[l])
        io.update(wsc)
        io.update(scr)
        io["x"] = x
        io["out"] = out
        io["hT32"] = hT[l % 2]
        io["hT32_next"] = hT[(l + 1) % 2]
        with K.scope():
          if "A" in stages:
            build_A(K, c, l == 0, io)
        lam_init = 0.8 - 0.6 * math.exp(-0.3 * l)
        with K.scope():
          if "B" in stages:
            build_B(K, c, lam_init, io, do=tuple(x for x, f in (("diff", "d"), ("mla", "m"), ("nsa", "n")) if f in stages) if any(f in stages for f in "dmn") else ("diff", "mla", "nsa"))
        with K.scope():
          if "C" in stages:
            build_C(K, c, (l == nlayers - 1) and not debug, io, l)
    if debug:
        with K.scope():
            t = K.sb([128, 4096], F32)
            src = hT[nlayers % 2]
            for ft in range(8):
                for hh in range(2):
                    K.dma(t[:, :], src[ft * 128:(ft + 1) * 128, hh * 4096:(hh + 1) * 4096])
                    K.outs.append(K.dma(dbg["hT32_dbg"].k((ft, hh))[ft * 128:(ft + 1) * 128, hh * 4096:(hh + 1) * 4096], t[:, :]))
    return K.finish()


def split3(v):
    v = np.asarray(v, np.float32)
    a = v.astype(NPBF); r = v - a.astype(np.float32)
    b = r.astype(NPBF); r2 = r - b.astype(np.float32)
    c = r2.astype(NPBF)
    return [a, b, c]

def slopes_all():
    n = 12
    return (2.0 ** (-8.0 * np.arange(1, n + 1, dtype=np.float32) / n)).astype(np.float32)

def qaug(slope, scale, n=S):
    t = np.arange(n, dtype=np.float32)
    rows = split3(-(np.float32(slope) * t) / np.float32(scale))
    s3 = split3(np.full(n, np.float32(slope) / np.float32(scale), np.float32))
    return np.stack(rows + s3 + s3, 0)

def kaug(pos):
    pos = np.asarray(pos)
    one = np.ones(len(pos), np.float32).astype(NPBF)
    a = (64 * (pos // 64)).astype(np.float32).astype(NPBF)
    b = (pos % 64).astype(np.float32).astype(NPBF)
    return np.stack([one] * 3 + [a] * 3 + [b] * 3, 0)

def perm_uq():
    idx = []
    for h in range(8):
        idx += list(range(96 * h + 64, 96 * h + 96)) + list(range(96 * h, 96 * h + 64))
    return np.array(idx)

def perm_ukv():
    idx = []
    for h in range(8):
        idx += list(range(128 * h, 128 * h + 64))
    for h in range(8):
        idx += list(range(128 * h + 64, 128 * h + 128))
    return np.array(idx)

def pvec(v, nft):
    return np.ascontiguousarray(np.asarray(v, np.float32).reshape(nft, 128).T)

def const_inputs(inp):
    m = {}
    half = 16
    freqs = (10000.0 ** (-np.arange(half, dtype=np.float32) / half)).astype(np.float32)
    ang = np.arange(S, dtype=np.float32)[:, None] * freqs[None, :]
    cos = np.cos(ang).astype(np.float32); sin = np.sin(ang).astype(np.float32)
    m["cosT"] = np.ascontiguousarray(np.concatenate([cos.T, cos.T], 0))
    m["sinT"] = np.ascontiguousarray(np.concatenate([sin.T, sin.T], 0))
    P = np.zeros((32, 32), np.float32)
    for i in range(16):
        P[i, i + 16] = -1.0
        P[i + 16, i] = 1.0
    m["protT"] = np.ascontiguousarray(P.T)
    k = np.arange(128)[:, None]; q = np.arange(512)[None, :]
    m["cm512"] = np.stack([np.where(128 * j + k <= q, 0.0, -1e9).astype(np.float32) for j in range(4)], 0)
    q1 = np.arange(128)[None, :]
    m["wlow"] = np.where(k > q1, 0.0, -1e9).astype(np.float32)
    m["kaug_tok"] = kaug(np.arange(S))
    sl = slopes_all()
    m["qaug_diff"] = np.stack([qaug(sl[8 + h], 0.125) for h in range(4)], 0)
    m["qaug_nsa"] = np.ascontiguousarray(np.stack([qaug(sl[h], 0.125) for h in range(8)], 1))
    m["kaug_cmp"] = kaug(np.arange(512) * 16 + 31)
    cm = np.zeros((2, 16, 128, 128), np.float32)
    cc = np.arange(128)[:, None]; qq = np.arange(128)[None, :]
    for r in range(16):
        vis = (16 * cc + 31 <= 128 * r + qq)
        cm[0, r] = np.where(vis, 0.0, -1e9)
        cm[1, r] = np.where(vis & (cc < 127), 0.0, -1e9)
    m["cmask"] = cm
    cp = np.zeros((128, 128), np.float32); cp[127, :15] = -1e9
    m["cprev"] = cp
    Am = np.zeros((512, 128), np.float32)
    for cidx in range(511):
        for i in (cidx, cidx + 1):
            Am[cidx, i // 4] += 1.0
    m["amat"] = np.ascontiguousarray(Am.reshape(4, 128, 128))
    adm = np.zeros((S // 128, 128, 128), np.float32)
    jj = np.arange(128)[None, :]
    for qb in range(S // 128):
        t = qb * 128 + np.arange(128)[:, None]
        cur = t // 64
        forced = (jj == 0) | (jj == cur) | (jj == cur - 1)
        started = jj * 64 <= t
        adm[qb] = np.where(started, np.where(forced, 1e4, 0.0), -1e4)
    m["addmask"] = adm
    m["E"] = np.ascontiguousarray((np.arange(S)[None, :] // 64 == np.arange(128)[:, None]).astype(np.float32).astype(NPBF))
    oh = np.zeros((12, 12, 64), np.float32)
    for j in range(12):
        oh[j, j, :] = 1.0
    m["oh"] = np.ascontiguousarray(oh.reshape(12, 768).astype(NPBF))
    m["router_w"] = np.ascontiguousarray(inp["router_w"].reshape(8, 128, 16).transpose(1, 0, 2))
    m["router_b"] = np.ascontiguousarray(np.broadcast_to(inp["router_b"].reshape(1, 16), (128, 16)))
    m["ln_g"] = pvec(inp["ln_in_g"], 8); m["ln_b"] = pvec(inp["ln_in_b"], 8)
    return m

def layer_inputs(inp, l):
    m = {}
    m["w_in_%d" % l] = np.ascontiguousarray(inp["w_in"][l])
    m["w_uq_%d" % l] = np.ascontiguousarray(inp["mla_w_uq"][l][:, perm_uq()])
    m["w_ukv_%d" % l] = np.ascontiguousarray(inp["mla_w_ukv"][l][:, perm_ukv()])
    for n in ("w_br_nsa", "w_br_diff", "w_br_mla", "w_out"):
        m["%s_%d" % (n, l)] = np.ascontiguousarray(inp[n][l])
    m["moe_w1_%d" % l] = np.ascontiguousarray(inp["moe_w1"][l].reshape(16 * D, 512))
    m["moe_w3_%d" % l] = np.ascontiguousarray(inp["moe_w3"][l].reshape(16 * D, 512))
    m["moe_w2_%d" % l] = np.ascontiguousarray(inp["moe_w2"][l].reshape(16 * 512, D))
    for n in ("cmp_w1_k", "cmp_w1_v", "cmp_w2_k", "cmp_w2_v"):
        m["%s_%d" % (n, l)] = np.ascontiguousarray(inp[n][l])
    m["qg_%d" % l] = pvec(inp["mla_q_norm_g"][l], 2)
    m["kvg_%d" % l] = pvec(inp["mla_kv_norm_g"][l], 1)
    m["lam_p_%d" % l] = np.ascontiguousarray(np.broadcast_to(inp["diff_lambda"][l].reshape(1, 256), (128, 256)))
    m["subg_%d" % l] = np.ascontiguousarray(inp["diff_subln_g"][l].reshape(128, 1))
    m["posk_%d" % l] = np.ascontiguousarray(inp["cmp_pos_k"][l].T)
    m["posv_%d" % l] = np.ascontiguousarray(inp["cmp_pos_v"][l].T)
    for n in ("ln1_g", "ln1_b", "ln2_g", "ln2_b"):
        m["%s_%d" % (n, l)] = pvec(inp[n][l], 8)
    return m

def all_inputs(inp, nlayers, ncores):
    base = const_inputs(inp)
    for l in range(nlayers):
        base.update(layer_inputs(inp, l))
    maps = []
    for b in range(ncores):
        m = dict(base)
        m["x"] = np.ascontiguousarray(inp["x"][b])
        maps.append(m)
    return maps


def kernel(**inputs):
    inp = {k: np.asarray(v, np.float32) for k, v in inputs.items()}
    nc = build_fused(debug=False, nlayers=2)
    maps = all_inputs(inp, 2, 4)
    res = run_bass_kernel_spmd(nc, maps, core_ids=[0, 1, 2, 3]).results
    out = np.stack([np.asarray(res[b]["out"], np.float32) for b in range(4)], 0)
    return out
```
